# Optimizing a Trainium2 kernel written in Bass

```python
import math
import jax, jax.numpy as jnp
from jax import lax
import numpy as np

D_MODEL = 2048
BATCH = 2
SEQ = 4096
DEPTH = 2

MLA_HEADS = 8
MLA_Q_LORA = 512
MLA_KV_LORA = 512
MLA_NOPE_DIM = 128
MLA_ROPE_DIM = 64
MLA_V_DIM = 128
DSA_HEADS = 8
DSA_HEAD_DIM = 128
DSA_ROPE_DIM = DSA_HEAD_DIM // 4
IDX_HEADS = 16
IDX_DIM = 64
IDX_ROPE_DIM = IDX_DIM // 4
DSA_MAX_TOPK = 256
ROPE_THETA = 500000.0
IN_SPLIT_WIDTHS = (MLA_Q_LORA, MLA_KV_LORA, MLA_ROPE_DIM,
                   DSA_HEADS * DSA_HEAD_DIM, DSA_HEADS * DSA_HEAD_DIM, DSA_HEADS * DSA_HEAD_DIM,
                   IDX_HEADS * IDX_DIM, IDX_DIM, IDX_HEADS,
                   D_MODEL, D_MODEL)
IN_WIDTH = sum(IN_SPLIT_WIDTHS)
N_GROUPS = 8
EXPERTS_PER_GROUP = 8
N_EXPERTS = N_GROUPS * EXPERTS_PER_GROUP
TOPK_IN_GROUP = 2
D_EXPERT = 512
Q_BLOCK = 128
MOE_ROW_BLOCK = 128
DEEPNORM_ALPHA = (2 * DEPTH) ** 0.25
DEEPNORM_BETA = (8 * DEPTH) ** -0.25
LN_EPS = 1e-5
RMS_EPS = 1e-6

kernel_name = "hybrid_mla_dsa_hmoe_deepnorm"


def _layer_norm(x, g, b):
    xf = x.astype(jnp.float32)
    mu = jnp.mean(xf, axis=-1, keepdims=True)
    var = jnp.mean(jnp.square(xf - mu), axis=-1, keepdims=True)
    y = (xf - mu) * lax.rsqrt(var + LN_EPS) * g.astype(jnp.float32) + b.astype(jnp.float32)
    return y.astype(x.dtype)


def _rms_norm(x, g):
    xf = x.astype(jnp.float32)
    y = xf * lax.rsqrt(jnp.mean(jnp.square(xf), axis=-1, keepdims=True) + RMS_EPS) * g.astype(jnp.float32)
    return y.astype(x.dtype)


def _rope_tables(positions, rot_dim):
    inv = ROPE_THETA ** (-jnp.arange(0, rot_dim, 2, dtype=jnp.float32) / rot_dim)
    ang = positions.astype(jnp.float32)[..., None] * inv
    return jnp.cos(ang)[:, :, None, :], jnp.sin(ang)[:, :, None, :]


def _apply_rope(x, cos, sin):
    half = cos.shape[-1]
    rot = 2 * half
    c = cos.astype(x.dtype)
    s = sin.astype(x.dtype)
    x1 = x[..., :half]
    x2 = x[..., half:rot]
    return jnp.concatenate([x1 * c - x2 * s, x2 * c + x1 * s, x[..., rot:]], axis=-1)


def _split_points():
    pts, acc = [], 0
    for w in IN_SPLIT_WIDTHS[:-1]:
        acc += w
        pts.append(acc)
    return pts


def _to_query_blocks(a):
    B, S = a.shape[0], a.shape[1]
    return jnp.swapaxes(a.reshape((B, S // Q_BLOCK, Q_BLOCK) + a.shape[2:]), 0, 1)


def _from_query_blocks(o):
    o = jnp.swapaxes(o, 0, 1)
    B, nb, qb, H, d = o.shape
    return o.reshape(B, nb * qb, H * d)


def _causal_block_attention(q, k, v, positions, scale):
    def one(args):
        qi, pi = args
        s = jnp.einsum('bqhd,bkhd->bhqk', qi, k).astype(jnp.float32) * scale
        mask = positions[:, None, None, :] <= pi[:, None, :, None]
        p = jax.nn.softmax(jnp.where(mask, s, -jnp.inf), axis=-1).astype(v.dtype)
        return jnp.einsum('bhqk,bkhd->bqhd', p, v)
    o = lax.map(one, (_to_query_blocks(q), _to_query_blocks(positions)))
    return _from_query_blocks(o)


def _gather_rows(arr, idx):
    return jax.vmap(lambda a, i: a[i])(arr, idx)


def _indexed_sparse_attention(q, k, v, q_idx, k_idx, w_idx, positions, scale):
    n_keys = k.shape[1]
    top_k = min(DSA_MAX_TOPK, n_keys // 4)

    def one(args):
        qi, qii, wi, pi = args
        rel = jax.nn.relu(jnp.einsum('bqhd,bsd->bqhs', qii, k_idx).astype(jnp.float32))
        score = jnp.einsum('bqhs,bqh->bqs', rel, wi.astype(jnp.float32))
        causal = positions[:, None, :] <= pi[:, :, None]
        score = jnp.where(causal, score, -jnp.inf)
        _, idx = lax.top_k(score, top_k)
        k_sel = _gather_rows(k, idx)
        v_sel = _gather_rows(v, idx)
        p_sel = _gather_rows(positions, idx)
        s = jnp.einsum('bqhd,bqkhd->bhqk', qi, k_sel).astype(jnp.float32) * scale
        valid = (p_sel <= pi[:, :, None])[:, None, :, :]
        p = jax.nn.softmax(jnp.where(valid, s, -jnp.inf), axis=-1).astype(v.dtype)
        return jnp.einsum('bhqk,bqkhd->bqhd', p, v_sel)

    o = lax.map(one, (_to_query_blocks(q), _to_query_blocks(q_idx),
                      _to_query_blocks(w_idx), _to_query_blocks(positions)))
    return _from_query_blocks(o)


def _hybrid_mixer(h, positions, ropes, w_in, g_q_lora, w_q_b, g_kv_lora, w_kv_b, w_o_a, w_o_b, w_out):
    B, S, _ = h.shape
    (cos_mla, sin_mla), (cos_dsa, sin_dsa), (cos_idx, sin_idx) = ropes
    proj = h @ w_in
    (c_q, c_kv, k_pe, q_b, k_b, v_b, q_idx, k_idx, w_idx, gate_a, gate_b) = jnp.split(
        proj, _split_points(), axis=-1)

    q_a = (_rms_norm(c_q, g_q_lora) @ w_q_b).reshape(B, S, MLA_HEADS, MLA_NOPE_DIM + MLA_ROPE_DIM)
    q_nope, q_pe = q_a[..., :MLA_NOPE_DIM], q_a[..., MLA_NOPE_DIM:]
    q_pe = _apply_rope(q_pe, cos_mla, sin_mla)
    kv = (_rms_norm(c_kv, g_kv_lora) @ w_kv_b).reshape(B, S, MLA_HEADS, MLA_NOPE_DIM + MLA_V_DIM)
    k_nope, v_a = kv[..., :MLA_NOPE_DIM], kv[..., MLA_NOPE_DIM:]
    k_pe = _apply_rope(k_pe[:, :, None, :], cos_mla, sin_mla)
    k_a = jnp.concatenate([k_nope, jnp.broadcast_to(k_pe, (B, S, MLA_HEADS, MLA_ROPE_DIM))], axis=-1)
    q_a = jnp.concatenate([q_nope, q_pe], axis=-1)
    o_a = _causal_block_attention(q_a, k_a, v_a, positions,
                                  (MLA_NOPE_DIM + MLA_ROPE_DIM) ** -0.5)

    q_b = _apply_rope(q_b.reshape(B, S, DSA_HEADS, DSA_HEAD_DIM), cos_dsa, sin_dsa)
    k_b = _apply_rope(k_b.reshape(B, S, DSA_HEADS, DSA_HEAD_DIM), cos_dsa, sin_dsa)
    v_b = v_b.reshape(B, S, DSA_HEADS, DSA_HEAD_DIM)
    q_idx = _apply_rope(q_idx.reshape(B, S, IDX_HEADS, IDX_DIM), cos_idx, sin_idx)
    k_idx = _apply_rope(k_idx[:, :, None, :], cos_idx, sin_idx)[:, :, 0, :]
    w_idx = w_idx * ((IDX_HEADS ** -0.5) * (IDX_DIM ** -0.5))
    o_b = _indexed_sparse_attention(q_b, k_b, v_b, q_idx, k_idx, w_idx, positions,
                                    DSA_HEAD_DIM ** -0.5)

    y = jax.nn.sigmoid(gate_a) * (o_a @ w_o_a) + jax.nn.sigmoid(gate_b) * (o_b @ w_o_b)
    return y @ w_out


def _hierarchical_moe(h, w_group, b_group, w_router, b_router, w_gate, w_up, w_down):
    B, S, D = h.shape
    t = h.reshape(-1, D)
    T = t.shape[0]
    g_prob = jax.nn.softmax((t @ w_group + b_group).astype(jnp.float32), axis=-1)
    g_p, g_idx = lax.top_k(g_prob, 1)
    e_logits = (t @ w_router + b_router).astype(jnp.float32).reshape(T, N_GROUPS, EXPERTS_PER_GROUP)
    in_group = jnp.take_along_axis(e_logits, g_idx[:, :, None], axis=1)[:, 0]
    e_p, e_local = lax.top_k(jax.nn.softmax(in_group, axis=-1), TOPK_IN_GROUP)
    e_p = e_p / jnp.sum(e_p, axis=-1, keepdims=True)
    weights = g_p * e_p
    expert_ids = g_idx * EXPERTS_PER_GROUP + e_local

    A = T * TOPK_IN_GROUP
    flat_e = expert_ids.reshape(-1).astype(jnp.int32)
    flat_tok = jnp.repeat(jnp.arange(T, dtype=jnp.int32), TOPK_IN_GROUP)
    flat_w = weights.reshape(-1)
    order = jnp.argsort(flat_e)
    se = flat_e[order]
    counts = jnp.bincount(flat_e, length=N_EXPERTS)
    starts = jnp.cumsum(counts) - counts
    padded = ((counts + MOE_ROW_BLOCK - 1) // MOE_ROW_BLOCK) * MOE_ROW_BLOCK
    pends = jnp.cumsum(padded)
    pstarts = pends - padded
    dest = pstarts[se] + (jnp.arange(A, dtype=jnp.int32) - starts[se])
    n_blocks = -(-A // MOE_ROW_BLOCK) + N_EXPERTS
    P = n_blocks * MOE_ROW_BLOCK
    row_tok = jnp.zeros((P,), jnp.int32).at[dest].set(flat_tok[order])
    row_w = jnp.zeros((P,), jnp.float32).at[dest].set(flat_w[order])
    block_e = jnp.minimum(jnp.searchsorted(pends, jnp.arange(n_blocks) * MOE_ROW_BLOCK, side='right'),
                          N_EXPERTS - 1).astype(jnp.int32)

    def expert_block(args):
        tok, w, e = args
        xb = t[tok]
        hid = jax.nn.silu(xb @ w_gate[e]) * (xb @ w_up[e])
        return ((hid @ w_down[e]) * w[:, None]).astype(t.dtype)

    ys = lax.map(expert_block, (row_tok.reshape(n_blocks, MOE_ROW_BLOCK),
                                row_w.reshape(n_blocks, MOE_ROW_BLOCK), block_e))
    out = jnp.zeros((T, D), t.dtype).at[row_tok].add(ys.reshape(P, D))
    return out.reshape(B, S, D)


def setup_inputs(seed: int = 0) -> dict:
    key = jax.random.key(seed)
    ks = jax.random.split(key, 21)
    f32 = jnp.float32
    n = lambda k, shape, scale: jax.random.normal(k, shape, f32) * scale
    x = jax.random.normal(ks[0], (BATCH, SEQ, D_MODEL), f32)
    offsets = jax.random.randint(ks[1], (BATCH, 1), 0, 2048, dtype=jnp.int32)
    positions = (offsets + jnp.arange(SEQ, dtype=jnp.int32)[None, :]).astype(jnp.int32)
    return {
        "x": x,
        "positions": positions,
        "w_in": n(ks[2], (DEPTH, D_MODEL, IN_WIDTH), D_MODEL ** -0.5),
        "g_q_lora": 1.0 + n(ks[3], (DEPTH, MLA_Q_LORA), 0.02),
        "w_q_b": n(ks[4], (DEPTH, MLA_Q_LORA, MLA_HEADS * (MLA_NOPE_DIM + MLA_ROPE_DIM)), MLA_Q_LORA ** -0.5),
        "g_kv_lora": 1.0 + n(ks[5], (DEPTH, MLA_KV_LORA), 0.02),
        "w_kv_b": n(ks[6], (DEPTH, MLA_KV_LORA, MLA_HEADS * (MLA_NOPE_DIM + MLA_V_DIM)), MLA_KV_LORA ** -0.5),
        "w_o_a": n(ks[7], (DEPTH, MLA_HEADS * MLA_V_DIM, D_MODEL), (MLA_HEADS * MLA_V_DIM) ** -0.5),
        "w_o_b": n(ks[8], (DEPTH, DSA_HEADS * DSA_HEAD_DIM, D_MODEL), (DSA_HEADS * DSA_HEAD_DIM) ** -0.5),
        "w_out": n(ks[9], (DEPTH, D_MODEL, D_MODEL), DEEPNORM_BETA * D_MODEL ** -0.5),
        "ln1_g": 1.0 + n(ks[10], (DEPTH, D_MODEL), 0.02),
        "ln1_b": n(ks[11], (DEPTH, D_MODEL), 0.02),
        "w_group": n(ks[12], (DEPTH, D_MODEL, N_GROUPS), D_MODEL ** -0.5),
        "b_group": n(ks[13], (DEPTH, N_GROUPS), 0.01),
        "w_router": n(ks[14], (DEPTH, D_MODEL, N_EXPERTS), D_MODEL ** -0.5),
        "b_router": n(ks[15], (DEPTH, N_EXPERTS), 0.01),
        "w_e_gate": n(ks[16], (DEPTH, N_EXPERTS, D_MODEL, D_EXPERT), D_MODEL ** -0.5),
        "w_e_up": n(ks[17], (DEPTH, N_EXPERTS, D_MODEL, D_EXPERT), D_MODEL ** -0.5),
        "w_e_down": n(ks[18], (DEPTH, N_EXPERTS, D_EXPERT, D_MODEL), DEEPNORM_BETA * D_EXPERT ** -0.5),
        "ln2_g": 1.0 + n(ks[19], (DEPTH, D_MODEL), 0.02),
        "ln2_b": n(ks[20], (DEPTH, D_MODEL), 0.02),
    }


def reference(x, positions, w_in, g_q_lora, w_q_b, g_kv_lora, w_kv_b, w_o_a, w_o_b, w_out,
              ln1_g, ln1_b, w_group, b_group, w_router, b_router, w_e_gate, w_e_up, w_e_down,
              ln2_g, ln2_b):
    ropes = (_rope_tables(positions, MLA_ROPE_DIM),
             _rope_tables(positions, DSA_ROPE_DIM),
             _rope_tables(positions, IDX_ROPE_DIM))
    h = x
    for l in range(DEPTH):
        mix = _hybrid_mixer(h, positions, ropes, w_in[l], g_q_lora[l], w_q_b[l], g_kv_lora[l],
                            w_kv_b[l], w_o_a[l], w_o_b[l], w_out[l])
        h = _layer_norm(DEEPNORM_ALPHA * h + mix, ln1_g[l], ln1_b[l])
        ff = _hierarchical_moe(h, w_group[l], b_group[l], w_router[l], b_router[l],
                               w_e_gate[l], w_e_up[l], w_e_down[l])
        h = _layer_norm(DEEPNORM_ALPHA * h + ff, ln2_g[l], ln2_b[l])
    return h
```

```python
import math
import numpy as np
from contextlib import ExitStack
import ml_dtypes
import concourse.bass as bass
import concourse.mybir as mybir
from concourse.bass_utils import run_bass_kernel_spmd

F32 = mybir.dt.float32; BF16 = mybir.dt.bfloat16; I32 = mybir.dt.int32
AF = mybir.ActivationFunctionType; ALU = mybir.AluOpType; AX = mybir.AxisListType
NPBF = ml_dtypes.bfloat16

D = 2048; TT = 1024; NCORE = 8; SEQ = 4096
IN_W = 9360
C_KPE = 1024; C_QB = 1088; C_KB = 2112; C_VB = 3136; C_QI = 4160; C_KI = 5184; C_WI = 5248; C_GA = 5264; C_GB = 7312
ALPHA = 4 ** 0.25
THETA = 500000.0


class T:
    __slots__ = ("ap", "w", "r", "name", "excl")

    def __init__(self, ap, name="", excl=False):
        self.ap = ap; self.w = None; self.r = {}; self.name = name; self.excl = excl

    def __getitem__(self, k):
        return self.ap[k]


class E:
    def __init__(self, name, obj, sem):
        self.name = name; self.obj = obj; self.sem = sem; self.count = 0; self.seen = {}


class Ctx:
    NDMASEM = 8

    def __init__(self):
        self.nc = nc = bass.Bass("TRN2", target_bir_lowering=False)
        self.es = ExitStack()
        self.eng = {}
        for name, obj in (("pe", nc.tensor), ("act", nc.scalar), ("dve", nc.vector), ("pool", nc.gpsimd), ("sp", nc.sync)):
            sem = self.es.enter_context(nc.semaphore("s_" + name))
            self.eng[name] = E(name, obj, sem)
        self.dq = {}
        for q in ("sp", "pool", "act"):
            lst = []
            for i in range(self.NDMASEM):
                sem = self.es.enter_context(nc.semaphore(f"d_{q}{i}"))
                lst.append(E(f"d_{q}{i}", None, sem))
            self.dq[q] = [lst, 0]
        self.ntile = 0
        self.rr = 0

    def sb(self, shape, dt, name=None):
        self.ntile += 1
        name = "sb_" + (name or f"t{self.ntile}")
        h = self.es.enter_context(self.nc.sbuf_tensor(name, list(shape), dt))
        return T(h, name)

    def ps(self, shape, dt=F32, name=None):
        self.ntile += 1
        name = "ps_" + (name or f"p{self.ntile}")
        h = self.es.enter_context(self.nc.psum_tensor(name, list(shape), dt))
        return T(h, name, excl=True)

    def dram(self, name, shape, dt, kind="Internal"):
        h = self.nc.dram_tensor(name, list(shape), dt, kind=kind)
        return T(h.ap(), name)

    def _deps(self, e, reads, writes, extra=()):
        deps = {}

        def add(d):
            if d is None:
                return
            p, idx = d
            if p is e and e.name in ("pe", "sp"):
                return
            if deps.get(p, 0) < idx:
                deps[p] = idx
        for t in reads:
            add(t.w)
            if t.excl:
                for p, idx in t.r.items():
                    add((p, idx))
        for t in writes:
            add(t.w)
            for p, idx in t.r.items():
                add((p, idx))
        for d in extra:
            add(d)
        for p, idx in deps.items():
            if e.seen.get(p, 0) < idx:
                e.obj.wait_ge(p.sem, idx)
                e.seen[p] = idx

    def op(self, en, fn, reads=(), writes=()):
        e = self.eng[en]
        self._deps(e, reads, writes)
        inst = fn(e.obj)
        e.count += 1
        inst.then_inc(e.sem, 1)
        for t in writes:
            t.w = (e, e.count); t.r = {}
        for t in reads:
            t.r[e] = e.count
        return inst

    def dma(self, q, out, in_, reads=(), writes=(), **kw):
        e = self.eng[q]
        lst, i = self.dq[q]
        d = lst[i % len(lst)]; self.dq[q][1] = i + 1
        extra = [(d, d.count)] if d.count else []
        self._deps(e, reads, writes, extra)
        inst = e.obj.dma_start(out=out, in_=in_, **kw)
        d.count += 16
        inst.then_inc(d.sem, 16)
        for t in writes:
            t.w = (d, d.count); t.r = {}
        for t in reads:
            t.r[d] = d.count
        return inst

    def finish(self):
        e = self.eng["sp"]
        for p in list(self.eng.values()):
            if p is not e and p.count and e.seen.get(p, 0) < p.count:
                e.obj.wait_ge(p.sem, p.count); e.seen[p] = p.count
        for q, (lst, _) in self.dq.items():
            for d in lst:
                if d.count and e.seen.get(d, 0) < d.count:
                    e.obj.wait_ge(d.sem, d.count); e.seen[d] = d.count
        self.es.close()

    def alt(self, engs=("dve", "act")):
        self.rr += 1
        return engs[self.rr % len(engs)]


def _rope_consts():
    inv = np.zeros((128, 3), np.float32)
    perm = np.zeros((3, 128, 128), np.float32)
    f = THETA ** (-np.arange(0, 64, 2, dtype=np.float32) / 64.0)
    inv[0:32, 0] = f; inv[32:64, 0] = f
    for i in range(32):
        perm[0, 32 + i, i] = -1.0; perm[0, i, 32 + i] = 1.0
    f = THETA ** (-np.arange(0, 32, 2, dtype=np.float32) / 32.0)
    inv[0:16, 1] = f; inv[16:32, 1] = f
    for i in range(16):
        perm[1, 16 + i, i] = -1.0; perm[1, i, 16 + i] = 1.0
    f = THETA ** (-np.arange(0, 16, 2, dtype=np.float32) / 16.0)
    for base in (0, 64):
        inv[base:base + 8, 2] = f; inv[base + 8:base + 16, 2] = f
        for i in range(8):
            perm[2, base + 8 + i, base + i] = -1.0; perm[2, base + i, base + 8 + i] = 1.0
    inv = (inv.astype(np.float64) / (2 * np.pi)).astype(np.float32)
    return inv, perm


def build_A(stage=99):
    c = Ctx(); nc = c.nc
    hT_d = c.dram("hT", [D, TT], F32, "ExternalInput")
    pos_d = c.dram("pos", [1, TT], I32, "ExternalInput")
    win_d = c.dram("w_in", [D, IN_W], F32, "ExternalInput")
    gq_d = c.dram("g_q", [512], F32, "ExternalInput")
    wqb_d = c.dram("w_q_b", [512, 1536], F32, "ExternalInput")
    gkv_d = c.dram("g_kv", [512], F32, "ExternalInput")
    wkvb_d = c.dram("w_kv_b", [512, 2048], F32, "ExternalInput")
    inv_d = c.dram("inv", [128, 3], F32, "ExternalInput")
    perm_d = c.dram("perm", [3, 128, 128], F32, "ExternalInput")
    outs = {}
    for nm, shp, dt in (("qaT", [8, 192, TT], BF16), ("kaT", [8, 128, TT], BF16), ("kpeT", [64, TT], BF16),
                        ("vaT", [8, 128, TT], BF16), ("qbT", [8, 128, TT], BF16), ("kbT", [8, 128, TT], BF16),
                        ("vbT", [8, 128, TT], BF16), ("qiT", [8, 128, TT], BF16), ("kiT", [64, TT], BF16),
                        ("widxT", [16, TT], F32)):
        outs[nm] = c.dram(nm, shp, dt, "ExternalOutput")

    hTb = c.sb([128, 16, TT], BF16, "hTb")
    for i in range(4):
        c.dma("pool", hTb[:, 4 * i:4 * i + 4, :], hT_d.ap[512 * i:512 * (i + 1), :].rearrange("(kc p) t -> p kc t", p=128),
              reads=[hT_d], writes=[hTb])
    inv = c.sb([128, 3], F32, "inv"); c.dma("sp", inv[:], inv_d[:], reads=[inv_d], writes=[inv])
    perm = c.sb([128, 3, 128], F32, "perm")
    c.dma("sp", perm[:], perm_d.ap.rearrange("r k m -> k r m"), reads=[perm_d], writes=[perm])
    gq = c.sb([128, 4], F32, "gq"); gkv = c.sb([128, 4], F32, "gkv")
    with nc.allow_non_contiguous_dma(reason="tiny gain vectors"):
        c.dma("sp", gq[:], gq_d.ap.rearrange("(c p) -> p c", p=128), reads=[gq_d], writes=[gq])
        c.dma("sp", gkv[:], gkv_d.ap.rearrange("(c p) -> p c", p=128), reads=[gkv_d], writes=[gkv])
    ones = c.sb([128, 128], BF16, "ones")
    c.op("pool", lambda e: e.memset(ones[:], 1.0), writes=[ones])

    if stage < 1:
        c.finish(); return nc
    posi = c.sb([128, TT], I32, "posi")
    c.dma("sp", posi[:], pos_d.ap[0, :].partition_broadcast(128), reads=[pos_d], writes=[posi])
    posf = c.sb([128, TT], F32, "posf")
    c.op("dve", lambda e: e.tensor_copy(out=posf[:], in_=posi[:]), reads=[posi], writes=[posf])
    CC = c.sb([128, 3, TT], F32, "CC"); SS = c.sb([128, 3, TT], F32, "SS")
    u = c.sb([128, TT], F32, "u"); ui = c.sb([128, TT], I32, "ui"); uf = c.sb([128, TT], F32, "uf")
    for r in range(3):
        for which, dst in ((0, SS), (1, CC)):
            c.op("dve", lambda e: e.tensor_scalar(out=u[:], in0=posf[:], scalar1=inv[:, r:r + 1], scalar2=0.25 * which,
                                                  op0=ALU.mult, op1=ALU.add), reads=[posf, inv], writes=[u])
            c.op("dve", lambda e: e.tensor_copy(out=ui[:], in_=u[:]), reads=[u], writes=[ui])
            c.op("dve", lambda e: e.tensor_copy(out=uf[:], in_=ui[:]), reads=[ui], writes=[uf])
            c.op("dve", lambda e: e.tensor_tensor(out=u[:], in0=u[:], in1=uf[:], op=ALU.subtract), reads=[u, uf], writes=[u])
            c.op("dve", lambda e: e.scalar_tensor_tensor(out=uf[:], in0=u[:], scalar=0.5, in1=u[:], op0=ALU.is_gt, op1=ALU.subtract),
                 reads=[u], writes=[uf])
            c.op("act", lambda e: e.activation(out=dst[:, r, :], in_=uf[:], func=AF.Sin, scale=-6.283185), reads=[uf], writes=[dst])

    if stage < 2:
        c.finish(); return nc
    wbufs = [c.sb([128, 16, 512], BF16, f"wbuf{i}") for i in range(2)]
    accs = [c.ps([128, TT], F32, f"acc{i}") for i in range(2)]
    rot_ps = c.ps([128, TT], F32, "rot_ps")
    rep_ps = c.ps([128, TT], F32, "rep_ps")
    stg = [c.sb([128, TT], BF16, f"stg{i}") for i in range(3)]
    pf = [c.sb([128, TT], F32, f"pf{i}") for i in range(2)]
    t1 = [c.sb([128, TT], F32, f"t1_{i}") for i in range(2)]
    st = {"w": 0, "a": 0, "s": 0, "p": 0}

    def linear(w_d, K, groups, rhs):
        KC = K // 128
        for (col0, ncols, chunks) in groups:
            wb = wbufs[st["w"] % 2]; st["w"] += 1
            c.dma("pool", wb[:, 0:KC, 0:ncols], w_d.ap[:, col0:col0 + ncols].rearrange("(kc p) m -> p kc m", p=128),
                  reads=[w_d], writes=[wb])
            if stage == 20:
                continue
            for (off, M, epi) in chunks:
                acc = accs[st["a"] % 2]; st["a"] += 1
                for th in range(2):
                    for kc in range(KC):
                        c.op("pe", lambda e: e.matmul(acc[0:M, th * 512:(th + 1) * 512], lhsT=wb[:, kc, off:off + M],
                                                      rhs=rhs[:, kc, th * 512:(th + 1) * 512], start=(kc == 0), stop=(kc == KC - 1)),
                             reads=[wb, rhs], writes=[acc])
                if stage == 21:
                    continue
                epi(acc, M)

    def epi_plain(dst_ap, dst_t, rep=None, scale=None, dt=BF16):
        def f(acc, M):
            s = stg[st["s"] % 3]; st["s"] += 1
            if dt == F32:
                s = pf[st["p"] % 2]; st["p"] += 1
            if rep is not None:
                c.op("dve", lambda e: e.tensor_tensor(out=s[0:M, :], in0=acc[0:M, :], in1=rep[0:M, :], op=ALU.mult),
                     reads=[acc, rep], writes=[s])
            elif scale is not None:
                c.op("act", lambda e: e.activation(out=s[0:M, :], in_=acc[0:M, :], func=AF.Copy, scale=scale), reads=[acc], writes=[s])
            else:
                en = c.alt()
                if en == "act":
                    c.op("act", lambda e: e.activation(out=s[0:M, :], in_=acc[0:M, :], func=AF.Copy), reads=[acc], writes=[s])
                else:
                    c.op("dve", lambda e: e.tensor_copy(out=s[0:M, :], in_=acc[0:M, :]), reads=[acc], writes=[s])
            c.dma("sp", dst_ap, s[0:M, :], reads=[s], writes=[dst_t])
        return f

    def epi_rope(r, dst_ap, dst_t, rep=None):
        def f(acc, M):
            p = pf[st["p"] % 2]; q = t1[st["p"] % 2]; st["p"] += 1
            if rep is not None:
                c.op("dve", lambda e: e.tensor_tensor(out=p[0:M, :], in0=acc[0:M, :], in1=rep[0:M, :], op=ALU.mult),
                     reads=[acc, rep], writes=[p])
            else:
                c.op("act", lambda e: e.activation(out=p[0:M, :], in_=acc[0:M, :], func=AF.Copy), reads=[acc], writes=[p])
            for th in range(2):
                c.op("pe", lambda e: e.matmul(rot_ps[0:M, th * 512:(th + 1) * 512], lhsT=perm[0:M, r, 0:M],
                                              rhs=p[0:M, th * 512:(th + 1) * 512], start=True, stop=True),
                     reads=[perm, p], writes=[rot_ps])
            c.op("dve", lambda e: e.tensor_tensor(out=q[0:M, :], in0=rot_ps[0:M, :], in1=SS[0:M, r, :], op=ALU.mult),
                 reads=[rot_ps, SS], writes=[q])
            c.op("pool", lambda e: e.tensor_tensor(out=p[0:M, :], in0=p[0:M, :], in1=CC[0:M, r, :], op=ALU.mult),
                 reads=[p, CC], writes=[p])
            s = stg[st["s"] % 3]; st["s"] += 1
            c.op("dve", lambda e: e.tensor_tensor(out=s[0:M, :], in0=p[0:M, :], in1=q[0:M, :], op=ALU.add),
                 reads=[p, q], writes=[s])
            c.dma("sp", dst_ap, s[0:M, :], reads=[s], writes=[dst_t])
        return f

    cn = {"q": c.sb([128, 4, TT], BF16, "cqn"), "kv": c.sb([128, 4, TT], BF16, "ckvn")}
    sq = c.sb([128, 4, TT], BF16, "sq")
    rstd = {"q": c.sb([128, TT], F32, "rstd_q"), "kv": c.sb([128, TT], F32, "rstd_kv")}
    eps_t = c.sb([128, 1], F32, "eps")
    c.op("pool", lambda e: e.memset(eps_t[:], 1e-6), writes=[eps_t])

    def epi_rms(which, i, g):
        def f(acc, M):
            if stage != 222:
                c.op("act", lambda e: e.activation(out=sq[:, i, :], in_=acc[:, :], func=AF.Square), reads=[acc], writes=[sq])
            if stage == 221:
                return
            c.op("dve", lambda e: e.tensor_scalar(out=cn[which][:, i, :], in0=acc[:, :], scalar1=g[:, i:i + 1], scalar2=None, op0=ALU.mult),
                 reads=[acc, g], writes=[cn[which]])
        return f

    for which, col0, g in (("q", 0, gq), ("kv", 512, gkv)):
        linear(win_d, D, [(col0, 512, [(128 * i, 128, epi_rms(which, i, g)) for i in range(4)])], hTb)
        if stage in (20, 21, 22, 221, 222):
            continue
        for th in range(2):
            for i in range(4):
                c.op("pe", lambda e: e.matmul(rep_ps[:, th * 512:(th + 1) * 512], lhsT=ones[:, :], rhs=sq[:, i, th * 512:(th + 1) * 512],
                                              start=(i == 0), stop=(i == 3)), reads=[ones, sq], writes=[rep_ps])
        if stage == 23:
            continue
        c.op("act", lambda e: e.activation(out=rstd[which][:], in_=rep_ps[:], func=AF.Sqrt, scale=1.0 / 512.0, bias=eps_t[:, 0:1]),
             reads=[rep_ps, eps_t], writes=[rstd[which]])
        c.op("dve", lambda e: e.reciprocal(out=rstd[which][:], in_=rstd[which][:]), reads=[rstd[which]], writes=[rstd[which]])

    if stage < 3:
        c.finish(); return nc
    o = outs
    linear(win_d, D, [(C_KPE, 64, [(0, 64, epi_rope(0, o["kpeT"][:, :], o["kpeT"]))])], hTb)
    if stage < 4:
        c.finish(); return nc
    for g in range(2):
        linear(win_d, D, [(C_QB + 512 * g, 512, [(128 * j, 128, epi_rope(1, o["qbT"][4 * g + j, :, :], o["qbT"])) for j in range(4)])], hTb)
    for g in range(2):
        linear(win_d, D, [(C_KB + 512 * g, 512, [(128 * j, 128, epi_rope(1, o["kbT"][4 * g + j, :, :], o["kbT"])) for j in range(4)])], hTb)
    for g in range(2):
        linear(win_d, D, [(C_VB + 512 * g, 512, [(128 * j, 128, epi_plain(o["vbT"][4 * g + j, :, :], o["vbT"])) for j in range(4)])], hTb)
    for g in range(2):
        linear(win_d, D, [(C_QI + 512 * g, 512, [(128 * j, 128, epi_rope(2, o["qiT"][4 * g + j, :, :], o["qiT"])) for j in range(4)])], hTb)
    linear(win_d, D, [(C_KI, 80, [(0, 64, epi_rope(2, o["kiT"][:, :], o["kiT"])),
                                  (64, 16, epi_plain(o["widxT"][:, :], o["widxT"], scale=1.0 / 32.0, dt=F32))])], hTb)
    if stage < 5:
        c.finish(); return nc
    for g in range(4):
        chunks = []
        for j in range(2):
            h = 2 * g + j
            chunks.append((192 * j, 128, epi_plain(o["qaT"][h, 0:128, :], o["qaT"], rep=rstd["q"])))
            chunks.append((192 * j + 128, 64, epi_rope(0, o["qaT"][h, 128:192, :], o["qaT"], rep=rstd["q"])))
        linear(wqb_d, 512, [(384 * g, 384, chunks)], cn["q"])
    for g in range(4):
        chunks = []
        for j in range(2):
            h = 2 * g + j
            chunks.append((256 * j, 128, epi_plain(o["kaT"][h, :, :], o["kaT"], rep=rstd["kv"])))
            chunks.append((256 * j + 128, 128, epi_plain(o["vaT"][h, :, :], o["vaT"], rep=rstd["kv"])))
        linear(wkvb_d, 512, [(512 * g, 512, chunks)], cn["kv"])
    c.finish()
    return nc


_CACHE = {}


def _get(name, fn):
    if name not in _CACHE:
        _CACHE[name] = fn()
    return _CACHE[name]


def run_A(hT_list, pos_list, w_in, g_q, w_q_b, g_kv, w_kv_b):
    nc = _get("A", build_A)
    inv, perm = _rope_consts()
    in_maps = []
    for ci in range(len(hT_list)):
        in_maps.append({"hT": hT_list[ci], "pos": pos_list[ci], "w_in": w_in, "g_q": g_q, "w_q_b": w_q_b,
                        "g_kv": g_kv, "w_kv_b": w_kv_b, "inv": inv, "perm": perm})
    res = run_bass_kernel_spmd(nc, in_maps, core_ids=list(range(len(hT_list))))
    return res.results


NIT = 18
TOPK = 256


def build_B1(do_mla=True, do_dsa=True):
    c = Ctx(); nc = c.nc
    NK = SEQ; NKC = NK // 128
    qaT_d = c.dram("qaT", [8, 192, TT], BF16, "ExternalInput")
    qbT_d = c.dram("qbT", [8, 128, TT], BF16, "ExternalInput")
    qiT_d = c.dram("qiT", [8, 128, TT], BF16, "ExternalInput")
    widx_d = c.dram("widx", [TT, 16], F32, "ExternalInput")
    kaT_d = c.dram("kaT", [8, 128, NK], BF16, "ExternalInput")
    kpeT_d = c.dram("kpeT", [64, NK], BF16, "ExternalInput")
    va_d = c.dram("va", [NK, 1024], BF16, "ExternalInput")
    kbT_d = c.dram("kbT", [8, 128, NK], BF16, "ExternalInput")
    vb_d = c.dram("vb", [NK, 1024], BF16, "ExternalInput")
    kiT_d = c.dram("kiT", [64, NK], BF16, "ExternalInput")
    posq_d = c.dram("posq", [1, TT], I32, "ExternalInput")
    posk_d = c.dram("posk", [1, NK], I32, "ExternalInput")
    ident_d = c.dram("ident", [128, 128], BF16, "ExternalInput")
    oaT_d = c.dram("oaT", [8, 128, TT], BF16, "ExternalOutput")
    obT_d = c.dram("obT", [8, 128, TT], BF16, "ExternalOutput")
    dbg_d = c.dram("dbg", [8, 128, 4], F32, "ExternalOutput")

    sc = c.sb([128, NK], F32, "sc")
    tmpi = sc[:, :].bitcast(I32)
    posq_bc = c.sb([128, TT], F32, "posq_bc")
    c.dma("sp", tmpi[:, 0:TT], posq_d.ap[0, :].partition_broadcast(128), reads=[posq_d], writes=[sc])
    c.op("dve", lambda e: e.tensor_copy(out=posq_bc[:], in_=tmpi[:, 0:TT]), reads=[sc], writes=[posq_bc])
    posk_bc = c.sb([128, NK], F32, "posk_bc")
    c.dma("sp", tmpi[:, :], posk_d.ap[0, :].partition_broadcast(128), reads=[posk_d], writes=[sc])
    c.op("dve", lambda e: e.tensor_copy(out=posk_bc[:], in_=tmpi[:, :]), reads=[sc], writes=[posk_bc])
    pci = c.sb([128, 40], I32, "pci"); posk_col = c.sb([128, NKC], F32, "posk_col"); posq_col = c.sb([128, 8], F32, "posq_col")
    with nc.allow_non_contiguous_dma(reason="tiny position columns"):
        c.dma("sp", pci[:, 0:NKC], posk_d.ap[0, :].rearrange("(kc p) -> p kc", p=128), reads=[posk_d], writes=[pci])
        c.dma("sp", pci[:, 32:40], posq_d.ap[0, :].rearrange("(kc p) -> p kc", p=128), reads=[posq_d], writes=[pci])
    c.op("dve", lambda e: e.tensor_copy(out=posk_col[:], in_=pci[:, 0:NKC]), reads=[pci], writes=[posk_col])
    c.op("dve", lambda e: e.tensor_copy(out=posq_col[:], in_=pci[:, 32:40]), reads=[pci], writes=[posq_col])
    ones = c.sb([128, 128], BF16, "ones")
    c.op("pool", lambda e: e.memset(ones[:], 1.0), writes=[ones])
    ident = c.sb([128, 128], BF16, "ident")
    c.dma("sp", ident[:], ident_d[:], reads=[ident_d], writes=[ident])

    KT = [c.sb([128, NK], BF16, f"KT{i}") for i in range(2)]
    VV = [c.sb([128, NKC, 128], BF16, f"VV{i}") for i in range(2)]
    QT = [c.sb([128, TT], BF16, f"QT{i}") for i in range(2)]
    QP = [c.sb([64, TT], BF16, f"QP{i}") for i in range(2)]
    kpe = c.sb([64, NK], BF16, "kpe")
    pts = [c.sb([128, 512], BF16, f"pt{i}") for i in range(3)]
    S_ps = [c.ps([128, 512], F32, f"S{i}") for i in range(3)]
    acc_ps = [c.ps([128, 512], F32, f"acc{i}") for i in range(1)] * 2
    den_ps = [c.ps([128, 512], F32, f"den{i}") for i in range(1)] * 2
    misc_ps = c.ps([128, 512], F32, "misc")
    rden = c.sb([128, 512], F32, "rden")
    ost = [c.sb([128, 512], BF16, f"ost{i}") for i in range(2)]
    maskB = c.sb([128, NKC, TT], BF16, "maskB")
    st = {"s": 0, "a": 0, "p": 0, "o": 0, "b": 0}

    def attention_head(h, mla):
        b = st["b"] % 2; st["b"] += 1
        kt, vv, qt_, qp = KT[b], VV[b], QT[b], QP[b]
        if mla:
            c.dma("sp", kt[:], kaT_d[h, :, :], reads=[kaT_d], writes=[kt])
            c.dma("sp", vv[:], va_d.ap[:, h * 128:(h + 1) * 128].rearrange("(kc p) d -> p kc d", p=128), reads=[va_d], writes=[vv])
            c.dma("sp", qt_[:], qaT_d[h, 0:128, :], reads=[qaT_d], writes=[qt_])
            c.dma("sp", qp[:], qaT_d[h, 128:192, :], reads=[qaT_d], writes=[qp])
            scale = 192.0 ** -0.5
        else:
            c.dma("sp", kt[:], kbT_d[h, :, :], reads=[kbT_d], writes=[kt])
            c.dma("sp", vv[:], vb_d.ap[:, h * 128:(h + 1) * 128].rearrange("(kc p) d -> p kc d", p=128), reads=[vb_d], writes=[vv])
            c.dma("sp", qt_[:], qbT_d[h, :, :], reads=[qbT_d], writes=[qt_])
            scale = 128.0 ** -0.5
        for qh in range(2):
            qs = slice(qh * 512, (qh + 1) * 512)
            acc = acc_ps[st["a"] % 2]; den = den_ps[st["a"] % 2]; st["a"] += 1
            for kc in range(NKC):
                ks = slice(kc * 128, (kc + 1) * 128)
                S = S_ps[st["s"] % 3]; st["s"] += 1
                pt = pts[st["p"] % 3]; st["p"] += 1
                c.op("pe", lambda e: e.matmul(S[:], lhsT=kt[:, ks], rhs=qt_[:, qs], start=True, stop=not mla), reads=[kt, qt_], writes=[S])
                if mla:
                    c.op("pe", lambda e: e.matmul(S[:], lhsT=kpe[0:64, ks], rhs=qp[0:64, qs], start=False, stop=True), reads=[kpe, qp], writes=[S])
                c.op("act", lambda e: e.activation(out=pt[:], in_=S[:], func=AF.Exp, scale=scale), reads=[S], writes=[pt])
                if mla:
                    c.op("dve", lambda e: e.scalar_tensor_tensor(out=pt[:], in0=posq_bc[:, qs], scalar=posk_col[:, kc:kc + 1], in1=pt[:],
                                                                 op0=ALU.is_ge, op1=ALU.mult), reads=[posq_bc, posk_col, pt], writes=[pt])
                else:
                    c.op("pool", lambda e: e.tensor_tensor(out=pt[:], in0=pt[:], in1=maskB[:, kc, qs], op=ALU.mult), reads=[pt, maskB], writes=[pt])
                c.op("pe", lambda e: e.matmul(acc[:], lhsT=vv[:, kc, :], rhs=pt[:], start=(kc == 0), stop=(kc == NKC - 1)), reads=[vv, pt], writes=[acc])
                c.op("pe", lambda e: e.matmul(den[:], lhsT=ones[:], rhs=pt[:], start=(kc == 0), stop=(kc == NKC - 1)), reads=[ones, pt], writes=[den])
            c.op("dve", lambda e: e.reciprocal(out=rden[:], in_=den[:]), reads=[den], writes=[rden])
            o = ost[st["o"] % 2]; st["o"] += 1
            c.op("dve", lambda e: e.tensor_tensor(out=o[:], in0=acc[:], in1=rden[:], op=ALU.mult), reads=[acc, rden], writes=[o])
            dst = oaT_d if mla else obT_d
            c.dma("sp", dst[h, :, qs], o[:], reads=[o], writes=[dst])

    if do_dsa:
        ki2 = c.sb([128, NK], BF16, "ki2")
        c.dma("sp", ki2[0:64, :], kiT_d[:, :], reads=[kiT_d], writes=[ki2])
        c.dma("sp", ki2[64:128, :], kiT_d[:, :], reads=[kiT_d], writes=[ki2])
        qi = c.sb([128, 8, TT], BF16, "qi")
        c.dma("sp", qi[:], qiT_d.ap.rearrange("g p t -> p g t"), reads=[qiT_d], writes=[qi])
        wcol = c.sb([128, 8, 16], F32, "wcol")
        c.dma("sp", wcol[:], widx_d.ap.rearrange("(qb p) h -> p qb h", p=128), reads=[widx_d], writes=[wcol])
        junk = c.sb([128, NK], BF16, "junk")
        rb = [c.sb([128, 512], F32, f"rb{i}") for i in range(3)]
        I_ps = [misc_ps, c.ps([128, 512], F32, "misc2")]
        sm = {k: c.sb([128, 1], F32, "sm_" + k) for k in ("lo", "hi", "mid", "cnt", "ge", "d1", "d2", "am")}
        ri = {"r": 0, "i": 0}

        def indexer(qb):
            qs = slice(qb * 128, (qb + 1) * 128)
            for kt_ in range(NK // 512):
                ksl = slice(kt_ * 512, (kt_ + 1) * 512)
                for h in range(16):
                    pb = 64 * (h % 2)
                    ip = I_ps[ri["i"] % 2]; ri["i"] += 1
                    r = rb[ri["r"] % 3]; ri["r"] += 1
                    c.op("pe", lambda e: e.matmul(ip[:], lhsT=qi[pb:pb + 64, h // 2, qs], rhs=ki2[pb:pb + 64, ksl], start=True, stop=True),
                         reads=[qi, ki2], writes=[ip])
                    c.op("act", lambda e: e.activation(out=r[:], in_=ip[:], func=AF.Relu), reads=[ip], writes=[r])
                    if h == 0:
                        c.op("dve", lambda e: e.tensor_scalar(out=sc[:, ksl], in0=r[:], scalar1=wcol[:, qb, 0:1], scalar2=None, op0=ALU.mult),
                             reads=[r, wcol], writes=[sc])
                    else:
                        c.op("dve", lambda e: e.scalar_tensor_tensor(out=sc[:, ksl], in0=r[:], scalar=wcol[:, qb, h:h + 1], in1=sc[:, ksl],
                                                                     op0=ALU.mult, op1=ALU.add), reads=[r, wcol, sc], writes=[sc])
            c.op("dve", lambda e: e.tensor_reduce(out=sm["am"][:], in_=sc[:], axis=AX.X, op=ALU.max, apply_absolute_value=True),
                 reads=[sc], writes=[sm["am"]])
            c.op("dve", lambda e: e.tensor_scalar(out=sm["hi"][:], in0=sm["am"][:], scalar1=1.001, scalar2=1e-6, op0=ALU.mult, op1=ALU.add),
                 reads=[sm["am"]], writes=[sm["hi"]])
            c.op("dve", lambda e: e.tensor_scalar(out=sm["lo"][:], in0=sm["hi"][:], scalar1=-1.0, scalar2=None, op0=ALU.mult),
                 reads=[sm["hi"]], writes=[sm["lo"]])
            c.op("pool", lambda e: e.tensor_scalar(out=junk[:], in0=posk_bc[:], scalar1=posq_col[:, qb:qb + 1], scalar2=-1e30,
                                                   op0=ALU.is_gt, op1=ALU.mult), reads=[posk_bc, posq_col], writes=[junk])
            c.op("pool", lambda e: e.tensor_tensor(out=sc[:], in0=sc[:], in1=junk[:], op=ALU.add), reads=[sc, junk], writes=[sc])
            for it in range(NIT):
                c.op("dve", lambda e: e.tensor_tensor(out=sm["mid"][:], in0=sm["lo"][:], in1=sm["hi"][:], op=ALU.add),
                     reads=[sm["lo"], sm["hi"]], writes=[sm["mid"]])
                c.op("dve", lambda e: e.tensor_scalar(out=sm["mid"][:], in0=sm["mid"][:], scalar1=0.5, scalar2=None, op0=ALU.mult),
                     reads=[sm["mid"]], writes=[sm["mid"]])
                c.op("dve", lambda e: e.tensor_scalar(out=junk[:], in0=sc[:], scalar1=sm["mid"][:, 0:1], scalar2=0.0, op0=ALU.is_ge, op1=ALU.add,
                                                      accum_out=sm["cnt"][:]), reads=[sc, sm["mid"]], writes=[junk, sm["cnt"]])
                c.op("dve", lambda e: e.tensor_scalar(out=sm["ge"][:], in0=sm["cnt"][:], scalar1=TOPK - 0.5, scalar2=None, op0=ALU.is_ge),
                     reads=[sm["cnt"]], writes=[sm["ge"]])
                c.op("dve", lambda e: e.tensor_tensor(out=sm["d1"][:], in0=sm["mid"][:], in1=sm["lo"][:], op=ALU.subtract),
                     reads=[sm["mid"], sm["lo"]], writes=[sm["d1"]])
                c.op("dve", lambda e: e.tensor_tensor(out=sm["d2"][:], in0=sm["hi"][:], in1=sm["mid"][:], op=ALU.subtract),
                     reads=[sm["hi"], sm["mid"]], writes=[sm["d2"]])
                c.op("dve", lambda e: e.scalar_tensor_tensor(out=sm["lo"][:], in0=sm["d1"][:], scalar=sm["ge"][:, 0:1], in1=sm["lo"][:],
                                                             op0=ALU.mult, op1=ALU.add), reads=[sm["d1"], sm["ge"], sm["lo"]], writes=[sm["lo"]])
                c.op("dve", lambda e: e.scalar_tensor_tensor(out=sm["hi"][:], in0=sm["d2"][:], scalar=sm["ge"][:, 0:1], in1=sm["mid"][:],
                                                             op0=ALU.mult, op1=ALU.add), reads=[sm["d2"], sm["ge"], sm["mid"]], writes=[sm["hi"]])
            c.op("dve", lambda e: e.tensor_scalar(out=junk[:], in0=sc[:], scalar1=sm["lo"][:, 0:1], scalar2=None, op0=ALU.is_ge),
                 reads=[sc, sm["lo"]], writes=[junk])
            for di, k in enumerate(("lo", "hi", "cnt", "am")):
                c.dma("sp", dbg_d[qb, :, di:di + 1], sm[k][:], reads=[sm[k]], writes=[dbg_d], allow_slow_non_contiguous=True)
            tp = c_tp
            for g in range(NKC // 4):
                for j in range(4):
                    kc = 4 * g + j
                    c.op("pe", lambda e: e.transpose(out=tp[:, j * 128:(j + 1) * 128], in_=junk[:, kc * 128:(kc + 1) * 128], identity=ident[:]),
                         reads=[junk, ident], writes=[tp])
                c.op("act", lambda e: e.activation(out=maskB[:, 4 * g:4 * g + 4, qs], in_=tp[:, :].rearrange("p (j q) -> p j q", j=4), func=AF.Copy),
                     reads=[tp], writes=[maskB])

        c_tp = c.ps([128, 512], BF16, "tp")

    if do_mla:
        c.dma("sp", kpe[:], kpeT_d[:, :], reads=[kpeT_d], writes=[kpe])
    for i in range(8):
        if do_dsa:
            indexer(i)
        if do_mla:
            attention_head(i, True)
    if do_dsa:
        for h in range(8):
            attention_head(h, False)
    c.finish()
    return nc


def run_B1(per_core, **kw):
    key = "B1" + str(sorted(kw.items()))
    nc = _get(key, lambda: build_B1(**kw))
    ident = np.eye(128, dtype=np.float32).astype(NPBF)
    in_maps = [dict(m, ident=ident) for m in per_core]
    res = run_bass_kernel_spmd(nc, in_maps, core_ids=list(range(len(per_core))))
    return res.results


def emit_ln(c, r, g_d, b_d, out_d, ps_a, ps_b, tmp, onesf):
    nc = c.nc
    gcol = c.sb([128, 16], F32, "ln_g"); bcol = c.sb([128, 16], F32, "ln_b")
    with nc.allow_non_contiguous_dma(reason="tiny ln vectors"):
        c.dma("sp", gcol[:], g_d.ap.rearrange("(c p) -> p c", p=128), reads=[g_d], writes=[gcol])
        c.dma("sp", bcol[:], b_d.ap.rearrange("(c p) -> p c", p=128), reads=[b_d], writes=[bcol])
    for th in range(2):
        ts_ = slice(th * 512, (th + 1) * 512)
        for m in range(16):
            c.op("pe", lambda e: e.matmul(ps_a[:, ts_], lhsT=onesf[:, :], rhs=r[:, m, ts_], start=(m == 0), stop=(m == 15)),
                 reads=[onesf, r], writes=[ps_a])
    for m in range(16):
        c.op("act", lambda e: e.activation(out=tmp[:, :], in_=r[:, m, :], func=AF.Square), reads=[r], writes=[tmp])
        for th in range(2):
            ts_ = slice(th * 512, (th + 1) * 512)
            c.op("pe", lambda e: e.matmul(ps_b[:, ts_], lhsT=onesf[:, :], rhs=tmp[:, ts_], start=(m == 0), stop=(m == 15), skip_group_check=True),
                 reads=[onesf, tmp], writes=[ps_b])
    mean = c.sb([128, TT], F32, "ln_mean"); rstd = c.sb([128, TT], F32, "ln_rstd")
    c.op("act", lambda e: e.activation(out=mean[:], in_=ps_a[:], func=AF.Copy, scale=1.0 / D), reads=[ps_a], writes=[mean])
    c.op("dve", lambda e: e.tensor_tensor(out=tmp[:], in0=mean[:], in1=mean[:], op=ALU.mult), reads=[mean], writes=[tmp])
    c.op("dve", lambda e: e.scalar_tensor_tensor(out=rstd[:], in0=ps_b[:], scalar=1.0 / D, in1=tmp[:], op0=ALU.mult, op1=ALU.subtract),
         reads=[ps_b, tmp], writes=[rstd])
    c.op("dve", lambda e: e.tensor_scalar(out=rstd[:], in0=rstd[:], scalar1=1e-5, scalar2=None, op0=ALU.add), reads=[rstd], writes=[rstd])
    c.op("act", lambda e: e.activation(out=rstd[:], in_=rstd[:], func=AF.Sqrt), reads=[rstd], writes=[rstd])
    c.op("dve", lambda e: e.reciprocal(out=rstd[:], in_=rstd[:]), reads=[rstd], writes=[rstd])
    for m in range(16):
        c.op("dve", lambda e: e.tensor_tensor(out=r[:, m, :], in0=r[:, m, :], in1=mean[:], op=ALU.subtract), reads=[r, mean], writes=[r])
        c.op("pool", lambda e: e.tensor_tensor(out=r[:, m, :], in0=r[:, m, :], in1=rstd[:], op=ALU.mult), reads=[r, rstd], writes=[r])
        c.op("dve", lambda e: e.tensor_scalar(out=r[:, m, :], in0=r[:, m, :], scalar1=gcol[:, m:m + 1], scalar2=bcol[:, m:m + 1],
                                              op0=ALU.mult, op1=ALU.add), reads=[r, gcol, bcol], writes=[r])
    for i in range(4):
        c.dma("sp", out_d.ap[512 * i:512 * (i + 1), :].rearrange("(kc p) t -> p kc t", p=128), r[:, 4 * i:4 * i + 4, :], reads=[r], writes=[out_d])


def build_B2():
    c = Ctx(); nc = c.nc
    hT_d = c.dram("hT", [D, TT], F32, "ExternalInput")
    oaT_d = c.dram("oaT", [1024, TT], BF16, "ExternalInput")
    obT_d = c.dram("obT", [1024, TT], BF16, "ExternalInput")
    win_d = c.dram("w_in", [D, IN_W], F32, "ExternalInput")
    woa_d = c.dram("w_o_a", [1024, D], F32, "ExternalInput")
    wob_d = c.dram("w_o_b", [1024, D], F32, "ExternalInput")
    wout_d = c.dram("w_out", [D, D], F32, "ExternalInput")
    g_d = c.dram("ln_g", [D], F32, "ExternalInput"); b_d = c.dram("ln_b", [D], F32, "ExternalInput")
    out_d = c.dram("h1T", [D, TT], F32, "ExternalOutput")

    hTb = c.sb([128, 16, TT], BF16, "hTb"); r = c.sb([128, 16, TT], F32, "r")
    for i in range(4):
        c.dma("pool", hTb[:, 4 * i:4 * i + 4, :], hT_d.ap[512 * i:512 * (i + 1), :].rearrange("(kc p) t -> p kc t", p=128), reads=[hT_d], writes=[hTb])
        c.dma("sp", r[:, 4 * i:4 * i + 4, :], hT_d.ap[512 * i:512 * (i + 1), :].rearrange("(kc p) t -> p kc t", p=128), reads=[hT_d], writes=[r])
    oa = c.sb([128, 8, TT], BF16, "oa"); ob = c.sb([128, 8, TT], BF16, "ob")
    c.dma("sp", oa[:], oaT_d.ap.rearrange("(kc p) t -> p kc t", p=128), reads=[oaT_d], writes=[oa])
    c.dma("sp", ob[:], obT_d.ap.rearrange("(kc p) t -> p kc t", p=128), reads=[obT_d], writes=[ob])
    yT = c.sb([128, 16, TT], BF16, "yT")
    onesf = c.sb([128, 128], F32, "onesf")
    c.op("pool", lambda e: e.memset(onesf[:], 1.0), writes=[onesf])
    wg = [c.sb([128, 16, 256], BF16, f"wg{i}") for i in range(2)]
    wo = [c.sb([128, 8, 256], BF16, f"wo{i}") for i in range(2)]
    ps = [c.ps([128, 512], F32, f"ps{i}") for i in range(8)]
    sg = [c.sb([128, 512], F32, f"sg{i}") for i in range(2)]
    ta = c.sb([128, 512], F32, "ta"); tb = c.sb([128, 512], F32, "tb")
    k = 0
    for grp in range(8):
        c.dma("pool", wg[0][:], win_d.ap[:, C_GA + 256 * grp:C_GA + 256 * (grp + 1)].rearrange("(kc p) m -> p kc m", p=128), reads=[win_d], writes=[wg[0]])
        c.dma("pool", wg[1][:], win_d.ap[:, C_GB + 256 * grp:C_GB + 256 * (grp + 1)].rearrange("(kc p) m -> p kc m", p=128), reads=[win_d], writes=[wg[1]])
        c.dma("pool", wo[0][:], woa_d.ap[:, 256 * grp:256 * (grp + 1)].rearrange("(kc p) m -> p kc m", p=128), reads=[woa_d], writes=[wo[0]])
        c.dma("pool", wo[1][:], wob_d.ap[:, 256 * grp:256 * (grp + 1)].rearrange("(kc p) m -> p kc m", p=128), reads=[wob_d], writes=[wo[1]])
        for j in range(2):
            m = 2 * grp + j
            ms = slice(128 * j, 128 * (j + 1))
            for th in range(2):
                ts_ = slice(th * 512, (th + 1) * 512)
                p4 = ps[4 * (k % 2):4 * (k % 2) + 4]; k += 1
                for kc in range(16):
                    c.op("pe", lambda e: e.matmul(p4[0][:], lhsT=wg[0][:, kc, ms], rhs=hTb[:, kc, ts_], start=(kc == 0), stop=(kc == 15)), reads=[wg[0], hTb], writes=[p4[0]])
                for kc in range(16):
                    c.op("pe", lambda e: e.matmul(p4[1][:], lhsT=wg[1][:, kc, ms], rhs=hTb[:, kc, ts_], start=(kc == 0), stop=(kc == 15)), reads=[wg[1], hTb], writes=[p4[1]])
                for kc in range(8):
                    c.op("pe", lambda e: e.matmul(p4[2][:], lhsT=wo[0][:, kc, ms], rhs=oa[:, kc, ts_], start=(kc == 0), stop=(kc == 7)), reads=[wo[0], oa], writes=[p4[2]])
                for kc in range(8):
                    c.op("pe", lambda e: e.matmul(p4[3][:], lhsT=wo[1][:, kc, ms], rhs=ob[:, kc, ts_], start=(kc == 0), stop=(kc == 7)), reads=[wo[1], ob], writes=[p4[3]])
                c.op("act", lambda e: e.activation(out=sg[0][:], in_=p4[0][:], func=AF.Sigmoid), reads=[p4[0]], writes=[sg[0]])
                c.op("act", lambda e: e.activation(out=sg[1][:], in_=p4[1][:], func=AF.Sigmoid), reads=[p4[1]], writes=[sg[1]])
                c.op("dve", lambda e: e.tensor_tensor(out=ta[:], in0=p4[2][:], in1=sg[0][:], op=ALU.mult), reads=[p4[2], sg[0]], writes=[ta])
                c.op("dve", lambda e: e.tensor_tensor(out=tb[:], in0=p4[3][:], in1=sg[1][:], op=ALU.mult), reads=[p4[3], sg[1]], writes=[tb])
                c.op("pool", lambda e: e.tensor_tensor(out=yT[:, m, ts_], in0=ta[:], in1=tb[:], op=ALU.add), reads=[ta, tb], writes=[yT])
    wb = wg
    for grp in range(8):
        w = wb[grp % 2]
        c.dma("pool", w[:], wout_d.ap[:, 256 * grp:256 * (grp + 1)].rearrange("(kc p) m -> p kc m", p=128), reads=[wout_d], writes=[w])
        for j in range(2):
            m = 2 * grp + j
            ms = slice(128 * j, 128 * (j + 1))
            for th in range(2):
                ts_ = slice(th * 512, (th + 1) * 512)
                p = ps[k % 8]; k += 1
                for kc in range(16):
                    c.op("pe", lambda e: e.matmul(p[:], lhsT=w[:, kc, ms], rhs=yT[:, kc, ts_], start=(kc == 0), stop=(kc == 15)), reads=[w, yT], writes=[p])
                c.op("dve", lambda e: e.scalar_tensor_tensor(out=r[:, m, ts_], in0=r[:, m, ts_], scalar=ALPHA, in1=p[:], op0=ALU.mult, op1=ALU.add),
                     reads=[r, p], writes=[r])
    ps_a = T(None, "ps_a", excl=True); ps_b = T(None, "ps_b", excl=True)
    emit_ln_psum(c, r, g_d, b_d, out_d, ps, onesf)
    c.finish()
    return nc


def emit_ln_psum(c, r, g_d, b_d, out_d, ps, onesf):
    nc = c.nc
    gcol = c.sb([128, 16], F32, "ln_g"); bcol = c.sb([128, 16], F32, "ln_b")
    with nc.allow_non_contiguous_dma(reason="tiny ln vectors"):
        c.dma("sp", gcol[:], g_d.ap.rearrange("(c p) -> p c", p=128), reads=[g_d], writes=[gcol])
        c.dma("sp", bcol[:], b_d.ap.rearrange("(c p) -> p c", p=128), reads=[b_d], writes=[bcol])
    tmp = c.sb([128, TT], F32, "ln_tmp")
    mean = c.sb([128, TT], F32, "ln_mean"); rstd = c.sb([128, TT], F32, "ln_rstd")
    for th in range(2):
        ts_ = slice(th * 512, (th + 1) * 512)
        for m in range(16):
            c.op("pe", lambda e: e.matmul(ps[th][:], lhsT=onesf[:, :], rhs=r[:, m, ts_], start=(m == 0), stop=(m == 15)),
                 reads=[onesf, r], writes=[ps[th]])
    for m in range(16):
        c.op("act", lambda e: e.activation(out=tmp[:, :], in_=r[:, m, :], func=AF.Square), reads=[r], writes=[tmp])
        for th in range(2):
            ts_ = slice(th * 512, (th + 1) * 512)
            c.op("pe", lambda e: e.matmul(ps[2 + th][:], lhsT=onesf[:, :], rhs=tmp[:, ts_], start=(m == 0), stop=(m == 15)),
                 reads=[onesf, tmp], writes=[ps[2 + th]])
    for th in range(2):
        ts_ = slice(th * 512, (th + 1) * 512)
        c.op("act", lambda e: e.activation(out=mean[:, ts_], in_=ps[th][:], func=AF.Copy, scale=1.0 / D), reads=[ps[th]], writes=[mean])
        c.op("dve", lambda e: e.tensor_tensor(out=tmp[:, ts_], in0=mean[:, ts_], in1=mean[:, ts_], op=ALU.mult), reads=[mean], writes=[tmp])
        c.op("dve", lambda e: e.scalar_tensor_tensor(out=rstd[:, ts_], in0=ps[2 + th][:], scalar=1.0 / D, in1=tmp[:, ts_], op0=ALU.mult, op1=ALU.subtract),
             reads=[ps[2 + th], tmp], writes=[rstd])
    c.op("dve", lambda e: e.tensor_scalar(out=rstd[:], in0=rstd[:], scalar1=1e-5, scalar2=None, op0=ALU.add), reads=[rstd], writes=[rstd])
    c.op("act", lambda e: e.activation(out=rstd[:], in_=rstd[:], func=AF.Sqrt), reads=[rstd], writes=[rstd])
    c.op("dve", lambda e: e.reciprocal(out=rstd[:], in_=rstd[:]), reads=[rstd], writes=[rstd])
    for m in range(16):
        c.op("dve", lambda e: e.tensor_tensor(out=r[:, m, :], in0=r[:, m, :], in1=mean[:], op=ALU.subtract), reads=[r, mean], writes=[r])
        c.op("pool", lambda e: e.tensor_tensor(out=r[:, m, :], in0=r[:, m, :], in1=rstd[:], op=ALU.mult), reads=[r, rstd], writes=[r])
        c.op("dve", lambda e: e.tensor_scalar(out=r[:, m, :], in0=r[:, m, :], scalar1=gcol[:, m:m + 1], scalar2=bcol[:, m:m + 1],
                                              op0=ALU.mult, op1=ALU.add), reads=[r, gcol, bcol], writes=[r])
    for i in range(4):
        c.dma("sp", out_d.ap[512 * i:512 * (i + 1), :].rearrange("(kc p) t -> p kc t", p=128), r[:, 4 * i:4 * i + 4, :], reads=[r], writes=[out_d])


def run_B2(per_core):
    nc = _get("B2", build_B2)
    res = run_bass_kernel_spmd(nc, per_core, core_ids=list(range(len(per_core))))
    return res.results


def build_B3(n_tt=8, n_e=64):
    c = Ctx(); nc = c.nc
    hT_d = c.dram("hT", [D, TT], F32, "ExternalInput")
    wgr_d = c.dram("w_group", [D, 8], F32, "ExternalInput"); bgr_d = c.dram("b_group", [1, 8], F32, "ExternalInput")
    wro_d = c.dram("w_router", [D, 64], F32, "ExternalInput"); bro_d = c.dram("b_router", [1, 64], F32, "ExternalInput")
    weg_d = c.dram("w_e_gate", [64, D, 512], F32, "ExternalInput")
    weu_d = c.dram("w_e_up", [64, D, 512], F32, "ExternalInput")
    wed_d = c.dram("w_e_down", [64, 512, D], F32, "ExternalInput")
    g_d = c.dram("ln_g", [D], F32, "ExternalInput"); b_d = c.dram("ln_b", [D], F32, "ExternalInput")
    identb_d = c.dram("identb", [128, 128], BF16, "ExternalInput")
    identf_d = c.dram("identf", [128, 128], F32, "ExternalInput")
    out_d = c.dram("h2T", [D, TT], F32, "ExternalOutput")

    hTb = c.sb([128, 16, TT], BF16, "hTb"); r = c.sb([128, 16, TT], F32, "r")
    for i in range(4):
        c.dma("pool", hTb[:, 4 * i:4 * i + 4, :], hT_d.ap[512 * i:512 * (i + 1), :].rearrange("(kc p) t -> p kc t", p=128), reads=[hT_d], writes=[hTb])
        c.dma("sp", r[:, 4 * i:4 * i + 4, :], hT_d.ap[512 * i:512 * (i + 1), :].rearrange("(kc p) t -> p kc t", p=128), reads=[hT_d], writes=[r])
    identb = c.sb([128, 128], BF16, "identb"); identf = c.sb([128, 128], F32, "identf")
    c.dma("sp", identb[:], identb_d[:], reads=[identb_d], writes=[identb])
    c.dma("sp", identf[:], identf_d[:], reads=[identf_d], writes=[identf])
    onesf = c.sb([128, 128], F32, "onesf")
    c.op("pool", lambda e: e.memset(onesf[:], 1.0), writes=[onesf])
    wr = c.sb([128, 16, 72], F32, "wr"); bias = c.sb([128, 72], F32, "bias")
    with nc.allow_non_contiguous_dma(reason="small router weights"):
        c.dma("sp", wr[:, :, 0:8], wgr_d.ap.rearrange("(kc p) m -> p kc m", p=128), reads=[wgr_d], writes=[wr])
        c.dma("sp", wr[:, :, 8:72], wro_d.ap.rearrange("(kc p) m -> p kc m", p=128), reads=[wro_d], writes=[wr])
    c.dma("sp", bias[:, 0:8], bgr_d.ap[0, :].partition_broadcast(128), reads=[bgr_d], writes=[bias])
    c.dma("sp", bias[:, 8:72], bro_d.ap[0, :].partition_broadcast(128), reads=[bro_d], writes=[bias])

    y_ps = [c.ps([128, 512], F32, f"y{i}") for i in range(4)]
    g_ps = c.ps([128, 512], F32, "g"); u_ps = c.ps([128, 512], F32, "u")
    tp_ps = c.ps([128, 512], BF16, "tp")

    CW = c.sb([128, 8, 64], F32, "CW")
    lg = c.sb([128, 72], F32, "lg"); em = c.sb([128, 64], F32, "em"); em2 = c.sb([128, 64], F32, "em2")
    mk1 = c.sb([128, 64], F32, "mk1"); mk2 = c.sb([128, 64], F32, "mk2"); gmask = c.sb([128, 8], F32, "gmask"); gex = c.sb([128, 8], F32, "gex")
    s = {k: c.sb([128, 1], F32, "r_" + k) for k in ("gmax", "ngmax", "gsum", "gp", "m1", "m2", "d", "ex", "w1", "w2")}
    pen = c.sb([128, 8], F32, "pen")
    for tt in range(8):
        tsl = slice(tt * 128, (tt + 1) * 128)
        for kc in range(16):
            c.op("pe", lambda e: e.matmul(g_ps[:, 0:72], lhsT=r[:, kc, tsl], rhs=wr[:, kc, :], start=(kc == 0), stop=(kc == 15)), reads=[r, wr], writes=[g_ps])
        c.op("dve", lambda e: e.tensor_tensor(out=lg[:], in0=g_ps[:, 0:72], in1=bias[:], op=ALU.add), reads=[g_ps, bias], writes=[lg])
        c.op("dve", lambda e: e.tensor_reduce(out=s["gmax"][:], in_=lg[:, 0:8], axis=AX.X, op=ALU.max), reads=[lg], writes=[s["gmax"]])
        c.op("dve", lambda e: e.tensor_scalar(out=gmask[:], in0=lg[:, 0:8], scalar1=s["gmax"][:, 0:1], scalar2=None, op0=ALU.is_ge), reads=[lg, s["gmax"]], writes=[gmask])
        c.op("dve", lambda e: e.tensor_scalar(out=s["ngmax"][:], in0=s["gmax"][:], scalar1=-1.0, scalar2=None, op0=ALU.mult), reads=[s["gmax"]], writes=[s["ngmax"]])
        c.op("act", lambda e: e.activation(out=gex[:], in_=lg[:, 0:8], func=AF.Exp, bias=s["ngmax"][:, 0:1], accum_out=s["gsum"][:]),
             reads=[lg, s["ngmax"]], writes=[gex, s["gsum"]])
        c.op("dve", lambda e: e.reciprocal(out=s["gp"][:], in_=s["gsum"][:]), reads=[s["gsum"]], writes=[s["gp"]])
        c.op("dve", lambda e: e.tensor_scalar(out=pen[:], in0=gmask[:], scalar1=-1.0, scalar2=1e30, op0=ALU.add, op1=ALU.mult), reads=[gmask], writes=[pen])
        for g in range(8):
            c.op("dve", lambda e: e.tensor_scalar(out=em[:, 8 * g:8 * g + 8], in0=lg[:, 8 + 8 * g:16 + 8 * g], scalar1=pen[:, g:g + 1], scalar2=None, op0=ALU.add),
                 reads=[lg, pen], writes=[em])
        c.op("dve", lambda e: e.tensor_reduce(out=s["m1"][:], in_=em[:], axis=AX.X, op=ALU.max), reads=[em], writes=[s["m1"]])
        c.op("dve", lambda e: e.tensor_scalar(out=mk1[:], in0=em[:], scalar1=s["m1"][:, 0:1], scalar2=None, op0=ALU.is_ge), reads=[em, s["m1"]], writes=[mk1])
        c.op("dve", lambda e: e.scalar_tensor_tensor(out=em2[:], in0=mk1[:], scalar=-1e30, in1=em[:], op0=ALU.mult, op1=ALU.add), reads=[mk1, em], writes=[em2])
        c.op("dve", lambda e: e.tensor_reduce(out=s["m2"][:], in_=em2[:], axis=AX.X, op=ALU.max), reads=[em2], writes=[s["m2"]])
        c.op("dve", lambda e: e.tensor_scalar(out=mk2[:], in0=em2[:], scalar1=s["m2"][:, 0:1], scalar2=None, op0=ALU.is_ge), reads=[em2, s["m2"]], writes=[mk2])
        c.op("dve", lambda e: e.tensor_tensor(out=s["d"][:], in0=s["m2"][:], in1=s["m1"][:], op=ALU.subtract), reads=[s["m1"], s["m2"]], writes=[s["d"]])
        c.op("act", lambda e: e.activation(out=s["ex"][:], in_=s["d"][:], func=AF.Exp), reads=[s["d"]], writes=[s["ex"]])
        c.op("dve", lambda e: e.tensor_scalar(out=s["ex"][:], in0=s["ex"][:], scalar1=1.0, scalar2=None, op0=ALU.add), reads=[s["ex"]], writes=[s["ex"]])
        c.op("dve", lambda e: e.reciprocal(out=s["ex"][:], in_=s["ex"][:]), reads=[s["ex"]], writes=[s["ex"]])
        c.op("dve", lambda e: e.tensor_tensor(out=s["w1"][:], in0=s["gp"][:], in1=s["ex"][:], op=ALU.mult), reads=[s["gp"], s["ex"]], writes=[s["w1"]])
        c.op("dve", lambda e: e.tensor_tensor(out=s["w2"][:], in0=s["gp"][:], in1=s["w1"][:], op=ALU.subtract), reads=[s["gp"], s["w1"]], writes=[s["w2"]])
        c.op("dve", lambda e: e.tensor_scalar(out=mk1[:], in0=mk1[:], scalar1=s["w1"][:, 0:1], scalar2=None, op0=ALU.mult), reads=[mk1, s["w1"]], writes=[mk1])
        c.op("dve", lambda e: e.scalar_tensor_tensor(out=CW[:, tt, :], in0=mk2[:], scalar=s["w2"][:, 0:1], in1=mk1[:], op0=ALU.mult, op1=ALU.add),
             reads=[mk2, s["w2"], mk1], writes=[CW])

    Wg = [c.sb([128, 16, 512], BF16, f"Wg{i}") for i in range(2)]
    Wu = [c.sb([128, 16, 512], BF16, f"Wu{i}") for i in range(1)] * 2
    Wd = [c.sb([128, 4, D], BF16, f"Wd{i}") for i in range(1)] * 2
    sg = c.sb([128, 512], F32, "sg"); hid = c.sb([128, 512], BF16, "hid"); hidT = c.sb([128, 4, 128], BF16, "hidT")
    ysb = c.sb([128, D], F32, "ysb")
    k = 0
    for tt in range(n_tt):
        tsl = slice(tt * 128, (tt + 1) * 128)
        for ex in range(n_e):
            wgb, wub, wdb = Wg[k % 2], Wu[k % 2], Wd[k % 2]; k += 1
            c.dma("pool", wgb[:], weg_d.ap[ex].rearrange("(kc p) m -> p kc m", p=128), reads=[weg_d], writes=[wgb])
            c.dma("pool", wub[:], weu_d.ap[ex].rearrange("(kc p) m -> p kc m", p=128), reads=[weu_d], writes=[wub])
            c.dma("pool", wdb[:], wed_d.ap[ex].rearrange("(kc p) m -> p kc m", p=128), reads=[wed_d], writes=[wdb])
            for kc in range(16):
                c.op("pe", lambda e: e.matmul(g_ps[:], lhsT=hTb[:, kc, tsl], rhs=wgb[:, kc, :], start=(kc == 0), stop=(kc == 15)), reads=[hTb, wgb], writes=[g_ps])
            for kc in range(16):
                c.op("pe", lambda e: e.matmul(u_ps[:], lhsT=hTb[:, kc, tsl], rhs=wub[:, kc, :], start=(kc == 0), stop=(kc == 15)), reads=[hTb, wub], writes=[u_ps])
            c.op("act", lambda e: e.activation(out=sg[:], in_=g_ps[:], func=AF.Silu), reads=[g_ps], writes=[sg])
            c.op("dve", lambda e: e.scalar_tensor_tensor(out=hid[:], in0=sg[:], scalar=CW[:, tt, ex:ex + 1], in1=u_ps[:], op0=ALU.mult, op1=ALU.mult),
                 reads=[sg, CW, u_ps], writes=[hid])
            for fc in range(4):
                c.op("pe", lambda e: e.transpose(out=tp_ps[:, fc * 128:(fc + 1) * 128], in_=hid[:, fc * 128:(fc + 1) * 128], identity=identb[:]),
                     reads=[hid, identb], writes=[tp_ps])
            c.op("act", lambda e: e.activation(out=hidT[:, :, :], in_=tp_ps[:, :].rearrange("p (f t) -> p f t", f=4), func=AF.Copy), reads=[tp_ps], writes=[hidT])
            for n in range(4):
                for fc in range(4):
                    c.op("pe", lambda e: e.matmul(y_ps[n][:], lhsT=hidT[:, fc, :], rhs=wdb[:, fc, n * 512:(n + 1) * 512],
                                                  start=(ex == 0 and fc == 0), stop=(ex == n_e - 1 and fc == 3)), reads=[hidT, wdb], writes=[y_ps[n]])
        for n in range(4):
            c.op("act", lambda e: e.activation(out=ysb[:, n * 512:(n + 1) * 512], in_=y_ps[n][:], func=AF.Copy), reads=[y_ps[n]], writes=[ysb])
        for m in range(16):
            c.op("pe", lambda e: e.transpose(out=u_ps[:, 0:128], in_=ysb[:, m * 128:(m + 1) * 128], identity=identf[:]), reads=[ysb, identf], writes=[u_ps])
            c.op("dve", lambda e: e.scalar_tensor_tensor(out=r[:, m, tsl], in0=r[:, m, tsl], scalar=ALPHA, in1=u_ps[:, 0:128], op0=ALU.mult, op1=ALU.add),
                 reads=[r, u_ps], writes=[r])
    for tt in range(n_tt, 8):
        tsl = slice(tt * 128, (tt + 1) * 128)
        c.op("dve", lambda e: e.tensor_scalar(out=r[:, :, tsl], in0=r[:, :, tsl], scalar1=ALPHA, scalar2=None, op0=ALU.mult), reads=[r], writes=[r])
    emit_ln_psum(c, r, g_d, b_d, out_d, y_ps, onesf)
    c.finish()
    return nc


def run_B3(per_core, **kw):
    nc = _get("B3" + str(sorted(kw.items())), lambda: build_B3(**kw))
    ib = np.eye(128, dtype=np.float32).astype(NPBF); idf = np.eye(128, dtype=np.float32)
    in_maps = [dict(m, identb=ib, identf=idf) for m in per_core]
    res = run_bass_kernel_spmd(nc, in_maps, core_ids=list(range(len(per_core))))
    return res.results


def kernel(x, positions, w_in, g_q_lora, w_q_b, g_kv_lora, w_kv_b, w_o_a, w_o_b, w_out, ln1_g, ln1_b,
           w_group, b_group, w_router, b_router, w_e_gate, w_e_up, w_e_down, ln2_g, ln2_b):
    x = np.asarray(x); positions = np.asarray(positions)
    nb = x.shape[0]
    cores = [(b, j) for b in range(nb) for j in range(4)]
    hT = [np.ascontiguousarray(x[b, j * TT:(j + 1) * TT, :].T) for (b, j) in cores]
    posq = [np.ascontiguousarray(positions[b, j * TT:(j + 1) * TT][None, :]).astype(np.int32) for (b, j) in cores]
    posk = [np.ascontiguousarray(positions[b][None, :]).astype(np.int32) for (b, j) in cores]
    depth = w_in.shape[0]
    for l in range(depth):
        A = run_A(hT, posq, np.asarray(w_in[l]), np.asarray(g_q_lora[l]), np.asarray(w_q_b[l]), np.asarray(g_kv_lora[l]), np.asarray(w_kv_b[l]))
        full = {}
        for b in range(nb):
            cs = [4 * b + j for j in range(4)]
            f = {}
            for nm in ("kaT", "kbT", "vaT", "vbT"):
                f[nm] = np.concatenate([A[ci][nm] for ci in cs], axis=2)
            for nm in ("kpeT", "kiT"):
                f[nm] = np.concatenate([A[ci][nm] for ci in cs], axis=1)
            f["va"] = np.ascontiguousarray(f["vaT"].transpose(2, 0, 1).reshape(SEQ, 1024))
            f["vb"] = np.ascontiguousarray(f["vbT"].transpose(2, 0, 1).reshape(SEQ, 1024))
            full[b] = f
        per_core = []
        for ci, (b, j) in enumerate(cores):
            f = full[b]
            per_core.append({"qaT": A[ci]["qaT"], "qbT": A[ci]["qbT"], "qiT": A[ci]["qiT"],
                             "widx": np.ascontiguousarray(A[ci]["widxT"].T),
                             "kaT": f["kaT"], "kpeT": f["kpeT"], "va": f["va"], "kbT": f["kbT"], "vb": f["vb"], "kiT": f["kiT"],
                             "posq": posq[ci], "posk": posk[ci]})
        B1 = run_B1(per_core, do_mla=True, do_dsa=True)
        per_core = []
        for ci in range(len(cores)):
            per_core.append({"hT": hT[ci], "oaT": np.ascontiguousarray(B1[ci]["oaT"].reshape(1024, TT)),
                             "obT": np.ascontiguousarray(B1[ci]["obT"].reshape(1024, TT)),
                             "w_in": np.asarray(w_in[l]), "w_o_a": np.asarray(w_o_a[l]), "w_o_b": np.asarray(w_o_b[l]),
                             "w_out": np.asarray(w_out[l]), "ln_g": np.asarray(ln1_g[l]), "ln_b": np.asarray(ln1_b[l])})
        B2 = run_B2(per_core)
        per_core = []
        for ci in range(len(cores)):
            per_core.append({"hT": B2[ci]["h1T"], "w_group": np.asarray(w_group[l]), "b_group": np.asarray(b_group[l])[None, :],
                             "w_router": np.asarray(w_router[l]), "b_router": np.asarray(b_router[l])[None, :],
                             "w_e_gate": np.asarray(w_e_gate[l]), "w_e_up": np.asarray(w_e_up[l]), "w_e_down": np.asarray(w_e_down[l]),
                             "ln_g": np.asarray(ln2_g[l]), "ln_b": np.asarray(ln2_b[l])})
        B3 = run_B3(per_core)
        hT = [B3[ci]["h2T"] for ci in range(len(cores))]
    out = np.empty(x.shape, np.float32)
    for ci, (b, j) in enumerate(cores):
        out[b, j * TT:(j + 1) * TT, :] = hT[ci].T
    return out
```

```python
import math
import numpy as np
from contextlib import ExitStack
import ml_dtypes
import concourse.bass as bass
import concourse.mybir as mybir
from concourse.bass_utils import run_bass_kernel_spmd

F32 = mybir.dt.float32; BF16 = mybir.dt.bfloat16; I32 = mybir.dt.int32
AF = mybir.ActivationFunctionType; ALU = mybir.AluOpType; AX = mybir.AxisListType
NPBF = ml_dtypes.bfloat16

D = 2048; TT = 1024; NCORE = 8; SEQ = 4096
IN_W = 9360
C_KPE = 1024; C_QB = 1088; C_KB = 2112; C_VB = 3136; C_QI = 4160; C_KI = 5184; C_WI = 5248; C_GA = 5264; C_GB = 7312
ALPHA = 4 ** 0.25
THETA = 500000.0


class T:
    __slots__ = ("ap", "w", "r", "name", "excl")

    def __init__(self, ap, name="", excl=False):
        self.ap = ap; self.w = None; self.r = {}; self.name = name; self.excl = excl

    def __getitem__(self, k):
        return self.ap[k]


class E:
    def __init__(self, name, obj, sem):
        self.name = name; self.obj = obj; self.sem = sem; self.count = 0; self.seen = {}


class Ctx:
    NDMASEM = 8

    def __init__(self):
        self.nc = nc = bass.Bass("TRN2", target_bir_lowering=False)
        self.es = ExitStack()
        self.eng = {}
        for name, obj in (("pe", nc.tensor), ("act", nc.scalar), ("dve", nc.vector), ("pool", nc.gpsimd), ("sp", nc.sync)):
            sem = self.es.enter_context(nc.semaphore("s_" + name))
            self.eng[name] = E(name, obj, sem)
        self.dq = {}
        for q in ("sp", "pool", "act"):
            lst = []
            for i in range(self.NDMASEM):
                sem = self.es.enter_context(nc.semaphore(f"d_{q}{i}"))
                lst.append(E(f"d_{q}{i}", None, sem))
            self.dq[q] = [lst, 0]
        self.cc = E("cc", None, self.es.enter_context(nc.semaphore("cc_sem")))
        self.pes = None
        self.ntile = 0
        self.rr = 0

    def sb(self, shape, dt, name=None):
        self.ntile += 1
        name = f"sb_{name or 't'}_{self.ntile}"
        h = (self.pes or self.es).enter_context(self.nc.sbuf_tensor(name, list(shape), dt))
        return T(h, name)

    def ps(self, shape, dt=F32, name=None):
        self.ntile += 1
        name = f"ps_{name or 'p'}_{self.ntile}"
        h = (self.pes or self.es).enter_context(self.nc.psum_tensor(name, list(shape), dt))
        return T(h, name, excl=True)

    def dram(self, name, shape, dt, kind="Internal"):
        h = self.nc.dram_tensor(name, list(shape), dt, kind=kind)
        return T(h.ap(), name)

    def _deps(self, e, reads, writes, extra=()):
        deps = {}

        def add(d):
            if d is None:
                return
            p, idx = d
            if p is e and e.name in ("pe", "sp"):
                return
            if deps.get(p, 0) < idx:
                deps[p] = idx
        for t in reads:
            add(t.w)
            if t.excl:
                for p, idx in t.r.items():
                    add((p, idx))
        for t in writes:
            add(t.w)
            for p, idx in t.r.items():
                add((p, idx))
        for d in extra:
            add(d)
        for p, idx in deps.items():
            if e.seen.get(p, 0) < idx:
                e.obj.wait_ge(p.sem, idx)
                e.seen[p] = idx

    def op(self, en, fn, reads=(), writes=()):
        e = self.eng[en]
        self._deps(e, reads, writes)
        inst = fn(e.obj)
        e.count += 1
        inst.then_inc(e.sem, 1)
        for t in writes:
            t.w = (e, e.count); t.r = {}
        for t in reads:
            t.r[e] = e.count
        return inst

    def dma(self, q, out, in_, reads=(), writes=(), **kw):
        e = self.eng[q]
        lst, i = self.dq[q]
        d = lst[i % len(lst)]; self.dq[q][1] = i + 1
        extra = [(d, d.count)] if d.count else []
        self._deps(e, reads, writes, extra)
        inst = e.obj.dma_start(out=out, in_=in_, **kw)
        d.count += 16
        inst.then_inc(d.sem, 16)
        for t in writes:
            t.w = (d, d.count); t.r = {}
        for t in reads:
            t.r[d] = d.count
        return inst

    def barrier(self):
        engs = list(self.eng.values())
        others = engs + [d for (lst, _) in self.dq.values() for d in lst] + [self.cc]
        for e in engs:
            for p in others:
                if p is e:
                    continue
                if p.count and e.seen.get(p, 0) < p.count:
                    e.obj.wait_ge(p.sem, p.count); e.seen[p] = p.count

    def phase_begin(self):
        self.pes = ExitStack()

    def phase_end(self):
        self.barrier()
        self.pes.close(); self.pes = None

    def collective(self, kind, in_t, in_ap, out_t, out_ap, groups):
        e = self.eng["pool"]; d = self.cc
        self._deps(e, [in_t], [out_t])
        inst = e.obj.collective_compute(kind, ALU.bypass, replica_groups=groups, ins=[in_ap], outs=[out_ap])
        d.count += 1
        inst.then_inc(d.sem, 1)
        out_t.w = (d, d.count); out_t.r = {}
        in_t.r[d] = d.count

    def finish(self):
        e = self.eng["sp"]
        for p in list(self.eng.values()):
            if p is not e and p.count and e.seen.get(p, 0) < p.count:
                e.obj.wait_ge(p.sem, p.count); e.seen[p] = p.count
        for q, (lst, _) in self.dq.items():
            for d in lst:
                if d.count and e.seen.get(d, 0) < d.count:
                    e.obj.wait_ge(d.sem, d.count); e.seen[d] = d.count
        if self.cc.count and e.seen.get(self.cc, 0) < self.cc.count:
            e.obj.wait_ge(self.cc.sem, self.cc.count)
        self.es.close()

    def alt(self, engs=("dve", "act")):
        self.rr += 1
        return engs[self.rr % len(engs)]


def _rope_consts():
    inv = np.zeros((128, 3), np.float32)
    perm = np.zeros((3, 128, 128), np.float32)
    f = THETA ** (-np.arange(0, 64, 2, dtype=np.float32) / 64.0)
    inv[0:32, 0] = f; inv[32:64, 0] = f
    for i in range(32):
        perm[0, 32 + i, i] = -1.0; perm[0, i, 32 + i] = 1.0
    f = THETA ** (-np.arange(0, 32, 2, dtype=np.float32) / 32.0)
    inv[0:16, 1] = f; inv[16:32, 1] = f
    for i in range(16):
        perm[1, 16 + i, i] = -1.0; perm[1, i, 16 + i] = 1.0
    f = THETA ** (-np.arange(0, 16, 2, dtype=np.float32) / 16.0)
    for base in (0, 64):
        inv[base:base + 8, 2] = f; inv[base + 8:base + 16, 2] = f
        for i in range(8):
            perm[2, base + 8 + i, base + i] = -1.0; perm[2, base + i, base + 8 + i] = 1.0
    inv = (inv.astype(np.float64) / (2 * np.pi)).astype(np.float32)
    return inv, perm


KA_OFF = 0; KB_OFF = 1024; VA_OFF = 2048; VB_OFF = 3072; KPE_OFF = 4096; KI_OFF = 4160; KVROWS = 4224
CH = 512; NCH = 9
NIT = 18
TOPK = 256
GROUPS = [[0, 1, 2, 3], [4, 5, 6, 7]]


def emit_A(c, io, l, hin):
    nc = c.nc
    win_d = io["w_in"]; wqb_d = io["w_q_b"]; wkvb_d = io["w_kv_b"]
    kv = io["kvpack"]
    hTb = c.sb([128, 16, TT], BF16, "hTb")
    for i in range(4):
        c.dma("pool", hTb[:, 4 * i:4 * i + 4, :], hin.ap[512 * i:512 * (i + 1), :].rearrange("(kc p) t -> p kc t", p=128),
              reads=[hin], writes=[hTb])
    inv = c.sb([128, 3], F32, "inv"); c.dma("sp", inv[:], io["inv"][:], reads=[io["inv"]], writes=[inv])
    perm = c.sb([128, 3, 128], F32, "perm")
    c.dma("sp", perm[:], io["perm"].ap.rearrange("r k m -> k r m"), reads=[io["perm"]], writes=[perm])
    gq = c.sb([128, 4], F32, "gq"); gkv = c.sb([128, 4], F32, "gkv")
    with nc.allow_non_contiguous_dma(reason="tiny gain vectors"):
        c.dma("sp", gq[:], io["g_q"].ap[l].rearrange("(c p) -> p c", p=128), reads=[io["g_q"]], writes=[gq])
        c.dma("sp", gkv[:], io["g_kv"].ap[l].rearrange("(c p) -> p c", p=128), reads=[io["g_kv"]], writes=[gkv])
    ones = c.sb([128, 128], BF16, "ones")
    c.op("pool", lambda e: e.memset(ones[:], 1.0), writes=[ones])

    posi = c.sb([128, TT], I32, "posi")
    c.dma("sp", posi[:], io["posq"].ap[0, :].partition_broadcast(128), reads=[io["posq"]], writes=[posi])
    posf = c.sb([128, TT], F32, "posf")
    c.op("dve", lambda e: e.tensor_copy(out=posf[:], in_=posi[:]), reads=[posi], writes=[posf])
    CC = c.sb([128, 3, TT], F32, "CC"); SS = c.sb([128, 3, TT], F32, "SS")
    u = c.sb([128, TT], F32, "u"); uf = c.sb([128, TT], F32, "uf")
    for r in range(3):
        for which, dst in ((0, SS), (1, CC)):
            c.op("dve", lambda e: e.tensor_scalar(out=u[:], in0=posf[:], scalar1=inv[:, r:r + 1], scalar2=0.25 * which,
                                                  op0=ALU.mult, op1=ALU.add), reads=[posf, inv], writes=[u])
            c.op("dve", lambda e: e.tensor_copy(out=posi[:], in_=u[:]), reads=[u], writes=[posi])
            c.op("dve", lambda e: e.tensor_copy(out=uf[:], in_=posi[:]), reads=[posi], writes=[uf])
            c.op("dve", lambda e: e.tensor_tensor(out=u[:], in0=u[:], in1=uf[:], op=ALU.subtract), reads=[u, uf], writes=[u])
            c.op("dve", lambda e: e.scalar_tensor_tensor(out=uf[:], in0=u[:], scalar=0.5, in1=u[:], op0=ALU.is_gt, op1=ALU.subtract),
                 reads=[u], writes=[uf])
            c.op("act", lambda e: e.activation(out=dst[:, r, :], in_=uf[:], func=AF.Sin, scale=-6.283185), reads=[uf], writes=[dst])

    wbufs = [c.sb([128, 16, 512], BF16, f"wbuf{i}") for i in range(2)]
    accs = [c.ps([128, TT], F32, f"acc{i}") for i in range(2)]
    rot_ps = c.ps([128, TT], F32, "rot_ps")
    rep_ps = c.ps([128, TT], F32, "rep_ps")
    stg = [c.sb([128, TT], BF16, f"stg{i}") for i in range(3)]
    pf = [c.sb([128, TT], F32, f"pf{i}") for i in range(2)]
    t1 = [c.sb([128, TT], F32, f"t1_{i}") for i in range(2)]
    st = {"w": 0, "a": 0, "s": 0, "p": 0}

    def load_w(w_ap, K, col0, ncols):
        KC = K // 128
        wb = wbufs[st["w"] % 2]; st["w"] += 1
        c.dma("pool", wb[:, 0:KC, 0:ncols], w_ap[:, col0:col0 + ncols].rearrange("(kc p) m -> p kc m", p=128),
              reads=[], writes=[wb])
        return wb

    def fm_chunk(wb, KC, off, M, rhs, epi):
        acc = accs[st["a"] % 2]; st["a"] += 1
        for th in range(2):
            for kc in range(KC):
                c.op("pe", lambda e: e.matmul(acc[0:M, th * 512:(th + 1) * 512], lhsT=wb[:, kc, off:off + M],
                                              rhs=rhs[:, kc, th * 512:(th + 1) * 512], start=(kc == 0), stop=(kc == KC - 1)),
                     reads=[wb, rhs], writes=[acc])
        epi(acc, M)

    def epi_plain(dst_ap, dst_t, rep=None):
        def f(acc, M):
            s = stg[st["s"] % 3]; st["s"] += 1
            if rep is not None:
                c.op("dve", lambda e: e.tensor_tensor(out=s[0:M, :], in0=acc[0:M, :], in1=rep[0:M, :], op=ALU.mult),
                     reads=[acc, rep], writes=[s])
            else:
                en = c.alt()
                if en == "act":
                    c.op("act", lambda e: e.activation(out=s[0:M, :], in_=acc[0:M, :], func=AF.Copy), reads=[acc], writes=[s])
                else:
                    c.op("dve", lambda e: e.tensor_copy(out=s[0:M, :], in_=acc[0:M, :]), reads=[acc], writes=[s])
            c.dma("sp", dst_ap, s[0:M, :], reads=[s], writes=[dst_t])
        return f

    def epi_rope(r, dst_ap, dst_t, rep=None):
        def f(acc, M):
            p = pf[st["p"] % 2]; q = t1[st["p"] % 2]; st["p"] += 1
            if rep is not None:
                c.op("dve", lambda e: e.tensor_tensor(out=p[0:M, :], in0=acc[0:M, :], in1=rep[0:M, :], op=ALU.mult),
                     reads=[acc, rep], writes=[p])
            else:
                c.op("act", lambda e: e.activation(out=p[0:M, :], in_=acc[0:M, :], func=AF.Copy), reads=[acc], writes=[p])
            for th in range(2):
                c.op("pe", lambda e: e.matmul(rot_ps[0:M, th * 512:(th + 1) * 512], lhsT=perm[0:M, r, 0:M],
                                              rhs=p[0:M, th * 512:(th + 1) * 512], start=True, stop=True),
                     reads=[perm, p], writes=[rot_ps])
            c.op("dve", lambda e: e.tensor_tensor(out=q[0:M, :], in0=rot_ps[0:M, :], in1=SS[0:M, r, :], op=ALU.mult),
                 reads=[rot_ps, SS], writes=[q])
            c.op("pool", lambda e: e.tensor_tensor(out=p[0:M, :], in0=p[0:M, :], in1=CC[0:M, r, :], op=ALU.mult),
                 reads=[p, CC], writes=[p])
            s = stg[st["s"] % 3]; st["s"] += 1
            c.op("dve", lambda e: e.tensor_tensor(out=s[0:M, :], in0=p[0:M, :], in1=q[0:M, :], op=ALU.add),
                 reads=[p, q], writes=[s])
            c.dma("sp", dst_ap, s[0:M, :], reads=[s], writes=[dst_t])
        return f

    cn = {"q": c.sb([128, 4, TT], BF16, "cqn"), "kv": c.sb([128, 4, TT], BF16, "ckvn")}
    sq = c.sb([128, 4, TT], BF16, "sq")
    rstd = {"q": c.sb([128, TT], F32, "rstd_q"), "kv": c.sb([128, TT], F32, "rstd_kv")}
    rstd_tm = c.sb([128, 8], F32, "rstd_tm")
    eps_t = c.sb([128, 1], F32, "eps")
    c.op("pool", lambda e: e.memset(eps_t[:], 1e-6), writes=[eps_t])

    def epi_rms(which, i, g):
        def f(acc, M):
            c.op("act", lambda e: e.activation(out=sq[:, i, :], in_=acc[:, :], func=AF.Square), reads=[acc], writes=[sq])
            c.op("dve", lambda e: e.tensor_scalar(out=cn[which][:, i, :], in0=acc[:, :], scalar1=g[:, i:i + 1], scalar2=None, op0=ALU.mult),
                 reads=[acc, g], writes=[cn[which]])
        return f

    win = win_d.ap[l]
    for which, col0, g in (("q", 0, gq), ("kv", 512, gkv)):
        wb = load_w(win, D, col0, 512)
        for i in range(4):
            fm_chunk(wb, 16, 128 * i, 128, hTb, epi_rms(which, i, g))
        for th in range(2):
            for i in range(4):
                c.op("pe", lambda e: e.matmul(rep_ps[:, th * 512:(th + 1) * 512], lhsT=ones[:, :], rhs=sq[:, i, th * 512:(th + 1) * 512],
                                              start=(i == 0), stop=(i == 3)), reads=[ones, sq], writes=[rep_ps])
        c.op("act", lambda e: e.activation(out=rstd[which][:], in_=rep_ps[:], func=AF.Sqrt, scale=1.0 / 512.0, bias=eps_t[:, 0:1]),
             reads=[rep_ps, eps_t], writes=[rstd[which]])
        c.op("dve", lambda e: e.reciprocal(out=rstd[which][:], in_=rstd[which][:]), reads=[rstd[which]], writes=[rstd[which]])
        if which == "kv":
            for tt in range(8):
                for i in range(4):
                    c.op("pe", lambda e: e.matmul(rep_ps[:, tt:tt + 1], lhsT=sq[:, i, tt * 128:(tt + 1) * 128], rhs=ones[:, 0:1],
                                                  start=(i == 0), stop=(i == 3)), reads=[sq, ones], writes=[rep_ps])
            c.op("act", lambda e: e.activation(out=rstd_tm[:], in_=rep_ps[:, 0:8], func=AF.Sqrt, scale=1.0 / 512.0, bias=eps_t[:, 0:1]),
                 reads=[rep_ps, eps_t], writes=[rstd_tm])
            c.op("dve", lambda e: e.reciprocal(out=rstd_tm[:], in_=rstd_tm[:]), reads=[rstd_tm], writes=[rstd_tm])

    wb = load_w(win, D, C_KPE, 64)
    fm_chunk(wb, 16, 0, 64, hTb, epi_rope(0, kv[KPE_OFF:KPE_OFF + 64, :], kv))
    for g in range(2):
        wb = load_w(win, D, C_QB + 512 * g, 512)
        for j in range(4):
            fm_chunk(wb, 16, 128 * j, 128, hTb, epi_rope(1, io["qbT"][4 * g + j, :, :], io["qbT"]))
    for g in range(2):
        wb = load_w(win, D, C_KB + 512 * g, 512)
        for j in range(4):
            h = 4 * g + j
            fm_chunk(wb, 16, 128 * j, 128, hTb, epi_rope(1, kv[KB_OFF + 128 * h:KB_OFF + 128 * (h + 1), :], kv))
    for g in range(2):
        wb = load_w(win, D, C_VB + 512 * g, 512)
        for tt in range(8):
            acc = accs[st["a"] % 2]; st["a"] += 1
            for kc in range(16):
                c.op("pe", lambda e: e.matmul(acc[:, 0:512], lhsT=hTb[:, kc, tt * 128:(tt + 1) * 128], rhs=wb[:, kc, 0:512],
                                              start=(kc == 0), stop=(kc == 15)), reads=[hTb, wb], writes=[acc])
            s = stg[st["s"] % 3]; st["s"] += 1
            en = c.alt()
            if en == "act":
                c.op("act", lambda e: e.activation(out=s[:, 0:512], in_=acc[:, 0:512], func=AF.Copy), reads=[acc], writes=[s])
            else:
                c.op("dve", lambda e: e.tensor_copy(out=s[:, 0:512], in_=acc[:, 0:512]), reads=[acc], writes=[s])
            c.dma("sp", kv[VB_OFF + tt * 128:VB_OFF + (tt + 1) * 128, 512 * g:512 * (g + 1)], s[:, 0:512], reads=[s], writes=[kv])
    for g in range(2):
        wb = load_w(win, D, C_QI + 512 * g, 512)
        for j in range(4):
            fm_chunk(wb, 16, 128 * j, 128, hTb, epi_rope(2, io["qiT"][4 * g + j, :, :], io["qiT"]))
    wb = load_w(win, D, C_KI, 80)
    fm_chunk(wb, 16, 0, 64, hTb, epi_rope(2, kv[KI_OFF:KI_OFF + 64, :], kv))
    wst = c.sb([128, 8, 16], F32, "wst")
    for tt in range(8):
        acc = accs[st["a"] % 2]; st["a"] += 1
        for kc in range(16):
            c.op("pe", lambda e: e.matmul(acc[:, 0:16], lhsT=hTb[:, kc, tt * 128:(tt + 1) * 128], rhs=wb[:, kc, 64:80],
                                          start=(kc == 0), stop=(kc == 15)), reads=[hTb, wb], writes=[acc])
        c.op("act", lambda e: e.activation(out=wst[:, tt, :], in_=acc[:, 0:16], func=AF.Copy, scale=1.0 / 32.0), reads=[acc], writes=[wst])
    c.dma("sp", io["widx"].ap.rearrange("(tt p) h -> p tt h", p=128), wst[:], reads=[wst], writes=[io["widx"]])
    wqb = wqb_d.ap[l]
    for g in range(4):
        wb = load_w(wqb, 512, 384 * g, 384)
        for j in range(2):
            h = 2 * g + j
            fm_chunk(wb, 4, 192 * j, 128, cn["q"], epi_plain(io["qaT"][h, 0:128, :], io["qaT"], rep=rstd["q"]))
            fm_chunk(wb, 4, 192 * j + 128, 64, cn["q"], epi_rope(0, io["qaT"][h, 128:192, :], io["qaT"], rep=rstd["q"]))
    wkvb = wkvb_d.ap[l]
    for g in range(4):
        wb = load_w(wkvb, 512, 512 * g, 512)
        for j in range(2):
            h = 2 * g + j
            fm_chunk(wb, 4, 256 * j, 128, cn["kv"], epi_plain(kv[KA_OFF + 128 * h:KA_OFF + 128 * (h + 1), :], kv, rep=rstd["kv"]))
        for tt in range(8):
            acc = accs[st["a"] % 2]; st["a"] += 1
            for j in range(2):
                for kc in range(4):
                    c.op("pe", lambda e: e.matmul(acc[:, 128 * j:128 * (j + 1)], lhsT=cn["kv"][:, kc, tt * 128:(tt + 1) * 128],
                                                  rhs=wb[:, kc, 256 * j + 128:256 * j + 256], start=(kc == 0), stop=(kc == 3)),
                         reads=[cn["kv"], wb], writes=[acc])
            s = stg[st["s"] % 3]; st["s"] += 1
            c.op("dve", lambda e: e.tensor_scalar(out=s[:, 0:256], in0=acc[:, 0:256], scalar1=rstd_tm[:, tt:tt + 1], scalar2=None, op0=ALU.mult),
                 reads=[acc, rstd_tm], writes=[s])
            c.dma("sp", kv[VA_OFF + tt * 128:VA_OFF + (tt + 1) * 128, 256 * g:256 * (g + 1)], s[:, 0:256], reads=[s], writes=[kv])


def emit_B1(c, io):
    nc = c.nc
    NK = SEQ; NKC = NK // 128; LA = 2
    gath = io["gath"]

    def gk(row0, nrows):
        k = row0 // CH; off = row0 % CH
        rows = min(CH, KVROWS - k * CH)
        return io["gath%d" % k].ap.rearrange("(r f) t -> f r t", r=4)[off:off + nrows, :, :], io["gath%d" % k]

    sc = c.sb([128, NK], F32, "sc")
    tmpi = sc[:, :].bitcast(I32)
    posq_bc = c.sb([128, TT], F32, "posq_bc")
    c.dma("sp", tmpi[:, 0:TT], io["posq"].ap[0, :].partition_broadcast(128), reads=[io["posq"]], writes=[sc])
    c.op("dve", lambda e: e.tensor_copy(out=posq_bc[:], in_=tmpi[:, 0:TT]), reads=[sc], writes=[posq_bc])
    posk_bc = c.sb([128, NK], F32, "posk_bc")
    c.dma("sp", tmpi[:, :], io["posk"].ap[0, :].partition_broadcast(128), reads=[io["posk"]], writes=[sc])
    c.op("dve", lambda e: e.tensor_copy(out=posk_bc[:], in_=tmpi[:, :]), reads=[sc], writes=[posk_bc])
    pci = c.sb([128, 40], I32, "pci"); posk_col = c.sb([128, NKC], F32, "posk_col"); posq_col = c.sb([128, 8], F32, "posq_col")
    with nc.allow_non_contiguous_dma(reason="tiny position columns"):
        c.dma("sp", pci[:, 0:NKC], io["posk"].ap[0, :].rearrange("(kc p) -> p kc", p=128), reads=[io["posk"]], writes=[pci])
        c.dma("sp", pci[:, 32:40], io["posq"].ap[0, :].rearrange("(kc p) -> p kc", p=128), reads=[io["posq"]], writes=[pci])
    c.op("dve", lambda e: e.tensor_copy(out=posk_col[:], in_=pci[:, 0:NKC]), reads=[pci], writes=[posk_col])
    c.op("dve", lambda e: e.tensor_copy(out=posq_col[:], in_=pci[:, 32:40]), reads=[pci], writes=[posq_col])
    ones = c.sb([128, 128], BF16, "ones")
    c.op("pool", lambda e: e.memset(ones[:], 1.0), writes=[ones])
    ident = c.sb([128, 128], BF16, "ident")
    c.dma("sp", ident[:], io["identb"][:], reads=[io["identb"]], writes=[ident])

    KT = [c.sb([128, NK], BF16, f"KT{i}") for i in range(2)]
    VV = [c.sb([128, NKC, 128], BF16, f"VV{i}") for i in range(2)]
    QT = [c.sb([128, TT], BF16, f"QT{i}") for i in range(2)]
    QP = [c.sb([64, TT], BF16, f"QP{i}") for i in range(2)]
    kpe = c.sb([64, NK], BF16, "kpe")
    pts = [c.sb([128, 512], BF16, f"pt{i}") for i in range(4)]
    S_ps = [c.ps([128, 512], F32, f"S{i}") for i in range(3)]
    acc = c.ps([128, 512], F32, "acc")
    den = c.ps([128, 512], F32, "den")
    rden = c.sb([128, 512], F32, "rden")
    ost = [c.sb([128, 512], BF16, f"ost{i}") for i in range(2)]
    maskB = c.sb([128, NKC, TT], BF16, "maskB")
    st = {"s": 0, "p": 0, "o": 0, "b": 0}

    def attention_head(h, mla):
        b = st["b"] % 2; st["b"] += 1
        kt, vv, qt_, qp = KT[b], VV[b], QT[b], QP[b]
        koff, voff = (KA_OFF, VA_OFF) if mla else (KB_OFF, VB_OFF)
        ap_, gt = gk(koff + 128 * h, 128)
        c.dma("sp", kt[:, :].rearrange("p (r t) -> p r t", r=4), ap_, reads=[gt], writes=[kt])
        for r in range(4):
            for cc_ in range(2):
                gt = io["gath%d" % (voff // CH + cc_)]
                c.dma("sp", vv[:, 8 * r + 4 * cc_:8 * r + 4 * cc_ + 4, :],
                      gt.ap[r * CH:(r + 1) * CH, 128 * h:128 * (h + 1)].rearrange("(kc p) d -> p kc d", p=128), reads=[gt], writes=[vv])
        if mla:
            c.dma("sp", qt_[:], io["qaT"][h, 0:128, :], reads=[io["qaT"]], writes=[qt_])
            c.dma("sp", qp[:], io["qaT"][h, 128:192, :], reads=[io["qaT"]], writes=[qp])
            scale = 192.0 ** -0.5
        else:
            c.dma("sp", qt_[:], io["qbT"][h, :, :], reads=[io["qbT"]], writes=[qt_])
            scale = 128.0 ** -0.5
        for qh in range(2):
            qs = slice(qh * 512, (qh + 1) * 512)
            ptq = {}

            def front(kc):
                ks = slice(kc * 128, (kc + 1) * 128)
                S = S_ps[st["s"] % 3]; st["s"] += 1
                pt = pts[st["p"] % 4]; st["p"] += 1
                ptq[kc] = pt
                c.op("pe", lambda e: e.matmul(S[:], lhsT=kt[:, ks], rhs=qt_[:, qs], start=True, stop=not mla), reads=[kt, qt_], writes=[S])
                if mla:
                    c.op("pe", lambda e: e.matmul(S[:], lhsT=kpe[0:64, ks], rhs=qp[0:64, qs], start=False, stop=True), reads=[kpe, qp], writes=[S])
                c.op("act", lambda e: e.activation(out=pt[:], in_=S[:], func=AF.Exp, scale=scale), reads=[S], writes=[pt])
                if mla:
                    c.op("dve", lambda e: e.scalar_tensor_tensor(out=pt[:], in0=posq_bc[:, qs], scalar=posk_col[:, kc:kc + 1], in1=pt[:],
                                                                 op0=ALU.is_ge, op1=ALU.mult), reads=[posq_bc, posk_col, pt], writes=[pt])
                else:
                    c.op("pool", lambda e: e.tensor_tensor(out=pt[:], in0=pt[:], in1=maskB[:, kc, qs], op=ALU.mult), reads=[pt, maskB], writes=[pt])

            def back(kc):
                pt = ptq.pop(kc)
                c.op("pe", lambda e: e.matmul(acc[:], lhsT=vv[:, kc, :], rhs=pt[:], start=(kc == 0), stop=(kc == NKC - 1)), reads=[vv, pt], writes=[acc])
                c.op("pe", lambda e: e.matmul(den[:], lhsT=ones[:], rhs=pt[:], start=(kc == 0), stop=(kc == NKC - 1)), reads=[ones, pt], writes=[den])

            for step in range(NKC + LA):
                if step < NKC:
                    front(step)
                if step >= LA:
                    back(step - LA)
            c.op("dve", lambda e: e.reciprocal(out=rden[:], in_=den[:]), reads=[den], writes=[rden])
            o = ost[st["o"] % 2]; st["o"] += 1
            c.op("dve", lambda e: e.tensor_tensor(out=o[:], in0=acc[:], in1=rden[:], op=ALU.mult), reads=[acc, rden], writes=[o])
            dst = io["oaT"] if mla else io["obT"]
            c.dma("sp", dst[128 * h:128 * (h + 1), qs], o[:], reads=[o], writes=[dst])

    ki2 = c.sb([128, NK], BF16, "ki2")
    ap_, gt = gk(KI_OFF, 64)
    c.dma("sp", ki2[0:64, :].rearrange("p (r t) -> p r t", r=4), ap_, reads=[gt], writes=[ki2])
    c.dma("sp", ki2[64:128, :].rearrange("p (r t) -> p r t", r=4), ap_, reads=[gt], writes=[ki2])
    qi = c.sb([128, 8, TT], BF16, "qi")
    c.dma("sp", qi[:], io["qiT"].ap.rearrange("g p t -> p g t"), reads=[io["qiT"]], writes=[qi])
    wcol = c.sb([128, 8, 16], F32, "wcol")
    c.dma("sp", wcol[:], io["widx"].ap.rearrange("(qb p) h -> p qb h", p=128), reads=[io["widx"]], writes=[wcol])
    junk = c.sb([128, NK], BF16, "junk")
    rb = [c.sb([128, 512], F32, f"rb{i}") for i in range(3)]
    I_ps = [c.ps([128, 512], F32, "I0"), c.ps([128, 512], F32, "I1")]
    tp = c.ps([128, 512], BF16, "tp")
    sm = {k: c.sb([128, 1], F32, "sm_" + k) for k in ("lo", "hi", "mid", "cnt", "ge", "d1", "d2", "am")}
    ri = {"r": 0, "i": 0}

    def indexer(qb):
        qs = slice(qb * 128, (qb + 1) * 128)
        for kt_ in range(NK // 512):
            ksl = slice(kt_ * 512, (kt_ + 1) * 512)
            for h in range(16):
                pb = 64 * (h % 2)
                ip = I_ps[ri["i"] % 2]; ri["i"] += 1
                r = rb[ri["r"] % 3]; ri["r"] += 1
                c.op("pe", lambda e: e.matmul(ip[:], lhsT=qi[pb:pb + 64, h // 2, qs], rhs=ki2[pb:pb + 64, ksl], start=True, stop=True),
                     reads=[qi, ki2], writes=[ip])
                c.op("act", lambda e: e.activation(out=r[:], in_=ip[:], func=AF.Relu), reads=[ip], writes=[r])
                if h == 0:
                    c.op("dve", lambda e: e.tensor_scalar(out=sc[:, ksl], in0=r[:], scalar1=wcol[:, qb, 0:1], scalar2=None, op0=ALU.mult),
                         reads=[r, wcol], writes=[sc])
                else:
                    c.op("dve", lambda e: e.scalar_tensor_tensor(out=sc[:, ksl], in0=r[:], scalar=wcol[:, qb, h:h + 1], in1=sc[:, ksl],
                                                                 op0=ALU.mult, op1=ALU.add), reads=[r, wcol, sc], writes=[sc])
        c.op("dve", lambda e: e.tensor_reduce(out=sm["am"][:], in_=sc[:], axis=AX.X, op=ALU.max, apply_absolute_value=True),
             reads=[sc], writes=[sm["am"]])
        c.op("dve", lambda e: e.tensor_scalar(out=sm["hi"][:], in0=sm["am"][:], scalar1=1.001, scalar2=1e-6, op0=ALU.mult, op1=ALU.add),
             reads=[sm["am"]], writes=[sm["hi"]])
        c.op("dve", lambda e: e.tensor_scalar(out=sm["lo"][:], in0=sm["hi"][:], scalar1=-1.0, scalar2=None, op0=ALU.mult),
             reads=[sm["hi"]], writes=[sm["lo"]])
        c.op("pool", lambda e: e.tensor_scalar(out=junk[:], in0=posk_bc[:], scalar1=posq_col[:, qb:qb + 1], scalar2=-1e30,
                                               op0=ALU.is_gt, op1=ALU.mult), reads=[posk_bc, posq_col], writes=[junk])
        c.op("pool", lambda e: e.tensor_tensor(out=sc[:], in0=sc[:], in1=junk[:], op=ALU.add), reads=[sc, junk], writes=[sc])
        for it in range(NIT):
            c.op("dve", lambda e: e.tensor_tensor(out=sm["mid"][:], in0=sm["lo"][:], in1=sm["hi"][:], op=ALU.add),
                 reads=[sm["lo"], sm["hi"]], writes=[sm["mid"]])
            c.op("dve", lambda e: e.tensor_scalar(out=sm["mid"][:], in0=sm["mid"][:], scalar1=0.5, scalar2=None, op0=ALU.mult),
                 reads=[sm["mid"]], writes=[sm["mid"]])
            c.op("dve", lambda e: e.tensor_scalar(out=junk[:], in0=sc[:], scalar1=sm["mid"][:, 0:1], scalar2=0.0, op0=ALU.is_ge, op1=ALU.add,
                                                  accum_out=sm["cnt"][:]), reads=[sc, sm["mid"]], writes=[junk, sm["cnt"]])
            c.op("dve", lambda e: e.tensor_scalar(out=sm["ge"][:], in0=sm["cnt"][:], scalar1=TOPK - 0.5, scalar2=None, op0=ALU.is_ge),
                 reads=[sm["cnt"]], writes=[sm["ge"]])
            c.op("dve", lambda e: e.tensor_tensor(out=sm["d1"][:], in0=sm["mid"][:], in1=sm["lo"][:], op=ALU.subtract),
                 reads=[sm["mid"], sm["lo"]], writes=[sm["d1"]])
            c.op("dve", lambda e: e.tensor_tensor(out=sm["d2"][:], in0=sm["hi"][:], in1=sm["mid"][:], op=ALU.subtract),
                 reads=[sm["hi"], sm["mid"]], writes=[sm["d2"]])
            c.op("dve", lambda e: e.scalar_tensor_tensor(out=sm["lo"][:], in0=sm["d1"][:], scalar=sm["ge"][:, 0:1], in1=sm["lo"][:],
                                                         op0=ALU.mult, op1=ALU.add), reads=[sm["d1"], sm["ge"], sm["lo"]], writes=[sm["lo"]])
            c.op("dve", lambda e: e.scalar_tensor_tensor(out=sm["hi"][:], in0=sm["d2"][:], scalar=sm["ge"][:, 0:1], in1=sm["mid"][:],
                                                         op0=ALU.mult, op1=ALU.add), reads=[sm["d2"], sm["ge"], sm["mid"]], writes=[sm["hi"]])
        c.op("dve", lambda e: e.tensor_scalar(out=junk[:], in0=sc[:], scalar1=sm["lo"][:, 0:1], scalar2=None, op0=ALU.is_ge),
             reads=[sc, sm["lo"]], writes=[junk])
        for g in range(NKC // 4):
            for j in range(4):
                kc = 4 * g + j
                c.op("pe", lambda e: e.transpose(out=tp[:, j * 128:(j + 1) * 128], in_=junk[:, kc * 128:(kc + 1) * 128], identity=ident[:]),
                     reads=[junk, ident], writes=[tp])
            c.op("act", lambda e: e.activation(out=maskB[:, 4 * g:4 * g + 4, qs], in_=tp[:, :].rearrange("p (j q) -> p j q", j=4), func=AF.Copy),
                 reads=[tp], writes=[maskB])

    ap_, gt = gk(KPE_OFF, 64)
    c.dma("sp", kpe[:, :].rearrange("p (r t) -> p r t", r=4), ap_, reads=[gt], writes=[kpe])
    for i in range(8):
        indexer(i)
        attention_head(i, True)
    for h in range(8):
        attention_head(h, False)


def emit_ln(c, r, g_ap, b_ap, out_t, ps, onesf):
    nc = c.nc
    gcol = c.sb([128, 16], F32, "ln_g"); bcol = c.sb([128, 16], F32, "ln_b")
    with nc.allow_non_contiguous_dma(reason="tiny ln vectors"):
        c.dma("sp", gcol[:], g_ap.rearrange("(c p) -> p c", p=128), reads=[], writes=[gcol])
        c.dma("sp", bcol[:], b_ap.rearrange("(c p) -> p c", p=128), reads=[], writes=[bcol])
    tmp = c.sb([128, TT], F32, "ln_tmp")
    mean = c.sb([128, TT], F32, "ln_mean"); rstd = c.sb([128, TT], F32, "ln_rstd")
    for th in range(2):
        ts_ = slice(th * 512, (th + 1) * 512)
        for m in range(16):
            c.op("pe", lambda e: e.matmul(ps[th][:], lhsT=onesf[:, :], rhs=r[:, m, ts_], start=(m == 0), stop=(m == 15)),
                 reads=[onesf, r], writes=[ps[th]])
    for m in range(16):
        c.op("act", lambda e: e.activation(out=tmp[:, :], in_=r[:, m, :], func=AF.Square), reads=[r], writes=[tmp])
        for th in range(2):
            ts_ = slice(th * 512, (th + 1) * 512)
            c.op("pe", lambda e: e.matmul(ps[2 + th][:], lhsT=onesf[:, :], rhs=tmp[:, ts_], start=(m == 0), stop=(m == 15)),
                 reads=[onesf, tmp], writes=[ps[2 + th]])
    for th in range(2):
        ts_ = slice(th * 512, (th + 1) * 512)
        c.op("act", lambda e: e.activation(out=mean[:, ts_], in_=ps[th][:], func=AF.Copy, scale=1.0 / D), reads=[ps[th]], writes=[mean])
        c.op("dve", lambda e: e.tensor_tensor(out=tmp[:, ts_], in0=mean[:, ts_], in1=mean[:, ts_], op=ALU.mult), reads=[mean], writes=[tmp])
        c.op("dve", lambda e: e.scalar_tensor_tensor(out=rstd[:, ts_], in0=ps[2 + th][:], scalar=1.0 / D, in1=tmp[:, ts_], op0=ALU.mult, op1=ALU.subtract),
             reads=[ps[2 + th], tmp], writes=[rstd])
    c.op("dve", lambda e: e.tensor_scalar(out=rstd[:], in0=rstd[:], scalar1=1e-5, scalar2=None, op0=ALU.add), reads=[rstd], writes=[rstd])
    c.op("act", lambda e: e.activation(out=rstd[:], in_=rstd[:], func=AF.Sqrt), reads=[rstd], writes=[rstd])
    c.op("dve", lambda e: e.reciprocal(out=rstd[:], in_=rstd[:]), reads=[rstd], writes=[rstd])
    for m in range(16):
        c.op("dve", lambda e: e.tensor_tensor(out=r[:, m, :], in0=r[:, m, :], in1=mean[:], op=ALU.subtract), reads=[r, mean], writes=[r])
        c.op("pool", lambda e: e.tensor_tensor(out=r[:, m, :], in0=r[:, m, :], in1=rstd[:], op=ALU.mult), reads=[r, rstd], writes=[r])
        c.op("dve", lambda e: e.tensor_scalar(out=r[:, m, :], in0=r[:, m, :], scalar1=gcol[:, m:m + 1], scalar2=bcol[:, m:m + 1],
                                              op0=ALU.mult, op1=ALU.add), reads=[r, gcol, bcol], writes=[r])
    for i in range(4):
        c.dma("sp", out_t.ap[512 * i:512 * (i + 1), :].rearrange("(kc p) t -> p kc t", p=128), r[:, 4 * i:4 * i + 4, :], reads=[r], writes=[out_t])


def emit_B2(c, io, l, hin, hout):
    nc = c.nc
    win = io["w_in"].ap[l]; woa = io["w_o_a"].ap[l]; wob = io["w_o_b"].ap[l]; wout = io["w_out"].ap[l]
    hTb = c.sb([128, 16, TT], BF16, "hTb"); r = c.sb([128, 16, TT], F32, "r")
    for i in range(4):
        c.dma("pool", hTb[:, 4 * i:4 * i + 4, :], hin.ap[512 * i:512 * (i + 1), :].rearrange("(kc p) t -> p kc t", p=128), reads=[hin], writes=[hTb])
        c.dma("sp", r[:, 4 * i:4 * i + 4, :], hin.ap[512 * i:512 * (i + 1), :].rearrange("(kc p) t -> p kc t", p=128), reads=[hin], writes=[r])
    oa = c.sb([128, 8, TT], BF16, "oa"); ob = c.sb([128, 8, TT], BF16, "ob")
    c.dma("sp", oa[:], io["oaT"].ap.rearrange("(kc p) t -> p kc t", p=128), reads=[io["oaT"]], writes=[oa])
    c.dma("sp", ob[:], io["obT"].ap.rearrange("(kc p) t -> p kc t", p=128), reads=[io["obT"]], writes=[ob])
    yT = c.sb([128, 16, TT], BF16, "yT")
    onesf = c.sb([128, 128], F32, "onesf")
    c.op("pool", lambda e: e.memset(onesf[:], 1.0), writes=[onesf])
    wg = [c.sb([128, 16, 256], BF16, f"wg{i}") for i in range(2)]
    wo = [c.sb([128, 8, 256], BF16, f"wo{i}") for i in range(2)]
    ps = [c.ps([128, 512], F32, f"ps{i}") for i in range(8)]
    sg = [c.sb([128, 512], F32, f"sg{i}") for i in range(2)]
    ta = c.sb([128, 512], F32, "ta"); tb = c.sb([128, 512], F32, "tb")
    k = 0
    for grp in range(8):
        c.dma("pool", wg[0][:], win[:, C_GA + 256 * grp:C_GA + 256 * (grp + 1)].rearrange("(kc p) m -> p kc m", p=128), reads=[], writes=[wg[0]])
        c.dma("pool", wg[1][:], win[:, C_GB + 256 * grp:C_GB + 256 * (grp + 1)].rearrange("(kc p) m -> p kc m", p=128), reads=[], writes=[wg[1]])
        c.dma("pool", wo[0][:], woa[:, 256 * grp:256 * (grp + 1)].rearrange("(kc p) m -> p kc m", p=128), reads=[], writes=[wo[0]])
        c.dma("pool", wo[1][:], wob[:, 256 * grp:256 * (grp + 1)].rearrange("(kc p) m -> p kc m", p=128), reads=[], writes=[wo[1]])
        for j in range(2):
            m = 2 * grp + j
            ms = slice(128 * j, 128 * (j + 1))
            for th in range(2):
                ts_ = slice(th * 512, (th + 1) * 512)
                p4 = ps[4 * (k % 2):4 * (k % 2) + 4]; k += 1
                for kc in range(16):
                    c.op("pe", lambda e: e.matmul(p4[0][:], lhsT=wg[0][:, kc, ms], rhs=hTb[:, kc, ts_], start=(kc == 0), stop=(kc == 15)), reads=[wg[0], hTb], writes=[p4[0]])
                for kc in range(16):
                    c.op("pe", lambda e: e.matmul(p4[1][:], lhsT=wg[1][:, kc, ms], rhs=hTb[:, kc, ts_], start=(kc == 0), stop=(kc == 15)), reads=[wg[1], hTb], writes=[p4[1]])
                for kc in range(8):
                    c.op("pe", lambda e: e.matmul(p4[2][:], lhsT=wo[0][:, kc, ms], rhs=oa[:, kc, ts_], start=(kc == 0), stop=(kc == 7)), reads=[wo[0], oa], writes=[p4[2]])
                for kc in range(8):
                    c.op("pe", lambda e: e.matmul(p4[3][:], lhsT=wo[1][:, kc, ms], rhs=ob[:, kc, ts_], start=(kc == 0), stop=(kc == 7)), reads=[wo[1], ob], writes=[p4[3]])
                c.op("act", lambda e: e.activation(out=sg[0][:], in_=p4[0][:], func=AF.Sigmoid), reads=[p4[0]], writes=[sg[0]])
                c.op("act", lambda e: e.activation(out=sg[1][:], in_=p4[1][:], func=AF.Sigmoid), reads=[p4[1]], writes=[sg[1]])
                c.op("dve", lambda e: e.tensor_tensor(out=ta[:], in0=p4[2][:], in1=sg[0][:], op=ALU.mult), reads=[p4[2], sg[0]], writes=[ta])
                c.op("dve", lambda e: e.tensor_tensor(out=tb[:], in0=p4[3][:], in1=sg[1][:], op=ALU.mult), reads=[p4[3], sg[1]], writes=[tb])
                c.op("pool", lambda e: e.tensor_tensor(out=yT[:, m, ts_], in0=ta[:], in1=tb[:], op=ALU.add), reads=[ta, tb], writes=[yT])
    for grp in range(8):
        w = wg[grp % 2]
        c.dma("pool", w[:], wout[:, 256 * grp:256 * (grp + 1)].rearrange("(kc p) m -> p kc m", p=128), reads=[], writes=[w])
        for j in range(2):
            m = 2 * grp + j
            ms = slice(128 * j, 128 * (j + 1))
            for th in range(2):
                ts_ = slice(th * 512, (th + 1) * 512)
                p = ps[k % 8]; k += 1
                for kc in range(16):
                    c.op("pe", lambda e: e.matmul(p[:], lhsT=w[:, kc, ms], rhs=yT[:, kc, ts_], start=(kc == 0), stop=(kc == 15)), reads=[w, yT], writes=[p])
                c.op("dve", lambda e: e.scalar_tensor_tensor(out=r[:, m, ts_], in0=r[:, m, ts_], scalar=ALPHA, in1=p[:], op0=ALU.mult, op1=ALU.add),
                     reads=[r, p], writes=[r])
    emit_ln(c, r, io["ln1_g"].ap[l], io["ln1_b"].ap[l], hout, ps, onesf)


def emit_router(c, io, l, r, g_ps):
    nc = c.nc
    wr = c.sb([128, 16, 72], F32, "wr"); bias = c.sb([128, 72], F32, "bias")
    with nc.allow_non_contiguous_dma(reason="small router weights"):
        c.dma("sp", wr[:, :, 0:8], io["w_group"].ap[l].rearrange("(kc p) m -> p kc m", p=128), reads=[], writes=[wr])
        c.dma("sp", wr[:, :, 8:72], io["w_router"].ap[l].rearrange("(kc p) m -> p kc m", p=128), reads=[], writes=[wr])
    c.dma("sp", bias[:, 0:8], io["b_group"].ap[l].partition_broadcast(128), reads=[], writes=[bias])
    c.dma("sp", bias[:, 8:72], io["b_router"].ap[l].partition_broadcast(128), reads=[], writes=[bias])
    CW = c.sb([128, 8, 64], F32, "CW")
    GM = c.sb([128, 8, 8], F32, "GM")
    lg = c.sb([128, 72], F32, "lg"); em = c.sb([128, 64], F32, "em"); em2 = c.sb([128, 64], F32, "em2")
    mk1 = c.sb([128, 64], F32, "mk1"); mk2 = c.sb([128, 64], F32, "mk2"); gex = c.sb([128, 8], F32, "gex")
    s = {k: c.sb([128, 1], F32, "r_" + k) for k in ("gmax", "ngmax", "gsum", "gp", "m1", "m2", "d", "ex", "w1", "w2")}
    pen = c.sb([128, 8], F32, "pen")
    for tt in range(8):
        tsl = slice(tt * 128, (tt + 1) * 128)
        gmask = GM[:, tt, :]
        for kc in range(16):
            c.op("pe", lambda e: e.matmul(g_ps[:, 0:72], lhsT=r[:, kc, tsl], rhs=wr[:, kc, :], start=(kc == 0), stop=(kc == 15)), reads=[r, wr], writes=[g_ps])
        c.op("dve", lambda e: e.tensor_tensor(out=lg[:], in0=g_ps[:, 0:72], in1=bias[:], op=ALU.add), reads=[g_ps, bias], writes=[lg])
        c.op("dve", lambda e: e.tensor_reduce(out=s["gmax"][:], in_=lg[:, 0:8], axis=AX.X, op=ALU.max), reads=[lg], writes=[s["gmax"]])
        c.op("dve", lambda e: e.tensor_scalar(out=gmask, in0=lg[:, 0:8], scalar1=s["gmax"][:, 0:1], scalar2=None, op0=ALU.is_ge), reads=[lg, s["gmax"]], writes=[GM])
        c.op("dve", lambda e: e.tensor_scalar(out=s["ngmax"][:], in0=s["gmax"][:], scalar1=-1.0, scalar2=None, op0=ALU.mult), reads=[s["gmax"]], writes=[s["ngmax"]])
        c.op("act", lambda e: e.activation(out=gex[:], in_=lg[:, 0:8], func=AF.Exp, bias=s["ngmax"][:, 0:1], accum_out=s["gsum"][:]),
             reads=[lg, s["ngmax"]], writes=[gex, s["gsum"]])
        c.op("dve", lambda e: e.reciprocal(out=s["gp"][:], in_=s["gsum"][:]), reads=[s["gsum"]], writes=[s["gp"]])
        c.op("dve", lambda e: e.tensor_scalar(out=pen[:], in0=gmask, scalar1=-1.0, scalar2=1e30, op0=ALU.add, op1=ALU.mult), reads=[GM], writes=[pen])
        for g in range(8):
            c.op("dve", lambda e: e.tensor_scalar(out=em[:, 8 * g:8 * g + 8], in0=lg[:, 8 + 8 * g:16 + 8 * g], scalar1=pen[:, g:g + 1], scalar2=None, op0=ALU.add),
                 reads=[lg, pen], writes=[em])
        c.op("dve", lambda e: e.tensor_reduce(out=s["m1"][:], in_=em[:], axis=AX.X, op=ALU.max), reads=[em], writes=[s["m1"]])
        c.op("dve", lambda e: e.tensor_scalar(out=mk1[:], in0=em[:], scalar1=s["m1"][:, 0:1], scalar2=None, op0=ALU.is_ge), reads=[em, s["m1"]], writes=[mk1])
        c.op("dve", lambda e: e.scalar_tensor_tensor(out=em2[:], in0=mk1[:], scalar=-1e30, in1=em[:], op0=ALU.mult, op1=ALU.add), reads=[mk1, em], writes=[em2])
        c.op("dve", lambda e: e.tensor_reduce(out=s["m2"][:], in_=em2[:], axis=AX.X, op=ALU.max), reads=[em2], writes=[s["m2"]])
        c.op("dve", lambda e: e.tensor_scalar(out=mk2[:], in0=em2[:], scalar1=s["m2"][:, 0:1], scalar2=None, op0=ALU.is_ge), reads=[em2, s["m2"]], writes=[mk2])
        c.op("dve", lambda e: e.tensor_tensor(out=s["d"][:], in0=s["m2"][:], in1=s["m1"][:], op=ALU.subtract), reads=[s["m1"], s["m2"]], writes=[s["d"]])
        c.op("act", lambda e: e.activation(out=s["ex"][:], in_=s["d"][:], func=AF.Exp), reads=[s["d"]], writes=[s["ex"]])
        c.op("dve", lambda e: e.tensor_scalar(out=s["ex"][:], in0=s["ex"][:], scalar1=1.0, scalar2=None, op0=ALU.add), reads=[s["ex"]], writes=[s["ex"]])
        c.op("dve", lambda e: e.reciprocal(out=s["ex"][:], in_=s["ex"][:]), reads=[s["ex"]], writes=[s["ex"]])
        c.op("dve", lambda e: e.tensor_tensor(out=s["w1"][:], in0=s["gp"][:], in1=s["ex"][:], op=ALU.mult), reads=[s["gp"], s["ex"]], writes=[s["w1"]])
        c.op("dve", lambda e: e.tensor_tensor(out=s["w2"][:], in0=s["gp"][:], in1=s["w1"][:], op=ALU.subtract), reads=[s["gp"], s["w1"]], writes=[s["w2"]])
        c.op("dve", lambda e: e.tensor_scalar(out=mk1[:], in0=mk1[:], scalar1=s["w1"][:, 0:1], scalar2=None, op0=ALU.mult), reads=[mk1, s["w1"]], writes=[mk1])
        c.op("dve", lambda e: e.scalar_tensor_tensor(out=CW[:, tt, :], in0=mk2[:], scalar=s["w2"][:, 0:1], in1=mk1[:], op0=ALU.mult, op1=ALU.add),
             reads=[mk2, s["w2"], mk1], writes=[CW])
    return CW, GM


def emit_B3_dense(c, io, l, hin, hout, n_tt=8, n_e=64):
    nc = c.nc
    hTb = c.sb([128, 16, TT], BF16, "hTb"); r = c.sb([128, 16, TT], F32, "r")
    for i in range(4):
        c.dma("pool", hTb[:, 4 * i:4 * i + 4, :], hin.ap[512 * i:512 * (i + 1), :].rearrange("(kc p) t -> p kc t", p=128), reads=[hin], writes=[hTb])
        c.dma("sp", r[:, 4 * i:4 * i + 4, :], hin.ap[512 * i:512 * (i + 1), :].rearrange("(kc p) t -> p kc t", p=128), reads=[hin], writes=[r])
    identb = c.sb([128, 128], BF16, "identb"); identf = c.sb([128, 128], F32, "identf")
    c.dma("sp", identb[:], io["identb"][:], reads=[], writes=[identb])
    c.dma("sp", identf[:], io["identf"][:], reads=[], writes=[identf])
    onesf = c.sb([128, 128], F32, "onesf")
    c.op("pool", lambda e: e.memset(onesf[:], 1.0), writes=[onesf])
    y_ps = [c.ps([128, 512], F32, f"y{i}") for i in range(4)]
    g_ps = c.ps([128, 512], F32, "g"); u_ps = c.ps([128, 512], F32, "u")
    tp_ps = c.ps([128, 512], BF16, "tp")
    CW, GM = emit_router(c, io, l, r, g_ps)
    Wg = [c.sb([128, 16, 512], BF16, f"Wg{i}") for i in range(2)]
    Wu = [c.sb([128, 16, 512], BF16, f"Wu{i}") for i in range(1)] * 2
    Wd = [c.sb([128, 4, D], BF16, f"Wd{i}") for i in range(1)] * 2
    sg = c.sb([128, 512], F32, "sg"); hid = c.sb([128, 512], BF16, "hid"); hidT = c.sb([128, 4, 128], BF16, "hidT")
    ysb = c.sb([128, D], F32, "ysb")
    k = 0
    for tt in range(n_tt):
        tsl = slice(tt * 128, (tt + 1) * 128)
        for ex in range(n_e):
            wgb, wub, wdb = Wg[k % 2], Wu[k % 2], Wd[k % 2]; k += 1
            c.dma("pool", wgb[:], io["w_e_gate"].ap[l, ex].rearrange("(kc p) m -> p kc m", p=128), reads=[], writes=[wgb])
            c.dma("pool", wub[:], io["w_e_up"].ap[l, ex].rearrange("(kc p) m -> p kc m", p=128), reads=[], writes=[wub])
            c.dma("pool", wdb[:], io["w_e_down"].ap[l, ex].rearrange("(kc p) m -> p kc m", p=128), reads=[], writes=[wdb])
            for kc in range(16):
                c.op("pe", lambda e: e.matmul(g_ps[:], lhsT=hTb[:, kc, tsl], rhs=wgb[:, kc, :], start=(kc == 0), stop=(kc == 15)), reads=[hTb, wgb], writes=[g_ps])
            for kc in range(16):
                c.op("pe", lambda e: e.matmul(u_ps[:], lhsT=hTb[:, kc, tsl], rhs=wub[:, kc, :], start=(kc == 0), stop=(kc == 15)), reads=[hTb, wub], writes=[u_ps])
            c.op("act", lambda e: e.activation(out=sg[:], in_=g_ps[:], func=AF.Silu), reads=[g_ps], writes=[sg])
            c.op("dve", lambda e: e.scalar_tensor_tensor(out=hid[:], in0=sg[:], scalar=CW[:, tt, ex:ex + 1], in1=u_ps[:], op0=ALU.mult, op1=ALU.mult),
                 reads=[sg, CW, u_ps], writes=[hid])
            for fc in range(4):
                c.op("pe", lambda e: e.transpose(out=tp_ps[:, fc * 128:(fc + 1) * 128], in_=hid[:, fc * 128:(fc + 1) * 128], identity=identb[:]),
                     reads=[hid, identb], writes=[tp_ps])
            c.op("act", lambda e: e.activation(out=hidT[:, :, :], in_=tp_ps[:, :].rearrange("p (f t) -> p f t", f=4), func=AF.Copy), reads=[tp_ps], writes=[hidT])
            for n in range(4):
                for fc in range(4):
                    c.op("pe", lambda e: e.matmul(y_ps[n][:], lhsT=hidT[:, fc, :], rhs=wdb[:, fc, n * 512:(n + 1) * 512],
                                                  start=(ex == 0 and fc == 0), stop=(ex == n_e - 1 and fc == 3)), reads=[hidT, wdb], writes=[y_ps[n]])
        for n in range(4):
            c.op("act", lambda e: e.activation(out=ysb[:, n * 512:(n + 1) * 512], in_=y_ps[n][:], func=AF.Copy), reads=[y_ps[n]], writes=[ysb])
        for m in range(16):
            c.op("pe", lambda e: e.transpose(out=u_ps[:, 0:128], in_=ysb[:, m * 128:(m + 1) * 128], identity=identf[:]), reads=[ysb, identf], writes=[u_ps])
            c.op("dve", lambda e: e.scalar_tensor_tensor(out=r[:, m, tsl], in0=r[:, m, tsl], scalar=ALPHA, in1=u_ps[:, 0:128], op0=ALU.mult, op1=ALU.add),
                 reads=[r, u_ps], writes=[r])
    for tt in range(n_tt, 8):
        tsl = slice(tt * 128, (tt + 1) * 128)
        c.op("dve", lambda e: e.tensor_scalar(out=r[:, :, tsl], in0=r[:, :, tsl], scalar1=ALPHA, scalar2=None, op0=ALU.mult), reads=[r], writes=[r])
    emit_ln(c, r, io["ln2_g"].ap[l], io["ln2_b"].ap[l], hout, y_ps, onesf)


W_SPECS = (("w_in", [D, IN_W]), ("g_q", [512]), ("w_q_b", [512, 1536]), ("g_kv", [512]), ("w_kv_b", [512, 2048]),
           ("w_o_a", [1024, D]), ("w_o_b", [1024, D]), ("w_out", [D, D]), ("ln1_g", [D]), ("ln1_b", [D]),
           ("w_group", [D, 8]), ("b_group", [8]), ("w_router", [D, 64]), ("b_router", [64]),
           ("w_e_gate", [64, D, 512]), ("w_e_up", [64, D, 512]), ("w_e_down", [64, 512, D]), ("ln2_g", [D]), ("ln2_b", [D]))


def build_fused(depth=2, moe="dense", moe_kw=None, n_exp=64):
    c = Ctx(); nc = c.nc
    io = {}
    io["xT"] = c.dram("xT", [D, TT], F32, "ExternalInput")
    io["posq"] = c.dram("posq", [1, TT], I32, "ExternalInput")
    io["posk"] = c.dram("posk", [1, SEQ], I32, "ExternalInput")
    for nm, shp in W_SPECS:
        if nm.startswith("w_e_"):
            shp = [n_exp] + shp[1:]
        io[nm] = c.dram(nm, [depth] + shp, F32, "ExternalInput")
    io["inv"] = c.dram("inv", [128, 3], F32, "ExternalInput")
    io["perm"] = c.dram("perm", [3, 128, 128], F32, "ExternalInput")
    io["identb"] = c.dram("identb", [128, 128], BF16, "ExternalInput")
    io["identf"] = c.dram("identf", [128, 128], F32, "ExternalInput")
    io["outT"] = c.dram("outT", [D, TT], F32, "ExternalOutput")
    io["qaT"] = c.dram("qaT", [8, 192, TT], BF16); io["qbT"] = c.dram("qbT", [8, 128, TT], BF16); io["qiT"] = c.dram("qiT", [8, 128, TT], BF16)
    io["widx"] = c.dram("widx", [TT, 16], F32)
    io["kvpack"] = c.dram("kvpack", [KVROWS, TT], BF16)
    io["gath"] = None
    for k in range(NCH):
        rows = min(CH, KVROWS - k * CH)
        io["gath%d" % k] = c.dram("gath%d" % k, [4 * rows, TT], BF16)
    io["oaT"] = c.dram("oaT", [1024, TT], BF16); io["obT"] = c.dram("obT", [1024, TT], BF16)
    io["hA"] = c.dram("hA", [D, TT], F32); io["hB"] = c.dram("hB", [D, TT], F32)
    hin = io["xT"]
    for l in range(depth):
        c.phase_begin(); emit_A(c, io, l, hin); c.phase_end()
        if UPTO < 1:
            break
        for k in range(NCH):
            rows = min(CH, KVROWS - k * CH)
            c.collective("AllGather", io["kvpack"], io["kvpack"].ap[k * CH:k * CH + rows, :], io["gath%d" % k], io["gath%d" % k].ap[:, :], GROUPS)
        if UPTO < 2:
            break
        c.phase_begin(); emit_B1(c, io); c.phase_end()
        if UPTO < 3:
            break
        c.phase_begin(); emit_B2(c, io, l, hin, io["hA"]); c.phase_end()
        if UPTO < 4:
            break
        hout = io["outT"] if l == depth - 1 else io["hB"]
        c.phase_begin()
        if moe == "dense":
            emit_B3_dense(c, io, l, io["hA"], hout, **(moe_kw or {}))
        else:
            emit_B3(c, io, l, io["hA"], hout, **(moe_kw or {}))
        c.phase_end()
        hin = hout
    c.finish()
    return nc


_CACHE = {}
MOE_MODE = "dense"
UPTO = 99


def kernel(x, positions, w_in, g_q_lora, w_q_b, g_kv_lora, w_kv_b, w_o_a, w_o_b, w_out, ln1_g, ln1_b,
           w_group, b_group, w_router, b_router, w_e_gate, w_e_up, w_e_down, ln2_g, ln2_b):
    x = np.asarray(x); positions = np.asarray(positions)
    depth = int(np.asarray(w_in).shape[0])
    key = ("fused", depth, MOE_MODE)
    if key not in _CACHE:
        _CACHE[key] = build_fused(depth, MOE_MODE)
    nc = _CACHE[key]
    inv, perm = _rope_consts()
    identf = np.eye(128, dtype=np.float32); identb = identf.astype(NPBF)
    wts = {"w_in": w_in, "g_q": g_q_lora, "w_q_b": w_q_b, "g_kv": g_kv_lora, "w_kv_b": w_kv_b, "w_o_a": w_o_a, "w_o_b": w_o_b,
           "w_out": w_out, "ln1_g": ln1_g, "ln1_b": ln1_b, "w_group": w_group, "b_group": b_group, "w_router": w_router,
           "b_router": b_router, "w_e_gate": w_e_gate, "w_e_up": w_e_up, "w_e_down": w_e_down, "ln2_g": ln2_g, "ln2_b": ln2_b}
    wts = {k: np.ascontiguousarray(np.asarray(v), dtype=np.float32) for k, v in wts.items()}
    nb = x.shape[0]
    cores = [(b, j) for b in range(nb) for j in range(4)]
    in_maps = []
    for (b, j) in cores:
        m = dict(wts)
        m["xT"] = np.ascontiguousarray(x[b, j * TT:(j + 1) * TT, :].T)
        m["posq"] = np.ascontiguousarray(positions[b, j * TT:(j + 1) * TT][None, :]).astype(np.int32)
        m["posk"] = np.ascontiguousarray(positions[b][None, :]).astype(np.int32)
        m["inv"] = inv; m["perm"] = perm; m["identb"] = identb; m["identf"] = identf
        in_maps.append(m)
    res = run_bass_kernel_spmd(nc, in_maps, core_ids=list(range(len(cores)))).results
    out = np.empty(x.shape, np.float32)
    for ci, (b, j) in enumerate(cores):
        out[b, j * TT:(j + 1) * TT, :] = res[ci]["outT"].T
    return out
```

```python
import math
import numpy as np
from contextlib import ExitStack
import ml_dtypes
import concourse.bass as bass
import concourse.mybir as mybir
from concourse.bass_utils import run_bass_kernel_spmd

F32 = mybir.dt.float32; BF16 = mybir.dt.bfloat16; I32 = mybir.dt.int32
AF = mybir.ActivationFunctionType; ALU = mybir.AluOpType; AX = mybir.AxisListType
NPBF = ml_dtypes.bfloat16

D = 2048; TT = 1024; NCORE = 8; SEQ = 4096
IN_W = 9360
C_KPE = 1024; C_QB = 1088; C_KB = 2112; C_VB = 3136; C_QI = 4160; C_KI = 5184; C_WI = 5248; C_GA = 5264; C_GB = 7312
ALPHA = 4 ** 0.25
THETA = 500000.0


class T:
    __slots__ = ("ap", "w", "r", "name", "excl")

    def __init__(self, ap, name="", excl=False):
        self.ap = ap; self.w = None; self.r = {}; self.name = name; self.excl = excl

    def __getitem__(self, k):
        return self.ap[k]


class E:
    def __init__(self, name, obj, sem):
        self.name = name; self.obj = obj; self.sem = sem; self.count = 0; self.seen = {}


class Ctx:
    NDMASEM = 8

    def __init__(self):
        self.nc = nc = bass.Bass("TRN2", target_bir_lowering=False)
        self.es = ExitStack()
        self.eng = {}
        for name, obj in (("pe", nc.tensor), ("act", nc.scalar), ("dve", nc.vector), ("pool", nc.gpsimd), ("sp", nc.sync)):
            sem = self.es.enter_context(nc.semaphore("s_" + name))
            self.eng[name] = E(name, obj, sem)
        self.dq = {}
        for q in ("sp", "pool", "act"):
            lst = []
            for i in range(self.NDMASEM):
                sem = self.es.enter_context(nc.semaphore(f"d_{q}{i}"))
                lst.append(E(f"d_{q}{i}", None, sem))
            self.dq[q] = [lst, 0]
        self.cc = E("cc", None, self.es.enter_context(nc.semaphore("cc_sem")))
        self.pes = None
        self.ntile = 0
        self.rr = 0

    def sb(self, shape, dt, name=None):
        self.ntile += 1
        name = f"sb_{name or 't'}_{self.ntile}"
        h = (self.pes or self.es).enter_context(self.nc.sbuf_tensor(name, list(shape), dt))
        return T(h, name)

    def ps(self, shape, dt=F32, name=None):
        self.ntile += 1
        name = f"ps_{name or 'p'}_{self.ntile}"
        h = (self.pes or self.es).enter_context(self.nc.psum_tensor(name, list(shape), dt))
        return T(h, name, excl=True)

    def dram(self, name, shape, dt, kind="Internal"):
        h = self.nc.dram_tensor(name, list(shape), dt, kind=kind)
        return T(h.ap(), name)

    def _deps(self, e, reads, writes, extra=()):
        deps = {}

        def add(d):
            if d is None:
                return
            p, idx = d
            if p is e and e.name in ("pe", "sp"):
                return
            if deps.get(p, 0) < idx:
                deps[p] = idx
        for t in reads:
            add(t.w)
            if t.excl:
                for p, idx in t.r.items():
                    add((p, idx))
        for t in writes:
            add(t.w)
            for p, idx in t.r.items():
                add((p, idx))
        for d in extra:
            add(d)
        for p, idx in deps.items():
            if e.seen.get(p, 0) < idx:
                e.obj.wait_ge(p.sem, idx)
                e.seen[p] = idx

    def op(self, en, fn, reads=(), writes=()):
        e = self.eng[en]
        self._deps(e, reads, writes)
        inst = fn(e.obj)
        e.count += 1
        inst.then_inc(e.sem, 1)
        for t in writes:
            t.w = (e, e.count); t.r = {}
        for t in reads:
            t.r[e] = e.count
        return inst

    def dma(self, q, out, in_, reads=(), writes=(), **kw):
        e = self.eng[q]
        lst, i = self.dq[q]
        d = lst[i % len(lst)]; self.dq[q][1] = i + 1
        extra = [(d, d.count)] if d.count else []
        self._deps(e, reads, writes, extra)
        inst = e.obj.dma_start(out=out, in_=in_, **kw)
        d.count += 16
        inst.then_inc(d.sem, 16)
        for t in writes:
            t.w = (d, d.count); t.r = {}
        for t in reads:
            t.r[d] = d.count
        return inst

    def barrier(self):
        engs = list(self.eng.values())
        others = engs + [d for (lst, _) in self.dq.values() for d in lst] + [self.cc]
        for e in engs:
            for p in others:
                if p is e:
                    continue
                if p.count and e.seen.get(p, 0) < p.count:
                    e.obj.wait_ge(p.sem, p.count); e.seen[p] = p.count

    def phase_begin(self):
        self.pes = ExitStack()

    def phase_end(self):
        self.barrier()
        self.pes.close(); self.pes = None

    def collective(self, kind, in_t, in_ap, out_t, out_ap, groups):
        e = self.eng["pool"]; d = self.cc
        self._deps(e, [in_t], [out_t])
        inst = e.obj.collective_compute(kind, ALU.bypass, replica_groups=groups, ins=[in_ap], outs=[out_ap])
        d.count += 1
        inst.then_inc(d.sem, 1)
        out_t.w = (d, d.count); out_t.r = {}
        in_t.r[d] = d.count

    def finish(self):
        e = self.eng["sp"]
        for p in list(self.eng.values()):
            if p is not e and p.count and e.seen.get(p, 0) < p.count:
                e.obj.wait_ge(p.sem, p.count); e.seen[p] = p.count
        for q, (lst, _) in self.dq.items():
            for d in lst:
                if d.count and e.seen.get(d, 0) < d.count:
                    e.obj.wait_ge(d.sem, d.count); e.seen[d] = d.count
        if self.cc.count and e.seen.get(self.cc, 0) < self.cc.count:
            e.obj.wait_ge(self.cc.sem, self.cc.count)
        self.es.close()

    def alt(self, engs=("dve", "act")):
        self.rr += 1
        return engs[self.rr % len(engs)]


def _rope_consts():
    inv = np.zeros((128, 3), np.float32)
    perm = np.zeros((3, 128, 128), np.float32)
    f = THETA ** (-np.arange(0, 64, 2, dtype=np.float32) / 64.0)
    inv[0:32, 0] = f; inv[32:64, 0] = f
    for i in range(32):
        perm[0, 32 + i, i] = -1.0; perm[0, i, 32 + i] = 1.0
    f = THETA ** (-np.arange(0, 32, 2, dtype=np.float32) / 32.0)
    inv[0:16, 1] = f; inv[16:32, 1] = f
    for i in range(16):
        perm[1, 16 + i, i] = -1.0; perm[1, i, 16 + i] = 1.0
    f = THETA ** (-np.arange(0, 16, 2, dtype=np.float32) / 16.0)
    for base in (0, 64):
        inv[base:base + 8, 2] = f; inv[base + 8:base + 16, 2] = f
        for i in range(8):
            perm[2, base + 8 + i, base + i] = -1.0; perm[2, base + i, base + 8 + i] = 1.0
    inv = (inv.astype(np.float64) / (2 * np.pi)).astype(np.float32)
    return inv, perm


KA_OFF = 0; KB_OFF = 1024; VA_OFF = 2048; VB_OFF = 3072; KPE_OFF = 4096; KI_OFF = 4160; KVROWS = 4224
CH = 512; NCH = 9
NIT = 18
TOPK = 256
GROUPS = [[0, 1, 2, 3], [4, 5, 6, 7]]


def emit_A(c, io, l, hin):
    nc = c.nc
    win_d = io["w_in"]; wqb_d = io["w_q_b"]; wkvb_d = io["w_kv_b"]
    kv = io["kvpack"]
    hTb = c.sb([128, 16, TT], BF16, "hTb")
    for i in range(4):
        c.dma("pool", hTb[:, 4 * i:4 * i + 4, :], hin.ap[512 * i:512 * (i + 1), :].rearrange("(kc p) t -> p kc t", p=128),
              reads=[hin], writes=[hTb])
    inv = c.sb([128, 3], F32, "inv"); c.dma("sp", inv[:], io["inv"][:], reads=[io["inv"]], writes=[inv])
    perm = c.sb([128, 3, 128], F32, "perm")
    c.dma("sp", perm[:], io["perm"].ap.rearrange("r k m -> k r m"), reads=[io["perm"]], writes=[perm])
    gq = c.sb([128, 4], F32, "gq"); gkv = c.sb([128, 4], F32, "gkv")
    with nc.allow_non_contiguous_dma(reason="tiny gain vectors"):
        c.dma("sp", gq[:], io["g_q"].ap[l].rearrange("(c p) -> p c", p=128), reads=[io["g_q"]], writes=[gq])
        c.dma("sp", gkv[:], io["g_kv"].ap[l].rearrange("(c p) -> p c", p=128), reads=[io["g_kv"]], writes=[gkv])
    ones = c.sb([128, 128], BF16, "ones")
    c.op("pool", lambda e: e.memset(ones[:], 1.0), writes=[ones])

    posi = c.sb([128, TT], I32, "posi")
    c.dma("sp", posi[:], io["posq"].ap[0, :].partition_broadcast(128), reads=[io["posq"]], writes=[posi])
    posf = c.sb([128, TT], F32, "posf")
    c.op("dve", lambda e: e.tensor_copy(out=posf[:], in_=posi[:]), reads=[posi], writes=[posf])
    CC = c.sb([128, 3, TT], F32, "CC"); SS = c.sb([128, 3, TT], F32, "SS")
    u = c.sb([128, TT], F32, "u"); uf = c.sb([128, TT], F32, "uf")
    for r in range(3):
        for which, dst in ((0, SS), (1, CC)):
            c.op("dve", lambda e: e.tensor_scalar(out=u[:], in0=posf[:], scalar1=inv[:, r:r + 1], scalar2=0.25 * which,
                                                  op0=ALU.mult, op1=ALU.add), reads=[posf, inv], writes=[u])
            c.op("dve", lambda e: e.tensor_copy(out=posi[:], in_=u[:]), reads=[u], writes=[posi])
            c.op("dve", lambda e: e.tensor_copy(out=uf[:], in_=posi[:]), reads=[posi], writes=[uf])
            c.op("dve", lambda e: e.tensor_tensor(out=u[:], in0=u[:], in1=uf[:], op=ALU.subtract), reads=[u, uf], writes=[u])
            c.op("dve", lambda e: e.scalar_tensor_tensor(out=uf[:], in0=u[:], scalar=0.5, in1=u[:], op0=ALU.is_gt, op1=ALU.subtract),
                 reads=[u], writes=[uf])
            c.op("act", lambda e: e.activation(out=dst[:, r, :], in_=uf[:], func=AF.Sin, scale=-6.283185), reads=[uf], writes=[dst])

    wbufs = [c.sb([128, 16, 512], BF16, f"wbuf{i}") for i in range(2)]
    accs = [c.ps([128, TT], F32, f"acc{i}") for i in range(2)]
    rot_ps = c.ps([128, TT], F32, "rot_ps")
    rep_ps = c.ps([128, TT], F32, "rep_ps")
    stg = [c.sb([128, TT], BF16, f"stg{i}") for i in range(3)]
    pf = [c.sb([128, TT], F32, f"pf{i}") for i in range(2)]
    t1 = [c.sb([128, TT], F32, f"t1_{i}") for i in range(2)]
    st = {"w": 0, "a": 0, "s": 0, "p": 0}

    def load_w(w_ap, K, col0, ncols):
        KC = K // 128
        wb = wbufs[st["w"] % 2]; st["w"] += 1
        c.dma("pool", wb[:, 0:KC, 0:ncols], w_ap[:, col0:col0 + ncols].rearrange("(kc p) m -> p kc m", p=128),
              reads=[], writes=[wb])
        return wb

    def fm_chunk(wb, KC, off, M, rhs, epi):
        acc = accs[st["a"] % 2]; st["a"] += 1
        for th in range(2):
            for kc in range(KC):
                c.op("pe", lambda e: e.matmul(acc[0:M, th * 512:(th + 1) * 512], lhsT=wb[:, kc, off:off + M],
                                              rhs=rhs[:, kc, th * 512:(th + 1) * 512], start=(kc == 0), stop=(kc == KC - 1)),
                     reads=[wb, rhs], writes=[acc])
        epi(acc, M)

    def epi_plain(dst_ap, dst_t, rep=None):
        def f(acc, M):
            s = stg[st["s"] % 3]; st["s"] += 1
            if rep is not None:
                c.op("dve", lambda e: e.tensor_tensor(out=s[0:M, :], in0=acc[0:M, :], in1=rep[0:M, :], op=ALU.mult),
                     reads=[acc, rep], writes=[s])
            else:
                en = c.alt()
                if en == "act":
                    c.op("act", lambda e: e.activation(out=s[0:M, :], in_=acc[0:M, :], func=AF.Copy), reads=[acc], writes=[s])
                else:
                    c.op("dve", lambda e: e.tensor_copy(out=s[0:M, :], in_=acc[0:M, :]), reads=[acc], writes=[s])
            c.dma("sp", dst_ap, s[0:M, :], reads=[s], writes=[dst_t])
        return f

    def epi_rope(r, dst_ap, dst_t, rep=None):
        def f(acc, M):
            p = pf[st["p"] % 2]; q = t1[st["p"] % 2]; st["p"] += 1
            if rep is not None:
                c.op("dve", lambda e: e.tensor_tensor(out=p[0:M, :], in0=acc[0:M, :], in1=rep[0:M, :], op=ALU.mult),
                     reads=[acc, rep], writes=[p])
            else:
                c.op("act", lambda e: e.activation(out=p[0:M, :], in_=acc[0:M, :], func=AF.Copy), reads=[acc], writes=[p])
            for th in range(2):
                c.op("pe", lambda e: e.matmul(rot_ps[0:M, th * 512:(th + 1) * 512], lhsT=perm[0:M, r, 0:M],
                                              rhs=p[0:M, th * 512:(th + 1) * 512], start=True, stop=True),
                     reads=[perm, p], writes=[rot_ps])
            c.op("dve", lambda e: e.tensor_tensor(out=q[0:M, :], in0=rot_ps[0:M, :], in1=SS[0:M, r, :], op=ALU.mult),
                 reads=[rot_ps, SS], writes=[q])
            c.op("pool", lambda e: e.tensor_tensor(out=p[0:M, :], in0=p[0:M, :], in1=CC[0:M, r, :], op=ALU.mult),
                 reads=[p, CC], writes=[p])
            s = stg[st["s"] % 3]; st["s"] += 1
            c.op("dve", lambda e: e.tensor_tensor(out=s[0:M, :], in0=p[0:M, :], in1=q[0:M, :], op=ALU.add),
                 reads=[p, q], writes=[s])
            c.dma("sp", dst_ap, s[0:M, :], reads=[s], writes=[dst_t])
        return f

    cn = {"q": c.sb([128, 4, TT], BF16, "cqn"), "kv": c.sb([128, 4, TT], BF16, "ckvn")}
    sq = c.sb([128, 4, TT], BF16, "sq")
    rstd = {"q": c.sb([128, TT], F32, "rstd_q"), "kv": c.sb([128, TT], F32, "rstd_kv")}
    rstd_tm = c.sb([128, 8], F32, "rstd_tm")
    eps_t = c.sb([128, 1], F32, "eps")
    c.op("pool", lambda e: e.memset(eps_t[:], 1e-6), writes=[eps_t])

    def epi_rms(which, i, g):
        def f(acc, M):
            c.op("act", lambda e: e.activation(out=sq[:, i, :], in_=acc[:, :], func=AF.Square), reads=[acc], writes=[sq])
            c.op("dve", lambda e: e.tensor_scalar(out=cn[which][:, i, :], in0=acc[:, :], scalar1=g[:, i:i + 1], scalar2=None, op0=ALU.mult),
                 reads=[acc, g], writes=[cn[which]])
        return f

    win = win_d.ap[l]
    for which, col0, g in (("q", 0, gq), ("kv", 512, gkv)):
        wb = load_w(win, D, col0, 512)
        for i in range(4):
            fm_chunk(wb, 16, 128 * i, 128, hTb, epi_rms(which, i, g))
        for th in range(2):
            for i in range(4):
                c.op("pe", lambda e: e.matmul(rep_ps[:, th * 512:(th + 1) * 512], lhsT=ones[:, :], rhs=sq[:, i, th * 512:(th + 1) * 512],
                                              start=(i == 0), stop=(i == 3)), reads=[ones, sq], writes=[rep_ps])
        c.op("act", lambda e: e.activation(out=rstd[which][:], in_=rep_ps[:], func=AF.Sqrt, scale=1.0 / 512.0, bias=eps_t[:, 0:1]),
             reads=[rep_ps, eps_t], writes=[rstd[which]])
        c.op("dve", lambda e: e.reciprocal(out=rstd[which][:], in_=rstd[which][:]), reads=[rstd[which]], writes=[rstd[which]])
        if which == "kv":
            for tt in range(8):
                for i in range(4):
                    c.op("pe", lambda e: e.matmul(rep_ps[:, tt:tt + 1], lhsT=sq[:, i, tt * 128:(tt + 1) * 128], rhs=ones[:, 0:1],
                                                  start=(i == 0), stop=(i == 3)), reads=[sq, ones], writes=[rep_ps])
            c.op("act", lambda e: e.activation(out=rstd_tm[:], in_=rep_ps[:, 0:8], func=AF.Sqrt, scale=1.0 / 512.0, bias=eps_t[:, 0:1]),
                 reads=[rep_ps, eps_t], writes=[rstd_tm])
            c.op("dve", lambda e: e.reciprocal(out=rstd_tm[:], in_=rstd_tm[:]), reads=[rstd_tm], writes=[rstd_tm])

    wb = load_w(win, D, C_KPE, 64)
    fm_chunk(wb, 16, 0, 64, hTb, epi_rope(0, kv[KPE_OFF:KPE_OFF + 64, :], kv))
    for g in range(2):
        wb = load_w(win, D, C_QB + 512 * g, 512)
        for j in range(4):
            fm_chunk(wb, 16, 128 * j, 128, hTb, epi_rope(1, io["qbT"][4 * g + j, :, :], io["qbT"]))
    for g in range(2):
        wb = load_w(win, D, C_KB + 512 * g, 512)
        for j in range(4):
            h = 4 * g + j
            fm_chunk(wb, 16, 128 * j, 128, hTb, epi_rope(1, kv[KB_OFF + 128 * h:KB_OFF + 128 * (h + 1), :], kv))
    for g in range(2):
        wb = load_w(win, D, C_VB + 512 * g, 512)
        for tt in range(8):
            acc = accs[st["a"] % 2]; st["a"] += 1
            for kc in range(16):
                c.op("pe", lambda e: e.matmul(acc[:, 0:512], lhsT=hTb[:, kc, tt * 128:(tt + 1) * 128], rhs=wb[:, kc, 0:512],
                                              start=(kc == 0), stop=(kc == 15)), reads=[hTb, wb], writes=[acc])
            s = stg[st["s"] % 3]; st["s"] += 1
            en = c.alt()
            if en == "act":
                c.op("act", lambda e: e.activation(out=s[:, 0:512], in_=acc[:, 0:512], func=AF.Copy), reads=[acc], writes=[s])
            else:
                c.op("dve", lambda e: e.tensor_copy(out=s[:, 0:512], in_=acc[:, 0:512]), reads=[acc], writes=[s])
            c.dma("sp", kv[VB_OFF + tt * 128:VB_OFF + (tt + 1) * 128, 512 * g:512 * (g + 1)], s[:, 0:512], reads=[s], writes=[kv])
    for g in range(2):
        wb = load_w(win, D, C_QI + 512 * g, 512)
        for j in range(4):
            fm_chunk(wb, 16, 128 * j, 128, hTb, epi_rope(2, io["qiT"][4 * g + j, :, :], io["qiT"]))
    wb = load_w(win, D, C_KI, 80)
    fm_chunk(wb, 16, 0, 64, hTb, epi_rope(2, kv[KI_OFF:KI_OFF + 64, :], kv))
    wst = c.sb([128, 8, 16], F32, "wst")
    for tt in range(8):
        acc = accs[st["a"] % 2]; st["a"] += 1
        for kc in range(16):
            c.op("pe", lambda e: e.matmul(acc[:, 0:16], lhsT=hTb[:, kc, tt * 128:(tt + 1) * 128], rhs=wb[:, kc, 64:80],
                                          start=(kc == 0), stop=(kc == 15)), reads=[hTb, wb], writes=[acc])
        c.op("act", lambda e: e.activation(out=wst[:, tt, :], in_=acc[:, 0:16], func=AF.Copy, scale=1.0 / 32.0), reads=[acc], writes=[wst])
    c.dma("sp", io["widx"].ap.rearrange("(tt p) h -> p tt h", p=128), wst[:], reads=[wst], writes=[io["widx"]])
    wqb = wqb_d.ap[l]
    for g in range(4):
        wb = load_w(wqb, 512, 384 * g, 384)
        for j in range(2):
            h = 2 * g + j
            fm_chunk(wb, 4, 192 * j, 128, cn["q"], epi_plain(io["qaT"][h, 0:128, :], io["qaT"], rep=rstd["q"]))
            fm_chunk(wb, 4, 192 * j + 128, 64, cn["q"], epi_rope(0, io["qaT"][h, 128:192, :], io["qaT"], rep=rstd["q"]))
    wkvb = wkvb_d.ap[l]
    for g in range(4):
        wb = load_w(wkvb, 512, 512 * g, 512)
        for j in range(2):
            h = 2 * g + j
            fm_chunk(wb, 4, 256 * j, 128, cn["kv"], epi_plain(kv[KA_OFF + 128 * h:KA_OFF + 128 * (h + 1), :], kv, rep=rstd["kv"]))
        for tt in range(8):
            acc = accs[st["a"] % 2]; st["a"] += 1
            for j in range(2):
                for kc in range(4):
                    c.op("pe", lambda e: e.matmul(acc[:, 128 * j:128 * (j + 1)], lhsT=cn["kv"][:, kc, tt * 128:(tt + 1) * 128],
                                                  rhs=wb[:, kc, 256 * j + 128:256 * j + 256], start=(kc == 0), stop=(kc == 3)),
                         reads=[cn["kv"], wb], writes=[acc])
            s = stg[st["s"] % 3]; st["s"] += 1
            c.op("dve", lambda e: e.tensor_scalar(out=s[:, 0:256], in0=acc[:, 0:256], scalar1=rstd_tm[:, tt:tt + 1], scalar2=None, op0=ALU.mult),
                 reads=[acc, rstd_tm], writes=[s])
            c.dma("sp", kv[VA_OFF + tt * 128:VA_OFF + (tt + 1) * 128, 256 * g:256 * (g + 1)], s[:, 0:256], reads=[s], writes=[kv])


def emit_B1(c, io):
    nc = c.nc
    NK = SEQ; NKC = NK // 128; LA = 2
    gath = io["gath"]

    def gk(row0, nrows):
        k = row0 // CH; off = row0 % CH
        rows = min(CH, KVROWS - k * CH)
        return io["gath%d" % k].ap.rearrange("(r f) t -> f r t", r=4)[off:off + nrows, :, :], io["gath%d" % k]

    sc = c.sb([128, NK], F32, "sc")
    tmpi = sc[:, :].bitcast(I32)
    posq_bc = c.sb([128, TT], F32, "posq_bc")
    c.dma("sp", tmpi[:, 0:TT], io["posq"].ap[0, :].partition_broadcast(128), reads=[io["posq"]], writes=[sc])
    c.op("dve", lambda e: e.tensor_copy(out=posq_bc[:], in_=tmpi[:, 0:TT]), reads=[sc], writes=[posq_bc])
    posk_bc = c.sb([128, NK], F32, "posk_bc")
    c.dma("sp", tmpi[:, :], io["posk"].ap[0, :].partition_broadcast(128), reads=[io["posk"]], writes=[sc])
    c.op("dve", lambda e: e.tensor_copy(out=posk_bc[:], in_=tmpi[:, :]), reads=[sc], writes=[posk_bc])
    pci = c.sb([128, 40], I32, "pci"); posk_col = c.sb([128, NKC], F32, "posk_col"); posq_col = c.sb([128, 8], F32, "posq_col")
    with nc.allow_non_contiguous_dma(reason="tiny position columns"):
        c.dma("sp", pci[:, 0:NKC], io["posk"].ap[0, :].rearrange("(kc p) -> p kc", p=128), reads=[io["posk"]], writes=[pci])
        c.dma("sp", pci[:, 32:40], io["posq"].ap[0, :].rearrange("(kc p) -> p kc", p=128), reads=[io["posq"]], writes=[pci])
    c.op("dve", lambda e: e.tensor_copy(out=posk_col[:], in_=pci[:, 0:NKC]), reads=[pci], writes=[posk_col])
    c.op("dve", lambda e: e.tensor_copy(out=posq_col[:], in_=pci[:, 32:40]), reads=[pci], writes=[posq_col])
    ones = c.sb([128, 128], BF16, "ones")
    c.op("pool", lambda e: e.memset(ones[:], 1.0), writes=[ones])
    ident = c.sb([128, 128], BF16, "ident")
    c.dma("sp", ident[:], io["identb"][:], reads=[io["identb"]], writes=[ident])

    KT = [c.sb([128, NK], BF16, f"KT{i}") for i in range(2)]
    VV = [c.sb([128, NKC, 128], BF16, f"VV{i}") for i in range(2)]
    QT = [c.sb([128, TT], BF16, f"QT{i}") for i in range(2)]
    QP = [c.sb([64, TT], BF16, f"QP{i}") for i in range(2)]
    kpe = c.sb([64, NK], BF16, "kpe")
    pts = [c.sb([128, 512], BF16, f"pt{i}") for i in range(4)]
    S_ps = [c.ps([128, 512], F32, f"S{i}") for i in range(3)]
    acc = c.ps([128, 512], F32, "acc")
    den = c.ps([128, 512], F32, "den")
    rden = c.sb([128, 512], F32, "rden")
    ost = [c.sb([128, 512], BF16, f"ost{i}") for i in range(2)]
    maskB = c.sb([128, NKC, TT], BF16, "maskB")
    st = {"s": 0, "p": 0, "o": 0, "b": 0}

    def attention_head(h, mla):
        b = st["b"] % 2; st["b"] += 1
        kt, vv, qt_, qp = KT[b], VV[b], QT[b], QP[b]
        koff, voff = (KA_OFF, VA_OFF) if mla else (KB_OFF, VB_OFF)
        ap_, gt = gk(koff + 128 * h, 128)
        c.dma("sp", kt[:, :].rearrange("p (r t) -> p r t", r=4), ap_, reads=[gt], writes=[kt])
        for r in range(4):
            for cc_ in range(2):
                gt = io["gath%d" % (voff // CH + cc_)]
                c.dma("sp", vv[:, 8 * r + 4 * cc_:8 * r + 4 * cc_ + 4, :],
                      gt.ap[r * CH:(r + 1) * CH, 128 * h:128 * (h + 1)].rearrange("(kc p) d -> p kc d", p=128), reads=[gt], writes=[vv])
        if mla:
            c.dma("sp", qt_[:], io["qaT"][h, 0:128, :], reads=[io["qaT"]], writes=[qt_])
            c.dma("sp", qp[:], io["qaT"][h, 128:192, :], reads=[io["qaT"]], writes=[qp])
            scale = 192.0 ** -0.5
        else:
            c.dma("sp", qt_[:], io["qbT"][h, :, :], reads=[io["qbT"]], writes=[qt_])
            scale = 128.0 ** -0.5
        for qh in range(2):
            qs = slice(qh * 512, (qh + 1) * 512)
            ptq = {}

            def front(kc):
                ks = slice(kc * 128, (kc + 1) * 128)
                S = S_ps[st["s"] % 3]; st["s"] += 1
                pt = pts[st["p"] % 4]; st["p"] += 1
                ptq[kc] = pt
                c.op("pe", lambda e: e.matmul(S[:], lhsT=kt[:, ks], rhs=qt_[:, qs], start=True, stop=not mla), reads=[kt, qt_], writes=[S])
                if mla:
                    c.op("pe", lambda e: e.matmul(S[:], lhsT=kpe[0:64, ks], rhs=qp[0:64, qs], start=False, stop=True), reads=[kpe, qp], writes=[S])
                c.op("act", lambda e: e.activation(out=pt[:], in_=S[:], func=AF.Exp, scale=scale), reads=[S], writes=[pt])
                if mla:
                    c.op("dve", lambda e: e.scalar_tensor_tensor(out=pt[:], in0=posq_bc[:, qs], scalar=posk_col[:, kc:kc + 1], in1=pt[:],
                                                                 op0=ALU.is_ge, op1=ALU.mult), reads=[posq_bc, posk_col, pt], writes=[pt])
                else:
                    c.op("pool", lambda e: e.tensor_tensor(out=pt[:], in0=pt[:], in1=maskB[:, kc, qs], op=ALU.mult), reads=[pt, maskB], writes=[pt])

            def back(kc):
                pt = ptq.pop(kc)
                c.op("pe", lambda e: e.matmul(acc[:], lhsT=vv[:, kc, :], rhs=pt[:], start=(kc == 0), stop=(kc == NKC - 1)), reads=[vv, pt], writes=[acc])
                c.op("pe", lambda e: e.matmul(den[:], lhsT=ones[:], rhs=pt[:], start=(kc == 0), stop=(kc == NKC - 1)), reads=[ones, pt], writes=[den])

            for step in range(NKC + LA):
                if step < NKC:
                    front(step)
                if step >= LA:
                    back(step - LA)
            c.op("dve", lambda e: e.reciprocal(out=rden[:], in_=den[:]), reads=[den], writes=[rden])
            o = ost[st["o"] % 2]; st["o"] += 1
            c.op("dve", lambda e: e.tensor_tensor(out=o[:], in0=acc[:], in1=rden[:], op=ALU.mult), reads=[acc, rden], writes=[o])
            dst = io["oaT"] if mla else io["obT"]
            c.dma("sp", dst[128 * h:128 * (h + 1), qs], o[:], reads=[o], writes=[dst])

    ki2 = c.sb([128, NK], BF16, "ki2")
    ap_, gt = gk(KI_OFF, 64)
    c.dma("sp", ki2[0:64, :].rearrange("p (r t) -> p r t", r=4), ap_, reads=[gt], writes=[ki2])
    c.dma("sp", ki2[64:128, :].rearrange("p (r t) -> p r t", r=4), ap_, reads=[gt], writes=[ki2])
    qi = c.sb([128, 8, TT], BF16, "qi")
    c.dma("sp", qi[:], io["qiT"].ap.rearrange("g p t -> p g t"), reads=[io["qiT"]], writes=[qi])
    wcol = c.sb([128, 8, 16], F32, "wcol")
    c.dma("sp", wcol[:], io["widx"].ap.rearrange("(qb p) h -> p qb h", p=128), reads=[io["widx"]], writes=[wcol])
    junk = c.sb([128, NK], BF16, "junk")
    rb = [c.sb([128, 512], F32, f"rb{i}") for i in range(3)]
    I_ps = [c.ps([128, 512], F32, "I0"), c.ps([128, 512], F32, "I1")]
    tp = c.ps([128, 512], BF16, "tp")
    sm = {k: c.sb([128, 1], F32, "sm_" + k) for k in ("lo", "hi", "mid", "cnt", "ge", "d1", "d2", "am")}
    ri = {"r": 0, "i": 0}

    def indexer(qb):
        qs = slice(qb * 128, (qb + 1) * 128)
        for kt_ in range(NK // 512):
            ksl = slice(kt_ * 512, (kt_ + 1) * 512)
            for h in range(16):
                pb = 64 * (h % 2)
                ip = I_ps[ri["i"] % 2]; ri["i"] += 1
                r = rb[ri["r"] % 3]; ri["r"] += 1
                c.op("pe", lambda e: e.matmul(ip[:], lhsT=qi[pb:pb + 64, h // 2, qs], rhs=ki2[pb:pb + 64, ksl], start=True, stop=True),
                     reads=[qi, ki2], writes=[ip])
                c.op("act", lambda e: e.activation(out=r[:], in_=ip[:], func=AF.Relu), reads=[ip], writes=[r])
                if h == 0:
                    c.op("dve", lambda e: e.tensor_scalar(out=sc[:, ksl], in0=r[:], scalar1=wcol[:, qb, 0:1], scalar2=None, op0=ALU.mult),
                         reads=[r, wcol], writes=[sc])
                else:
                    c.op("dve", lambda e: e.scalar_tensor_tensor(out=sc[:, ksl], in0=r[:], scalar=wcol[:, qb, h:h + 1], in1=sc[:, ksl],
                                                                 op0=ALU.mult, op1=ALU.add), reads=[r, wcol, sc], writes=[sc])
        c.op("dve", lambda e: e.tensor_reduce(out=sm["am"][:], in_=sc[:], axis=AX.X, op=ALU.max, apply_absolute_value=True),
             reads=[sc], writes=[sm["am"]])
        c.op("dve", lambda e: e.tensor_scalar(out=sm["hi"][:], in0=sm["am"][:], scalar1=1.001, scalar2=1e-6, op0=ALU.mult, op1=ALU.add),
             reads=[sm["am"]], writes=[sm["hi"]])
        c.op("dve", lambda e: e.tensor_scalar(out=sm["lo"][:], in0=sm["hi"][:], scalar1=-1.0, scalar2=None, op0=ALU.mult),
             reads=[sm["hi"]], writes=[sm["lo"]])
        c.op("pool", lambda e: e.tensor_scalar(out=junk[:], in0=posk_bc[:], scalar1=posq_col[:, qb:qb + 1], scalar2=-1e30,
                                               op0=ALU.is_gt, op1=ALU.mult), reads=[posk_bc, posq_col], writes=[junk])
        c.op("pool", lambda e: e.tensor_tensor(out=sc[:], in0=sc[:], in1=junk[:], op=ALU.add), reads=[sc, junk], writes=[sc])
        for it in range(NIT):
            c.op("dve", lambda e: e.tensor_tensor(out=sm["mid"][:], in0=sm["lo"][:], in1=sm["hi"][:], op=ALU.add),
                 reads=[sm["lo"], sm["hi"]], writes=[sm["mid"]])
            c.op("dve", lambda e: e.tensor_scalar(out=sm["mid"][:], in0=sm["mid"][:], scalar1=0.5, scalar2=None, op0=ALU.mult),
                 reads=[sm["mid"]], writes=[sm["mid"]])
            c.op("dve", lambda e: e.tensor_scalar(out=junk[:], in0=sc[:], scalar1=sm["mid"][:, 0:1], scalar2=0.0, op0=ALU.is_ge, op1=ALU.add,
                                                  accum_out=sm["cnt"][:]), reads=[sc, sm["mid"]], writes=[junk, sm["cnt"]])
            c.op("dve", lambda e: e.tensor_scalar(out=sm["ge"][:], in0=sm["cnt"][:], scalar1=TOPK - 0.5, scalar2=None, op0=ALU.is_ge),
                 reads=[sm["cnt"]], writes=[sm["ge"]])
            c.op("dve", lambda e: e.tensor_tensor(out=sm["d1"][:], in0=sm["mid"][:], in1=sm["lo"][:], op=ALU.subtract),
                 reads=[sm["mid"], sm["lo"]], writes=[sm["d1"]])
            c.op("dve", lambda e: e.tensor_tensor(out=sm["d2"][:], in0=sm["hi"][:], in1=sm["mid"][:], op=ALU.subtract),
                 reads=[sm["hi"], sm["mid"]], writes=[sm["d2"]])
            c.op("dve", lambda e: e.scalar_tensor_tensor(out=sm["lo"][:], in0=sm["d1"][:], scalar=sm["ge"][:, 0:1], in1=sm["lo"][:],
                                                         op0=ALU.mult, op1=ALU.add), reads=[sm["d1"], sm["ge"], sm["lo"]], writes=[sm["lo"]])
            c.op("dve", lambda e: e.scalar_tensor_tensor(out=sm["hi"][:], in0=sm["d2"][:], scalar=sm["ge"][:, 0:1], in1=sm["mid"][:],
                                                         op0=ALU.mult, op1=ALU.add), reads=[sm["d2"], sm["ge"], sm["mid"]], writes=[sm["hi"]])
        c.op("dve", lambda e: e.tensor_scalar(out=junk[:], in0=sc[:], scalar1=sm["lo"][:, 0:1], scalar2=None, op0=ALU.is_ge),
             reads=[sc, sm["lo"]], writes=[junk])
        for g in range(NKC // 4):
            for j in range(4):
                kc = 4 * g + j
                c.op("pe", lambda e: e.transpose(out=tp[:, j * 128:(j + 1) * 128], in_=junk[:, kc * 128:(kc + 1) * 128], identity=ident[:]),
                     reads=[junk, ident], writes=[tp])
            c.op("act", lambda e: e.activation(out=maskB[:, 4 * g:4 * g + 4, qs], in_=tp[:, :].rearrange("p (j q) -> p j q", j=4), func=AF.Copy),
                 reads=[tp], writes=[maskB])

    ap_, gt = gk(KPE_OFF, 64)
    c.dma("sp", kpe[:, :].rearrange("p (r t) -> p r t", r=4), ap_, reads=[gt], writes=[kpe])
    for i in range(8):
        indexer(i)
        attention_head(i, True)
    for h in range(8):
        attention_head(h, False)


def emit_ln(c, r, g_ap, b_ap, out_t, ps, onesf):
    nc = c.nc
    gcol = c.sb([128, 16], F32, "ln_g"); bcol = c.sb([128, 16], F32, "ln_b")
    with nc.allow_non_contiguous_dma(reason="tiny ln vectors"):
        c.dma("sp", gcol[:], g_ap.rearrange("(c p) -> p c", p=128), reads=[], writes=[gcol])
        c.dma("sp", bcol[:], b_ap.rearrange("(c p) -> p c", p=128), reads=[], writes=[bcol])
    tmp = c.sb([128, TT], F32, "ln_tmp")
    mean = c.sb([128, TT], F32, "ln_mean"); rstd = c.sb([128, TT], F32, "ln_rstd")
    for th in range(2):
        ts_ = slice(th * 512, (th + 1) * 512)
        for m in range(16):
            c.op("pe", lambda e: e.matmul(ps[th][:], lhsT=onesf[:, :], rhs=r[:, m, ts_], start=(m == 0), stop=(m == 15)),
                 reads=[onesf, r], writes=[ps[th]])
    for m in range(16):
        c.op("act", lambda e: e.activation(out=tmp[:, :], in_=r[:, m, :], func=AF.Square), reads=[r], writes=[tmp])
        for th in range(2):
            ts_ = slice(th * 512, (th + 1) * 512)
            c.op("pe", lambda e: e.matmul(ps[2 + th][:], lhsT=onesf[:, :], rhs=tmp[:, ts_], start=(m == 0), stop=(m == 15)),
                 reads=[onesf, tmp], writes=[ps[2 + th]])
    for th in range(2):
        ts_ = slice(th * 512, (th + 1) * 512)
        c.op("act", lambda e: e.activation(out=mean[:, ts_], in_=ps[th][:], func=AF.Copy, scale=1.0 / D), reads=[ps[th]], writes=[mean])
        c.op("dve", lambda e: e.tensor_tensor(out=tmp[:, ts_], in0=mean[:, ts_], in1=mean[:, ts_], op=ALU.mult), reads=[mean], writes=[tmp])
        c.op("dve", lambda e: e.scalar_tensor_tensor(out=rstd[:, ts_], in0=ps[2 + th][:], scalar=1.0 / D, in1=tmp[:, ts_], op0=ALU.mult, op1=ALU.subtract),
             reads=[ps[2 + th], tmp], writes=[rstd])
    c.op("dve", lambda e: e.tensor_scalar(out=rstd[:], in0=rstd[:], scalar1=1e-5, scalar2=None, op0=ALU.add), reads=[rstd], writes=[rstd])
    c.op("act", lambda e: e.activation(out=rstd[:], in_=rstd[:], func=AF.Sqrt), reads=[rstd], writes=[rstd])
    c.op("dve", lambda e: e.reciprocal(out=rstd[:], in_=rstd[:]), reads=[rstd], writes=[rstd])
    for m in range(16):
        c.op("dve", lambda e: e.tensor_tensor(out=r[:, m, :], in0=r[:, m, :], in1=mean[:], op=ALU.subtract), reads=[r, mean], writes=[r])
        c.op("pool", lambda e: e.tensor_tensor(out=r[:, m, :], in0=r[:, m, :], in1=rstd[:], op=ALU.mult), reads=[r, rstd], writes=[r])
        c.op("dve", lambda e: e.tensor_scalar(out=r[:, m, :], in0=r[:, m, :], scalar1=gcol[:, m:m + 1], scalar2=bcol[:, m:m + 1],
                                              op0=ALU.mult, op1=ALU.add), reads=[r, gcol, bcol], writes=[r])
    for i in range(4):
        c.dma("sp", out_t.ap[512 * i:512 * (i + 1), :].rearrange("(kc p) t -> p kc t", p=128), r[:, 4 * i:4 * i + 4, :], reads=[r], writes=[out_t])


def emit_B2(c, io, l, hin, hout):
    nc = c.nc
    win = io["w_in"].ap[l]; woa = io["w_o_a"].ap[l]; wob = io["w_o_b"].ap[l]; wout = io["w_out"].ap[l]
    hTb = c.sb([128, 16, TT], BF16, "hTb"); r = c.sb([128, 16, TT], F32, "r")
    for i in range(4):
        c.dma("pool", hTb[:, 4 * i:4 * i + 4, :], hin.ap[512 * i:512 * (i + 1), :].rearrange("(kc p) t -> p kc t", p=128), reads=[hin], writes=[hTb])
        c.dma("sp", r[:, 4 * i:4 * i + 4, :], hin.ap[512 * i:512 * (i + 1), :].rearrange("(kc p) t -> p kc t", p=128), reads=[hin], writes=[r])
    oa = c.sb([128, 8, TT], BF16, "oa"); ob = c.sb([128, 8, TT], BF16, "ob")
    c.dma("sp", oa[:], io["oaT"].ap.rearrange("(kc p) t -> p kc t", p=128), reads=[io["oaT"]], writes=[oa])
    c.dma("sp", ob[:], io["obT"].ap.rearrange("(kc p) t -> p kc t", p=128), reads=[io["obT"]], writes=[ob])
    yT = c.sb([128, 16, TT], BF16, "yT")
    onesf = c.sb([128, 128], F32, "onesf")
    c.op("pool", lambda e: e.memset(onesf[:], 1.0), writes=[onesf])
    wg = [c.sb([128, 16, 256], BF16, f"wg{i}") for i in range(2)]
    wo = [c.sb([128, 8, 256], BF16, f"wo{i}") for i in range(2)]
    ps = [c.ps([128, 512], F32, f"ps{i}") for i in range(8)]
    sg = [c.sb([128, 512], F32, f"sg{i}") for i in range(2)]
    ta = c.sb([128, 512], F32, "ta"); tb = c.sb([128, 512], F32, "tb")
    k = 0
    for grp in range(8):
        c.dma("pool", wg[0][:], win[:, C_GA + 256 * grp:C_GA + 256 * (grp + 1)].rearrange("(kc p) m -> p kc m", p=128), reads=[], writes=[wg[0]])
        c.dma("pool", wg[1][:], win[:, C_GB + 256 * grp:C_GB + 256 * (grp + 1)].rearrange("(kc p) m -> p kc m", p=128), reads=[], writes=[wg[1]])
        c.dma("pool", wo[0][:], woa[:, 256 * grp:256 * (grp + 1)].rearrange("(kc p) m -> p kc m", p=128), reads=[], writes=[wo[0]])
        c.dma("pool", wo[1][:], wob[:, 256 * grp:256 * (grp + 1)].rearrange("(kc p) m -> p kc m", p=128), reads=[], writes=[wo[1]])
        for j in range(2):
            m = 2 * grp + j
            ms = slice(128 * j, 128 * (j + 1))
            for th in range(2):
                ts_ = slice(th * 512, (th + 1) * 512)
                p4 = ps[4 * (k % 2):4 * (k % 2) + 4]; k += 1
                for kc in range(16):
                    c.op("pe", lambda e: e.matmul(p4[0][:], lhsT=wg[0][:, kc, ms], rhs=hTb[:, kc, ts_], start=(kc == 0), stop=(kc == 15)), reads=[wg[0], hTb], writes=[p4[0]])
                for kc in range(16):
                    c.op("pe", lambda e: e.matmul(p4[1][:], lhsT=wg[1][:, kc, ms], rhs=hTb[:, kc, ts_], start=(kc == 0), stop=(kc == 15)), reads=[wg[1], hTb], writes=[p4[1]])
                for kc in range(8):
                    c.op("pe", lambda e: e.matmul(p4[2][:], lhsT=wo[0][:, kc, ms], rhs=oa[:, kc, ts_], start=(kc == 0), stop=(kc == 7)), reads=[wo[0], oa], writes=[p4[2]])
                for kc in range(8):
                    c.op("pe", lambda e: e.matmul(p4[3][:], lhsT=wo[1][:, kc, ms], rhs=ob[:, kc, ts_], start=(kc == 0), stop=(kc == 7)), reads=[wo[1], ob], writes=[p4[3]])
                c.op("act", lambda e: e.activation(out=sg[0][:], in_=p4[0][:], func=AF.Sigmoid), reads=[p4[0]], writes=[sg[0]])
                c.op("act", lambda e: e.activation(out=sg[1][:], in_=p4[1][:], func=AF.Sigmoid), reads=[p4[1]], writes=[sg[1]])
                c.op("dve", lambda e: e.tensor_tensor(out=ta[:], in0=p4[2][:], in1=sg[0][:], op=ALU.mult), reads=[p4[2], sg[0]], writes=[ta])
                c.op("dve", lambda e: e.tensor_tensor(out=tb[:], in0=p4[3][:], in1=sg[1][:], op=ALU.mult), reads=[p4[3], sg[1]], writes=[tb])
                c.op("pool", lambda e: e.tensor_tensor(out=yT[:, m, ts_], in0=ta[:], in1=tb[:], op=ALU.add), reads=[ta, tb], writes=[yT])
    for grp in range(8):
        w = wg[grp % 2]
        c.dma("pool", w[:], wout[:, 256 * grp:256 * (grp + 1)].rearrange("(kc p) m -> p kc m", p=128), reads=[], writes=[w])
        for j in range(2):
            m = 2 * grp + j
            ms = slice(128 * j, 128 * (j + 1))
            for th in range(2):
                ts_ = slice(th * 512, (th + 1) * 512)
                p = ps[k % 8]; k += 1
                for kc in range(16):
                    c.op("pe", lambda e: e.matmul(p[:], lhsT=w[:, kc, ms], rhs=yT[:, kc, ts_], start=(kc == 0), stop=(kc == 15)), reads=[w, yT], writes=[p])
                c.op("dve", lambda e: e.scalar_tensor_tensor(out=r[:, m, ts_], in0=r[:, m, ts_], scalar=ALPHA, in1=p[:], op0=ALU.mult, op1=ALU.add),
                     reads=[r, p], writes=[r])
    emit_ln(c, r, io["ln1_g"].ap[l], io["ln1_b"].ap[l], hout, ps, onesf)


def emit_router(c, io, l, r, g_ps):
    nc = c.nc
    wr = c.sb([128, 16, 72], F32, "wr"); bias = c.sb([128, 72], F32, "bias")
    with nc.allow_non_contiguous_dma(reason="small router weights"):
        c.dma("sp", wr[:, :, 0:8], io["w_group"].ap[l].rearrange("(kc p) m -> p kc m", p=128), reads=[], writes=[wr])
        c.dma("sp", wr[:, :, 8:72], io["w_router"].ap[l].rearrange("(kc p) m -> p kc m", p=128), reads=[], writes=[wr])
    c.dma("sp", bias[:, 0:8], io["b_group"].ap[l].partition_broadcast(128), reads=[], writes=[bias])
    c.dma("sp", bias[:, 8:72], io["b_router"].ap[l].partition_broadcast(128), reads=[], writes=[bias])
    CW = c.sb([128, 8, 64], F32, "CW")
    GM = c.sb([128, 8, 8], F32, "GM")
    lg = c.sb([128, 72], F32, "lg"); em = c.sb([128, 64], F32, "em"); em2 = c.sb([128, 64], F32, "em2")
    mk1 = c.sb([128, 64], F32, "mk1"); mk2 = c.sb([128, 64], F32, "mk2"); gex = c.sb([128, 8], F32, "gex")
    s = {k: c.sb([128, 1], F32, "r_" + k) for k in ("gmax", "ngmax", "gsum", "gp", "m1", "m2", "d", "ex", "w1", "w2")}
    pen = c.sb([128, 8], F32, "pen")
    for tt in range(8):
        tsl = slice(tt * 128, (tt + 1) * 128)
        gmask = GM[:, tt, :]
        for kc in range(16):
            c.op("pe", lambda e: e.matmul(g_ps[:, 0:72], lhsT=r[:, kc, tsl], rhs=wr[:, kc, :], start=(kc == 0), stop=(kc == 15)), reads=[r, wr], writes=[g_ps])
        c.op("dve", lambda e: e.tensor_tensor(out=lg[:], in0=g_ps[:, 0:72], in1=bias[:], op=ALU.add), reads=[g_ps, bias], writes=[lg])
        c.op("dve", lambda e: e.tensor_reduce(out=s["gmax"][:], in_=lg[:, 0:8], axis=AX.X, op=ALU.max), reads=[lg], writes=[s["gmax"]])
        c.op("dve", lambda e: e.tensor_scalar(out=gmask, in0=lg[:, 0:8], scalar1=s["gmax"][:, 0:1], scalar2=None, op0=ALU.is_ge), reads=[lg, s["gmax"]], writes=[GM])
        c.op("dve", lambda e: e.tensor_scalar(out=s["ngmax"][:], in0=s["gmax"][:], scalar1=-1.0, scalar2=None, op0=ALU.mult), reads=[s["gmax"]], writes=[s["ngmax"]])
        c.op("act", lambda e: e.activation(out=gex[:], in_=lg[:, 0:8], func=AF.Exp, bias=s["ngmax"][:, 0:1], accum_out=s["gsum"][:]),
             reads=[lg, s["ngmax"]], writes=[gex, s["gsum"]])
        c.op("dve", lambda e: e.reciprocal(out=s["gp"][:], in_=s["gsum"][:]), reads=[s["gsum"]], writes=[s["gp"]])
        c.op("dve", lambda e: e.tensor_scalar(out=pen[:], in0=gmask, scalar1=-1.0, scalar2=1e30, op0=ALU.add, op1=ALU.mult), reads=[GM], writes=[pen])
        for g in range(8):
            c.op("dve", lambda e: e.tensor_scalar(out=em[:, 8 * g:8 * g + 8], in0=lg[:, 8 + 8 * g:16 + 8 * g], scalar1=pen[:, g:g + 1], scalar2=None, op0=ALU.add),
                 reads=[lg, pen], writes=[em])
        c.op("dve", lambda e: e.tensor_reduce(out=s["m1"][:], in_=em[:], axis=AX.X, op=ALU.max), reads=[em], writes=[s["m1"]])
        c.op("dve", lambda e: e.tensor_scalar(out=mk1[:], in0=em[:], scalar1=s["m1"][:, 0:1], scalar2=None, op0=ALU.is_ge), reads=[em, s["m1"]], writes=[mk1])
        c.op("dve", lambda e: e.scalar_tensor_tensor(out=em2[:], in0=mk1[:], scalar=-1e30, in1=em[:], op0=ALU.mult, op1=ALU.add), reads=[mk1, em], writes=[em2])
        c.op("dve", lambda e: e.tensor_reduce(out=s["m2"][:], in_=em2[:], axis=AX.X, op=ALU.max), reads=[em2], writes=[s["m2"]])
        c.op("dve", lambda e: e.tensor_scalar(out=mk2[:], in0=em2[:], scalar1=s["m2"][:, 0:1], scalar2=None, op0=ALU.is_ge), reads=[em2, s["m2"]], writes=[mk2])
        c.op("dve", lambda e: e.tensor_tensor(out=s["d"][:], in0=s["m2"][:], in1=s["m1"][:], op=ALU.subtract), reads=[s["m1"], s["m2"]], writes=[s["d"]])
        c.op("act", lambda e: e.activation(out=s["ex"][:], in_=s["d"][:], func=AF.Exp), reads=[s["d"]], writes=[s["ex"]])
        c.op("dve", lambda e: e.tensor_scalar(out=s["ex"][:], in0=s["ex"][:], scalar1=1.0, scalar2=None, op0=ALU.add), reads=[s["ex"]], writes=[s["ex"]])
        c.op("dve", lambda e: e.reciprocal(out=s["ex"][:], in_=s["ex"][:]), reads=[s["ex"]], writes=[s["ex"]])
        c.op("dve", lambda e: e.tensor_tensor(out=s["w1"][:], in0=s["gp"][:], in1=s["ex"][:], op=ALU.mult), reads=[s["gp"], s["ex"]], writes=[s["w1"]])
        c.op("dve", lambda e: e.tensor_tensor(out=s["w2"][:], in0=s["gp"][:], in1=s["w1"][:], op=ALU.subtract), reads=[s["gp"], s["w1"]], writes=[s["w2"]])
        c.op("dve", lambda e: e.tensor_scalar(out=mk1[:], in0=mk1[:], scalar1=s["w1"][:, 0:1], scalar2=None, op0=ALU.mult), reads=[mk1, s["w1"]], writes=[mk1])
        c.op("dve", lambda e: e.scalar_tensor_tensor(out=CW[:, tt, :], in0=mk2[:], scalar=s["w2"][:, 0:1], in1=mk1[:], op0=ALU.mult, op1=ALU.add),
             reads=[mk2, s["w2"], mk1], writes=[CW])
    return CW, GM


def emit_B3_dense(c, io, l, hin, hout, n_tt=8, n_e=64):
    nc = c.nc
    hTb = c.sb([128, 16, TT], BF16, "hTb"); r = c.sb([128, 16, TT], F32, "r")
    for i in range(4):
        c.dma("pool", hTb[:, 4 * i:4 * i + 4, :], hin.ap[512 * i:512 * (i + 1), :].rearrange("(kc p) t -> p kc t", p=128), reads=[hin], writes=[hTb])
        c.dma("sp", r[:, 4 * i:4 * i + 4, :], hin.ap[512 * i:512 * (i + 1), :].rearrange("(kc p) t -> p kc t", p=128), reads=[hin], writes=[r])
    identb = c.sb([128, 128], BF16, "identb"); identf = c.sb([128, 128], F32, "identf")
    c.dma("sp", identb[:], io["identb"][:], reads=[], writes=[identb])
    c.dma("sp", identf[:], io["identf"][:], reads=[], writes=[identf])
    onesf = c.sb([128, 128], F32, "onesf")
    c.op("pool", lambda e: e.memset(onesf[:], 1.0), writes=[onesf])
    y_ps = [c.ps([128, 512], F32, f"y{i}") for i in range(4)]
    g_ps = c.ps([128, 512], F32, "g"); u_ps = c.ps([128, 512], F32, "u")
    tp_ps = c.ps([128, 512], BF16, "tp")
    CW, GM = emit_router(c, io, l, r, g_ps)
    Wg = [c.sb([128, 16, 512], BF16, f"Wg{i}") for i in range(2)]
    Wu = [c.sb([128, 16, 512], BF16, f"Wu{i}") for i in range(1)] * 2
    Wd = [c.sb([128, 4, D], BF16, f"Wd{i}") for i in range(1)] * 2
    sg = c.sb([128, 512], F32, "sg"); hid = c.sb([128, 512], BF16, "hid"); hidT = c.sb([128, 4, 128], BF16, "hidT")
    ysb = c.sb([128, D], F32, "ysb")
    k = 0
    for tt in range(n_tt):
        tsl = slice(tt * 128, (tt + 1) * 128)
        for ex in range(n_e):
            wgb, wub, wdb = Wg[k % 2], Wu[k % 2], Wd[k % 2]; k += 1
            c.dma("pool", wgb[:], io["w_e_gate"].ap[l, ex].rearrange("(kc p) m -> p kc m", p=128), reads=[], writes=[wgb])
            c.dma("pool", wub[:], io["w_e_up"].ap[l, ex].rearrange("(kc p) m -> p kc m", p=128), reads=[], writes=[wub])
            c.dma("pool", wdb[:], io["w_e_down"].ap[l, ex].rearrange("(kc p) m -> p kc m", p=128), reads=[], writes=[wdb])
            for kc in range(16):
                c.op("pe", lambda e: e.matmul(g_ps[:], lhsT=hTb[:, kc, tsl], rhs=wgb[:, kc, :], start=(kc == 0), stop=(kc == 15)), reads=[hTb, wgb], writes=[g_ps])
            for kc in range(16):
                c.op("pe", lambda e: e.matmul(u_ps[:], lhsT=hTb[:, kc, tsl], rhs=wub[:, kc, :], start=(kc == 0), stop=(kc == 15)), reads=[hTb, wub], writes=[u_ps])
            c.op("act", lambda e: e.activation(out=sg[:], in_=g_ps[:], func=AF.Silu), reads=[g_ps], writes=[sg])
            c.op("dve", lambda e: e.scalar_tensor_tensor(out=hid[:], in0=sg[:], scalar=CW[:, tt, ex:ex + 1], in1=u_ps[:], op0=ALU.mult, op1=ALU.mult),
                 reads=[sg, CW, u_ps], writes=[hid])
            for fc in range(4):
                c.op("pe", lambda e: e.transpose(out=tp_ps[:, fc * 128:(fc + 1) * 128], in_=hid[:, fc * 128:(fc + 1) * 128], identity=identb[:]),
                     reads=[hid, identb], writes=[tp_ps])
            c.op("act", lambda e: e.activation(out=hidT[:, :, :], in_=tp_ps[:, :].rearrange("p (f t) -> p f t", f=4), func=AF.Copy), reads=[tp_ps], writes=[hidT])
            for n in range(4):
                for fc in range(4):
                    c.op("pe", lambda e: e.matmul(y_ps[n][:], lhsT=hidT[:, fc, :], rhs=wdb[:, fc, n * 512:(n + 1) * 512],
                                                  start=(ex == 0 and fc == 0), stop=(ex == n_e - 1 and fc == 3)), reads=[hidT, wdb], writes=[y_ps[n]])
        for n in range(4):
            c.op("act", lambda e: e.activation(out=ysb[:, n * 512:(n + 1) * 512], in_=y_ps[n][:], func=AF.Copy), reads=[y_ps[n]], writes=[ysb])
        for m in range(16):
            c.op("pe", lambda e: e.transpose(out=u_ps[:, 0:128], in_=ysb[:, m * 128:(m + 1) * 128], identity=identf[:]), reads=[ysb, identf], writes=[u_ps])
            c.op("dve", lambda e: e.scalar_tensor_tensor(out=r[:, m, tsl], in0=r[:, m, tsl], scalar=ALPHA, in1=u_ps[:, 0:128], op0=ALU.mult, op1=ALU.add),
                 reads=[r, u_ps], writes=[r])
    for tt in range(n_tt, 8):
        tsl = slice(tt * 128, (tt + 1) * 128)
        c.op("dve", lambda e: e.tensor_scalar(out=r[:, :, tsl], in0=r[:, :, tsl], scalar1=ALPHA, scalar2=None, op0=ALU.mult), reads=[r], writes=[r])
    emit_ln(c, r, io["ln2_g"].ap[l], io["ln2_b"].ap[l], hout, y_ps, onesf)


def emit_B3(c, io, l, hin, hout, n_e=64):
    nc = c.nc
    c.phase_begin()
    r = c.sb([128, 16, TT], F32, "r")
    for i in range(4):
        c.dma("sp", r[:, 4 * i:4 * i + 4, :], hin.ap[512 * i:512 * (i + 1), :].rearrange("(kc p) t -> p kc t", p=128), reads=[hin], writes=[r])
    g_ps = c.ps([128, 512], F32, "g")
    CW, GM = emit_router(c, io, l, r, g_ps)
    c.dma("sp", io["cwd"].ap.rearrange("(tt p) e -> p tt e", p=128), CW[:], reads=[CW], writes=[io["cwd"]])
    c.phase_end()
    c.phase_begin()
    hTb = c.sb([128, 16, TT], BF16, "hTb")
    for i in range(4):
        c.dma("pool", hTb[:, 4 * i:4 * i + 4, :], hin.ap[512 * i:512 * (i + 1), :].rearrange("(kc p) t -> p kc t", p=128), reads=[hin], writes=[hTb])
    CW = c.sb([128, 8, 64], F32, "CW2")
    c.dma("sp", CW[:], io["cwd"].ap.rearrange("(tt p) e -> p tt e", p=128), reads=[io["cwd"]], writes=[CW])
    identb = c.sb([128, 128], BF16, "identb")
    c.dma("sp", identb[:], io["identb"][:], reads=[], writes=[identb])
    y_ps = [c.ps([128, 512], F32, f"y{i}") for i in range(4)]
    g_ps = c.ps([128, 512], F32, "g"); u_ps = c.ps([128, 512], F32, "u")
    tp_ps = c.ps([128, 512], BF16, "tp")
    y_sb = [c.sb([128, D], F32, f"ysb{i}") for i in range(8)]
    Wg = [c.sb([128, 16, 512], BF16, f"Wg{i}") for i in range(2)]
    Wu = [c.sb([128, 16, 512], BF16, f"Wu{i}") for i in range(2)]
    Wd = [c.sb([128, 4, D], BF16, f"Wd{i}") for i in range(2)]
    sg = c.sb([128, 512], F32, "sg"); hid = c.sb([128, 512], BF16, "hid"); hidT = c.sb([128, 4, 128], BF16, "hidT")
    for ex in range(n_e):
        wgb, wub, wdb = Wg[ex % 2], Wu[ex % 2], Wd[ex % 2]
        c.dma("pool", wgb[:], io["w_e_gate"].ap[l, ex].rearrange("(kc p) m -> p kc m", p=128), reads=[], writes=[wgb])
        c.dma("pool", wub[:], io["w_e_up"].ap[l, ex].rearrange("(kc p) m -> p kc m", p=128), reads=[], writes=[wub])
        c.dma("pool", wdb[:], io["w_e_down"].ap[l, ex].rearrange("(kc p) m -> p kc m", p=128), reads=[], writes=[wdb])
        for tt in range(8):
            tsl = slice(tt * 128, (tt + 1) * 128)
            for kc in range(16):
                c.op("pe", lambda e: e.matmul(g_ps[:], lhsT=hTb[:, kc, tsl], rhs=wgb[:, kc, :], start=(kc == 0), stop=(kc == 15)), reads=[hTb, wgb], writes=[g_ps])
            for kc in range(16):
                c.op("pe", lambda e: e.matmul(u_ps[:], lhsT=hTb[:, kc, tsl], rhs=wub[:, kc, :], start=(kc == 0), stop=(kc == 15)), reads=[hTb, wub], writes=[u_ps])
            c.op("act", lambda e: e.activation(out=sg[:], in_=g_ps[:], func=AF.Silu), reads=[g_ps], writes=[sg])
            c.op("dve", lambda e: e.scalar_tensor_tensor(out=hid[:], in0=sg[:], scalar=CW[:, tt, ex:ex + 1], in1=u_ps[:], op0=ALU.mult, op1=ALU.mult),
                 reads=[sg, CW, u_ps], writes=[hid])
            for fc in range(4):
                c.op("pe", lambda e: e.transpose(out=tp_ps[:, fc * 128:(fc + 1) * 128], in_=hid[:, fc * 128:(fc + 1) * 128], identity=identb[:]),
                     reads=[hid, identb], writes=[tp_ps])
            c.op("act", lambda e: e.activation(out=hidT[:, :, :], in_=tp_ps[:, :].rearrange("p (f t) -> p f t", f=4), func=AF.Copy), reads=[tp_ps], writes=[hidT])
            for n in range(4):
                for fc in range(4):
                    c.op("pe", lambda e: e.matmul(y_ps[n][:], lhsT=hidT[:, fc, :], rhs=wdb[:, fc, n * 512:(n + 1) * 512],
                                                  start=(fc == 0), stop=(fc == 3)), reads=[hidT, wdb], writes=[y_ps[n]])
            for n in range(4):
                ns = slice(n * 512, (n + 1) * 512)
                if ex == 0:
                    c.op("act", lambda e: e.activation(out=y_sb[tt][:, ns], in_=y_ps[n][:], func=AF.Copy), reads=[y_ps[n]], writes=[y_sb[tt]])
                else:
                    c.op("dve", lambda e: e.tensor_tensor(out=y_sb[tt][:, ns], in0=y_sb[tt][:, ns], in1=y_ps[n][:], op=ALU.add),
                         reads=[y_sb[tt], y_ps[n]], writes=[y_sb[tt]])
    for tt in range(8):
        c.dma("sp", io["yd"].ap[tt * 128:(tt + 1) * 128, :], y_sb[tt][:], reads=[y_sb[tt]], writes=[io["yd"]])
    c.phase_end()
    c.phase_begin()
    r = c.sb([128, 16, TT], F32, "r")
    for i in range(4):
        c.dma("sp", r[:, 4 * i:4 * i + 4, :], hin.ap[512 * i:512 * (i + 1), :].rearrange("(kc p) t -> p kc t", p=128), reads=[hin], writes=[r])
    identf = c.sb([128, 128], F32, "identf")
    c.dma("sp", identf[:], io["identf"][:], reads=[], writes=[identf])
    onesf = c.sb([128, 128], F32, "onesf")
    c.op("pool", lambda e: e.memset(onesf[:], 1.0), writes=[onesf])
    ps = [c.ps([128, 512], F32, f"ps{i}") for i in range(6)]
    yl = [c.sb([128, D], F32, f"yl{i}") for i in range(2)]
    k = 0
    for tt in range(8):
        tsl = slice(tt * 128, (tt + 1) * 128)
        y = yl[tt % 2]
        c.dma("sp", y[:], io["yd"].ap[tt * 128:(tt + 1) * 128, :], reads=[io["yd"]], writes=[y])
        for m in range(16):
            p = ps[4 + (k % 2)]; k += 1
            c.op("pe", lambda e: e.transpose(out=p[:, 0:128], in_=y[:, m * 128:(m + 1) * 128], identity=identf[:]), reads=[y, identf], writes=[p])
            c.op("dve", lambda e: e.scalar_tensor_tensor(out=r[:, m, tsl], in0=r[:, m, tsl], scalar=ALPHA, in1=p[:, 0:128], op0=ALU.mult, op1=ALU.add),
                 reads=[r, p], writes=[r])
    emit_ln(c, r, io["ln2_g"].ap[l], io["ln2_b"].ap[l], hout, ps, onesf)
    c.phase_end()

W_SPECS = (("w_in", [D, IN_W]), ("g_q", [512]), ("w_q_b", [512, 1536]), ("g_kv", [512]), ("w_kv_b", [512, 2048]),
           ("w_o_a", [1024, D]), ("w_o_b", [1024, D]), ("w_out", [D, D]), ("ln1_g", [D]), ("ln1_b", [D]),
           ("w_group", [D, 8]), ("b_group", [8]), ("w_router", [D, 64]), ("b_router", [64]),
           ("w_e_gate", [64, D, 512]), ("w_e_up", [64, D, 512]), ("w_e_down", [64, 512, D]), ("ln2_g", [D]), ("ln2_b", [D]))


def build_fused(depth=2, moe="dense", moe_kw=None, n_exp=64):
    c = Ctx(); nc = c.nc
    io = {}
    io["xT"] = c.dram("xT", [D, TT], F32, "ExternalInput")
    io["posq"] = c.dram("posq", [1, TT], I32, "ExternalInput")
    io["posk"] = c.dram("posk", [1, SEQ], I32, "ExternalInput")
    for nm, shp in W_SPECS:
        if nm.startswith("w_e_"):
            shp = [n_exp] + shp[1:]
        io[nm] = c.dram(nm, [depth] + shp, F32, "ExternalInput")
    io["inv"] = c.dram("inv", [128, 3], F32, "ExternalInput")
    io["perm"] = c.dram("perm", [3, 128, 128], F32, "ExternalInput")
    io["identb"] = c.dram("identb", [128, 128], BF16, "ExternalInput")
    io["identf"] = c.dram("identf", [128, 128], F32, "ExternalInput")
    io["outT"] = c.dram("outT", [D, TT], F32, "ExternalOutput")
    io["qaT"] = c.dram("qaT", [8, 192, TT], BF16); io["qbT"] = c.dram("qbT", [8, 128, TT], BF16); io["qiT"] = c.dram("qiT", [8, 128, TT], BF16)
    io["widx"] = c.dram("widx", [TT, 16], F32)
    io["kvpack"] = c.dram("kvpack", [KVROWS, TT], BF16)
    io["gath"] = None
    for k in range(NCH):
        rows = min(CH, KVROWS - k * CH)
        io["gath%d" % k] = c.dram("gath%d" % k, [4 * rows, TT], BF16)
    io["oaT"] = c.dram("oaT", [1024, TT], BF16); io["obT"] = c.dram("obT", [1024, TT], BF16)
    io["hA"] = c.dram("hA", [D, TT], F32); io["hB"] = c.dram("hB", [D, TT], F32)
    io["cwd"] = c.dram("cwd", [TT, 64], F32); io["yd"] = c.dram("yd", [TT, D], F32)
    hin = io["xT"]
    for l in range(depth):
        c.phase_begin(); emit_A(c, io, l, hin); c.phase_end()
        if UPTO < 1:
            break
        for k in range(NCH):
            rows = min(CH, KVROWS - k * CH)
            c.collective("AllGather", io["kvpack"], io["kvpack"].ap[k * CH:k * CH + rows, :], io["gath%d" % k], io["gath%d" % k].ap[:, :], GROUPS)
        if UPTO < 2:
            break
        c.phase_begin(); emit_B1(c, io); c.phase_end()
        if UPTO < 3:
            break
        c.phase_begin(); emit_B2(c, io, l, hin, io["hA"]); c.phase_end()
        if UPTO < 4:
            break
        hout = io["outT"] if l == depth - 1 else io["hB"]
        if moe == "dense":
            c.phase_begin()
            emit_B3_dense(c, io, l, io["hA"], hout, **(moe_kw or {}))
            c.phase_end()
        else:
            emit_B3(c, io, l, io["hA"], hout, **(moe_kw or {}))
        hin = hout
    c.finish()
    return nc


_CACHE = {}
MOE_MODE = "v2"
UPTO = 99


def kernel(x, positions, w_in, g_q_lora, w_q_b, g_kv_lora, w_kv_b, w_o_a, w_o_b, w_out, ln1_g, ln1_b,
           w_group, b_group, w_router, b_router, w_e_gate, w_e_up, w_e_down, ln2_g, ln2_b):
    x = np.asarray(x); positions = np.asarray(positions)
    depth = int(np.asarray(w_in).shape[0])
    key = ("fused", depth, MOE_MODE)
    if key not in _CACHE:
        _CACHE[key] = build_fused(depth, MOE_MODE)
    nc = _CACHE[key]
    inv, perm = _rope_consts()
    identf = np.eye(128, dtype=np.float32); identb = identf.astype(NPBF)
    wts = {"w_in": w_in, "g_q": g_q_lora, "w_q_b": w_q_b, "g_kv": g_kv_lora, "w_kv_b": w_kv_b, "w_o_a": w_o_a, "w_o_b": w_o_b,
           "w_out": w_out, "ln1_g": ln1_g, "ln1_b": ln1_b, "w_group": w_group, "b_group": b_group, "w_router": w_router,
           "b_router": b_router, "w_e_gate": w_e_gate, "w_e_up": w_e_up, "w_e_down": w_e_down, "ln2_g": ln2_g, "ln2_b": ln2_b}
    wts = {k: np.ascontiguousarray(np.asarray(v), dtype=np.float32) for k, v in wts.items()}
    nb = x.shape[0]
    cores = [(b, j) for b in range(nb) for j in range(4)]
    in_maps = []
    for (b, j) in cores:
        m = dict(wts)
        m["xT"] = np.ascontiguousarray(x[b, j * TT:(j + 1) * TT, :].T)
        m["posq"] = np.ascontiguousarray(positions[b, j * TT:(j + 1) * TT][None, :]).astype(np.int32)
        m["posk"] = np.ascontiguousarray(positions[b][None, :]).astype(np.int32)
        m["inv"] = inv; m["perm"] = perm; m["identb"] = identb; m["identf"] = identf
        in_maps.append(m)
    res = run_bass_kernel_spmd(nc, in_maps, core_ids=list(range(len(cores)))).results
    out = np.empty(x.shape, np.float32)
    for ci, (b, j) in enumerate(cores):
        out[b, j * TT:(j + 1) * TT, :] = res[ci]["outT"].T
    return out
```

```python
import math
import numpy as np
from contextlib import ExitStack
import ml_dtypes
import concourse.bass as bass
import concourse.mybir as mybir
from concourse.bass_utils import run_bass_kernel_spmd

F32 = mybir.dt.float32; BF16 = mybir.dt.bfloat16; I32 = mybir.dt.int32
AF = mybir.ActivationFunctionType; ALU = mybir.AluOpType; AX = mybir.AxisListType
NPBF = ml_dtypes.bfloat16

D = 2048; TT = 1024; NCORE = 8; SEQ = 4096
IN_W = 9360
C_KPE = 1024; C_QB = 1088; C_KB = 2112; C_VB = 3136; C_QI = 4160; C_KI = 5184; C_WI = 5248; C_GA = 5264; C_GB = 7312
ALPHA = 4 ** 0.25
THETA = 500000.0


class T:
    __slots__ = ("ap", "w", "r", "name", "excl")

    def __init__(self, ap, name="", excl=False):
        self.ap = ap; self.w = None; self.r = {}; self.name = name; self.excl = excl

    def __getitem__(self, k):
        return self.ap[k]


class E:
    def __init__(self, name, obj, sem):
        self.name = name; self.obj = obj; self.sem = sem; self.count = 0; self.seen = {}


class Ctx:
    NDMASEM = 8

    def __init__(self):
        self.nc = nc = bass.Bass("TRN2", target_bir_lowering=False)
        self.es = ExitStack()
        self.eng = {}
        for name, obj in (("pe", nc.tensor), ("act", nc.scalar), ("dve", nc.vector), ("pool", nc.gpsimd), ("sp", nc.sync)):
            sem = self.es.enter_context(nc.semaphore("s_" + name))
            self.eng[name] = E(name, obj, sem)
        self.dq = {}
        for q in ("sp", "pool", "act"):
            lst = []
            for i in range(self.NDMASEM):
                sem = self.es.enter_context(nc.semaphore(f"d_{q}{i}"))
                lst.append(E(f"d_{q}{i}", None, sem))
            self.dq[q] = [lst, 0]
        self.cc = E("cc", None, self.es.enter_context(nc.semaphore("cc_sem")))
        self.pes = None
        self.ntile = 0
        self.rr = 0

    def sb(self, shape, dt, name=None):
        self.ntile += 1
        name = f"sb_{name or 't'}_{self.ntile}"
        h = (self.pes or self.es).enter_context(self.nc.sbuf_tensor(name, list(shape), dt))
        return T(h, name)

    def ps(self, shape, dt=F32, name=None):
        self.ntile += 1
        name = f"ps_{name or 'p'}_{self.ntile}"
        h = (self.pes or self.es).enter_context(self.nc.psum_tensor(name, list(shape), dt))
        return T(h, name, excl=True)

    def dram(self, name, shape, dt, kind="Internal"):
        h = self.nc.dram_tensor(name, list(shape), dt, kind=kind)
        return T(h.ap(), name)

    def _deps(self, e, reads, writes, extra=()):
        deps = {}

        def add(d):
            if d is None:
                return
            p, idx = d
            if p is e and e.name in ("pe", "sp"):
                return
            if deps.get(p, 0) < idx:
                deps[p] = idx
        for t in reads:
            add(t.w)
            if t.excl:
                for p, idx in t.r.items():
                    add((p, idx))
        for t in writes:
            add(t.w)
            for p, idx in t.r.items():
                add((p, idx))
        for d in extra:
            add(d)
        for p, idx in deps.items():
            if e.seen.get(p, 0) < idx:
                e.obj.wait_ge(p.sem, idx)
                e.seen[p] = idx

    def op(self, en, fn, reads=(), writes=()):
        e = self.eng[en]
        self._deps(e, reads, writes)
        inst = fn(e.obj)
        e.count += 1
        inst.then_inc(e.sem, 1)
        for t in writes:
            t.w = (e, e.count); t.r = {}
        for t in reads:
            t.r[e] = e.count
        return inst

    def dma(self, q, out, in_, reads=(), writes=(), **kw):
        e = self.eng[q]
        lst, i = self.dq[q]
        d = lst[i % len(lst)]; self.dq[q][1] = i + 1
        extra = [(d, d.count)] if d.count else []
        self._deps(e, reads, writes, extra)
        inst = e.obj.dma_start(out=out, in_=in_, **kw)
        d.count += 16
        inst.then_inc(d.sem, 16)
        for t in writes:
            t.w = (d, d.count); t.r = {}
        for t in reads:
            t.r[d] = d.count
        return inst

    def barrier(self):
        engs = list(self.eng.values())
        others = engs + [d for (lst, _) in self.dq.values() for d in lst] + [self.cc]
        for e in engs:
            for p in others:
                if p is e:
                    continue
                if p.count and e.seen.get(p, 0) < p.count:
                    e.obj.wait_ge(p.sem, p.count); e.seen[p] = p.count

    def phase_begin(self):
        self.pes = ExitStack()

    def phase_end(self):
        self.barrier()
        self.pes.close(); self.pes = None

    def collective(self, kind, in_t, in_ap, out_t, out_ap, groups):
        e = self.eng["pool"]; d = self.cc
        self._deps(e, [in_t], [out_t])
        inst = e.obj.collective_compute(kind, ALU.bypass, replica_groups=groups, ins=[in_ap], outs=[out_ap])
        d.count += 1
        inst.then_inc(d.sem, 1)
        out_t.w = (d, d.count); out_t.r = {}
        in_t.r[d] = d.count

    def finish(self):
        e = self.eng["sp"]
        for p in list(self.eng.values()):
            if p is not e and p.count and e.seen.get(p, 0) < p.count:
                e.obj.wait_ge(p.sem, p.count); e.seen[p] = p.count
        for q, (lst, _) in self.dq.items():
            for d in lst:
                if d.count and e.seen.get(d, 0) < d.count:
                    e.obj.wait_ge(d.sem, d.count); e.seen[d] = d.count
        if self.cc.count and e.seen.get(self.cc, 0) < self.cc.count:
            e.obj.wait_ge(self.cc.sem, self.cc.count)
        self.es.close()

    def alt(self, engs=("dve", "act")):
        self.rr += 1
        return engs[self.rr % len(engs)]


def _rope_consts():
    inv = np.zeros((128, 3), np.float32)
    perm = np.zeros((3, 128, 128), np.float32)
    f = THETA ** (-np.arange(0, 64, 2, dtype=np.float32) / 64.0)
    inv[0:32, 0] = f; inv[32:64, 0] = f
    for i in range(32):
        perm[0, 32 + i, i] = -1.0; perm[0, i, 32 + i] = 1.0
    f = THETA ** (-np.arange(0, 32, 2, dtype=np.float32) / 32.0)
    inv[0:16, 1] = f; inv[16:32, 1] = f
    for i in range(16):
        perm[1, 16 + i, i] = -1.0; perm[1, i, 16 + i] = 1.0
    f = THETA ** (-np.arange(0, 16, 2, dtype=np.float32) / 16.0)
    for base in (0, 64):
        inv[base:base + 8, 2] = f; inv[base + 8:base + 16, 2] = f
        for i in range(8):
            perm[2, base + 8 + i, base + i] = -1.0; perm[2, base + i, base + 8 + i] = 1.0
    inv = (inv.astype(np.float64) / (2 * np.pi)).astype(np.float32)
    return inv, perm


KA_OFF = 0; KB_OFF = 1024; VA_OFF = 2048; VB_OFF = 3072; KPE_OFF = 4096; KI_OFF = 4160; KVROWS = 4224
CH = 512; NCH = 9
NIT = 18
TOPK = 256
GROUPS = [[0, 1, 2, 3], [4, 5, 6, 7]]


def emit_A(c, io, l, hin):
    nc = c.nc
    win_d = io["w_in"]; wqb_d = io["w_q_b"]; wkvb_d = io["w_kv_b"]
    kv = io["kvpack"]
    hTb = c.sb([128, 16, TT], BF16, "hTb")
    for i in range(4):
        c.dma("pool", hTb[:, 4 * i:4 * i + 4, :], hin.ap[512 * i:512 * (i + 1), :].rearrange("(kc p) t -> p kc t", p=128),
              reads=[hin], writes=[hTb])
    inv = c.sb([128, 3], F32, "inv"); c.dma("sp", inv[:], io["inv"][:], reads=[io["inv"]], writes=[inv])
    perm = c.sb([128, 3, 128], F32, "perm")
    c.dma("sp", perm[:], io["perm"].ap.rearrange("r k m -> k r m"), reads=[io["perm"]], writes=[perm])
    gq = c.sb([128, 4], F32, "gq"); gkv = c.sb([128, 4], F32, "gkv")
    with nc.allow_non_contiguous_dma(reason="tiny gain vectors"):
        c.dma("sp", gq[:], io["g_q"].ap[l].rearrange("(c p) -> p c", p=128), reads=[io["g_q"]], writes=[gq])
        c.dma("sp", gkv[:], io["g_kv"].ap[l].rearrange("(c p) -> p c", p=128), reads=[io["g_kv"]], writes=[gkv])
    ones = c.sb([128, 128], BF16, "ones")
    c.op("pool", lambda e: e.memset(ones[:], 1.0), writes=[ones])

    posi = c.sb([128, TT], I32, "posi")
    c.dma("sp", posi[:], io["posq"].ap[0, :].partition_broadcast(128), reads=[io["posq"]], writes=[posi])
    posf = c.sb([128, TT], F32, "posf")
    c.op("dve", lambda e: e.tensor_copy(out=posf[:], in_=posi[:]), reads=[posi], writes=[posf])
    CC = c.sb([128, 3, TT], F32, "CC"); SS = c.sb([128, 3, TT], F32, "SS")
    u = c.sb([128, TT], F32, "u"); uf = c.sb([128, TT], F32, "uf")
    for r in range(3):
        for which, dst in ((0, SS), (1, CC)):
            c.op("dve", lambda e: e.tensor_scalar(out=u[:], in0=posf[:], scalar1=inv[:, r:r + 1], scalar2=0.25 * which,
                                                  op0=ALU.mult, op1=ALU.add), reads=[posf, inv], writes=[u])
            c.op("dve", lambda e: e.tensor_copy(out=posi[:], in_=u[:]), reads=[u], writes=[posi])
            c.op("dve", lambda e: e.tensor_copy(out=uf[:], in_=posi[:]), reads=[posi], writes=[uf])
            c.op("dve", lambda e: e.tensor_tensor(out=u[:], in0=u[:], in1=uf[:], op=ALU.subtract), reads=[u, uf], writes=[u])
            c.op("dve", lambda e: e.scalar_tensor_tensor(out=uf[:], in0=u[:], scalar=0.5, in1=u[:], op0=ALU.is_gt, op1=ALU.subtract),
                 reads=[u], writes=[uf])
            c.op("act", lambda e: e.activation(out=dst[:, r, :], in_=uf[:], func=AF.Sin, scale=-6.283185), reads=[uf], writes=[dst])

    wbufs = [c.sb([128, 16, 512], BF16, f"wbuf{i}") for i in range(2)]
    accs = [c.ps([128, TT], F32, f"acc{i}") for i in range(2)]
    rot_ps = c.ps([128, TT], F32, "rot_ps")
    rep_ps = c.ps([128, TT], F32, "rep_ps")
    stg = [c.sb([128, TT], BF16, f"stg{i}") for i in range(3)]
    pf = [c.sb([128, TT], F32, f"pf{i}") for i in range(2)]
    t1 = [c.sb([128, TT], F32, f"t1_{i}") for i in range(2)]
    st = {"w": 0, "a": 0, "s": 0, "p": 0}

    def load_w(w_ap, K, col0, ncols):
        KC = K // 128
        wb = wbufs[st["w"] % 2]; st["w"] += 1
        c.dma("pool", wb[:, 0:KC, 0:ncols], w_ap[:, col0:col0 + ncols].rearrange("(kc p) m -> p kc m", p=128),
              reads=[], writes=[wb])
        return wb

    def fm_chunk(wb, KC, off, M, rhs, epi):
        acc = accs[st["a"] % 2]; st["a"] += 1
        for th in range(2):
            for kc in range(KC):
                c.op("pe", lambda e: e.matmul(acc[0:M, th * 512:(th + 1) * 512], lhsT=wb[:, kc, off:off + M],
                                              rhs=rhs[:, kc, th * 512:(th + 1) * 512], start=(kc == 0), stop=(kc == KC - 1)),
                     reads=[wb, rhs], writes=[acc])
        epi(acc, M)

    def epi_plain(dst_ap, dst_t, rep=None):
        def f(acc, M):
            s = stg[st["s"] % 3]; st["s"] += 1
            if rep is not None:
                c.op("dve", lambda e: e.tensor_tensor(out=s[0:M, :], in0=acc[0:M, :], in1=rep[0:M, :], op=ALU.mult),
                     reads=[acc, rep], writes=[s])
            else:
                en = c.alt()
                if en == "act":
                    c.op("act", lambda e: e.activation(out=s[0:M, :], in_=acc[0:M, :], func=AF.Copy), reads=[acc], writes=[s])
                else:
                    c.op("dve", lambda e: e.tensor_copy(out=s[0:M, :], in_=acc[0:M, :]), reads=[acc], writes=[s])
            c.dma("sp", dst_ap, s[0:M, :], reads=[s], writes=[dst_t])
        return f

    def epi_rope(r, dst_ap, dst_t, rep=None):
        def f(acc, M):
            p = pf[st["p"] % 2]; q = t1[st["p"] % 2]; st["p"] += 1
            if rep is not None:
                c.op("dve", lambda e: e.tensor_tensor(out=p[0:M, :], in0=acc[0:M, :], in1=rep[0:M, :], op=ALU.mult),
                     reads=[acc, rep], writes=[p])
            else:
                c.op("act", lambda e: e.activation(out=p[0:M, :], in_=acc[0:M, :], func=AF.Copy), reads=[acc], writes=[p])
            for th in range(2):
                c.op("pe", lambda e: e.matmul(rot_ps[0:M, th * 512:(th + 1) * 512], lhsT=perm[0:M, r, 0:M],
                                              rhs=p[0:M, th * 512:(th + 1) * 512], start=True, stop=True),
                     reads=[perm, p], writes=[rot_ps])
            c.op("dve", lambda e: e.tensor_tensor(out=q[0:M, :], in0=rot_ps[0:M, :], in1=SS[0:M, r, :], op=ALU.mult),
                 reads=[rot_ps, SS], writes=[q])
            c.op("pool", lambda e: e.tensor_tensor(out=p[0:M, :], in0=p[0:M, :], in1=CC[0:M, r, :], op=ALU.mult),
                 reads=[p, CC], writes=[p])
            s = stg[st["s"] % 3]; st["s"] += 1
            c.op("dve", lambda e: e.tensor_tensor(out=s[0:M, :], in0=p[0:M, :], in1=q[0:M, :], op=ALU.add),
                 reads=[p, q], writes=[s])
            c.dma("sp", dst_ap, s[0:M, :], reads=[s], writes=[dst_t])
        return f

    cn = {"q": c.sb([128, 4, TT], BF16, "cqn"), "kv": c.sb([128, 4, TT], BF16, "ckvn")}
    sq = c.sb([128, 4, TT], BF16, "sq")
    rstd = {"q": c.sb([128, TT], F32, "rstd_q"), "kv": c.sb([128, TT], F32, "rstd_kv")}
    rstd_tm = c.sb([128, 8], F32, "rstd_tm")
    eps_t = c.sb([128, 1], F32, "eps")
    c.op("pool", lambda e: e.memset(eps_t[:], 1e-6), writes=[eps_t])

    def epi_rms(which, i, g):
        def f(acc, M):
            c.op("act", lambda e: e.activation(out=sq[:, i, :], in_=acc[:, :], func=AF.Square), reads=[acc], writes=[sq])
            c.op("dve", lambda e: e.tensor_scalar(out=cn[which][:, i, :], in0=acc[:, :], scalar1=g[:, i:i + 1], scalar2=None, op0=ALU.mult),
                 reads=[acc, g], writes=[cn[which]])
        return f

    win = win_d.ap[l]
    for which, col0, g in (("q", 0, gq), ("kv", 512, gkv)):
        wb = load_w(win, D, col0, 512)
        for i in range(4):
            fm_chunk(wb, 16, 128 * i, 128, hTb, epi_rms(which, i, g))
        for th in range(2):
            for i in range(4):
                c.op("pe", lambda e: e.matmul(rep_ps[:, th * 512:(th + 1) * 512], lhsT=ones[:, :], rhs=sq[:, i, th * 512:(th + 1) * 512],
                                              start=(i == 0), stop=(i == 3)), reads=[ones, sq], writes=[rep_ps])
        c.op("act", lambda e: e.activation(out=rstd[which][:], in_=rep_ps[:], func=AF.Sqrt, scale=1.0 / 512.0, bias=eps_t[:, 0:1]),
             reads=[rep_ps, eps_t], writes=[rstd[which]])
        c.op("dve", lambda e: e.reciprocal(out=rstd[which][:], in_=rstd[which][:]), reads=[rstd[which]], writes=[rstd[which]])
        if which == "kv":
            for tt in range(8):
                for i in range(4):
                    c.op("pe", lambda e: e.matmul(rep_ps[:, tt:tt + 1], lhsT=sq[:, i, tt * 128:(tt + 1) * 128], rhs=ones[:, 0:1],
                                                  start=(i == 0), stop=(i == 3)), reads=[sq, ones], writes=[rep_ps])
            c.op("act", lambda e: e.activation(out=rstd_tm[:], in_=rep_ps[:, 0:8], func=AF.Sqrt, scale=1.0 / 512.0, bias=eps_t[:, 0:1]),
                 reads=[rep_ps, eps_t], writes=[rstd_tm])
            c.op("dve", lambda e: e.reciprocal(out=rstd_tm[:], in_=rstd_tm[:]), reads=[rstd_tm], writes=[rstd_tm])

    wb = load_w(win, D, C_KPE, 64)
    fm_chunk(wb, 16, 0, 64, hTb, epi_rope(0, kv[KPE_OFF:KPE_OFF + 64, :], kv))
    for g in range(2):
        wb = load_w(win, D, C_QB + 512 * g, 512)
        for j in range(4):
            fm_chunk(wb, 16, 128 * j, 128, hTb, epi_rope(1, io["qbT"][4 * g + j, :, :], io["qbT"]))
    for g in range(2):
        wb = load_w(win, D, C_KB + 512 * g, 512)
        for j in range(4):
            h = 4 * g + j
            fm_chunk(wb, 16, 128 * j, 128, hTb, epi_rope(1, kv[KB_OFF + 128 * h:KB_OFF + 128 * (h + 1), :], kv))
    for g in range(2):
        wb = load_w(win, D, C_VB + 512 * g, 512)
        for tt in range(8):
            acc = accs[st["a"] % 2]; st["a"] += 1
            for kc in range(16):
                c.op("pe", lambda e: e.matmul(acc[:, 0:512], lhsT=hTb[:, kc, tt * 128:(tt + 1) * 128], rhs=wb[:, kc, 0:512],
                                              start=(kc == 0), stop=(kc == 15)), reads=[hTb, wb], writes=[acc])
            s = stg[st["s"] % 3]; st["s"] += 1
            en = c.alt()
            if en == "act":
                c.op("act", lambda e: e.activation(out=s[:, 0:512], in_=acc[:, 0:512], func=AF.Copy), reads=[acc], writes=[s])
            else:
                c.op("dve", lambda e: e.tensor_copy(out=s[:, 0:512], in_=acc[:, 0:512]), reads=[acc], writes=[s])
            c.dma("sp", kv[VB_OFF + tt * 128:VB_OFF + (tt + 1) * 128, 512 * g:512 * (g + 1)], s[:, 0:512], reads=[s], writes=[kv])
    for g in range(2):
        wb = load_w(win, D, C_QI + 512 * g, 512)
        for j in range(4):
            fm_chunk(wb, 16, 128 * j, 128, hTb, epi_rope(2, io["qiT"][4 * g + j, :, :], io["qiT"]))
    wb = load_w(win, D, C_KI, 80)
    fm_chunk(wb, 16, 0, 64, hTb, epi_rope(2, kv[KI_OFF:KI_OFF + 64, :], kv))
    wst = c.sb([128, 8, 16], F32, "wst")
    for tt in range(8):
        acc = accs[st["a"] % 2]; st["a"] += 1
        for kc in range(16):
            c.op("pe", lambda e: e.matmul(acc[:, 0:16], lhsT=hTb[:, kc, tt * 128:(tt + 1) * 128], rhs=wb[:, kc, 64:80],
                                          start=(kc == 0), stop=(kc == 15)), reads=[hTb, wb], writes=[acc])
        c.op("act", lambda e: e.activation(out=wst[:, tt, :], in_=acc[:, 0:16], func=AF.Copy, scale=1.0 / 32.0), reads=[acc], writes=[wst])
    c.dma("sp", io["widx"].ap.rearrange("(tt p) h -> p tt h", p=128), wst[:], reads=[wst], writes=[io["widx"]])
    wqb = wqb_d.ap[l]
    for g in range(4):
        wb = load_w(wqb, 512, 384 * g, 384)
        for j in range(2):
            h = 2 * g + j
            fm_chunk(wb, 4, 192 * j, 128, cn["q"], epi_plain(io["qaT"][h, 0:128, :], io["qaT"], rep=rstd["q"]))
            fm_chunk(wb, 4, 192 * j + 128, 64, cn["q"], epi_rope(0, io["qaT"][h, 128:192, :], io["qaT"], rep=rstd["q"]))
    wkvb = wkvb_d.ap[l]
    for g in range(4):
        wb = load_w(wkvb, 512, 512 * g, 512)
        for j in range(2):
            h = 2 * g + j
            fm_chunk(wb, 4, 256 * j, 128, cn["kv"], epi_plain(kv[KA_OFF + 128 * h:KA_OFF + 128 * (h + 1), :], kv, rep=rstd["kv"]))
        for tt in range(8):
            acc = accs[st["a"] % 2]; st["a"] += 1
            for j in range(2):
                for kc in range(4):
                    c.op("pe", lambda e: e.matmul(acc[:, 128 * j:128 * (j + 1)], lhsT=cn["kv"][:, kc, tt * 128:(tt + 1) * 128],
                                                  rhs=wb[:, kc, 256 * j + 128:256 * j + 256], start=(kc == 0), stop=(kc == 3)),
                         reads=[cn["kv"], wb], writes=[acc])
            s = stg[st["s"] % 3]; st["s"] += 1
            c.op("dve", lambda e: e.tensor_scalar(out=s[:, 0:256], in0=acc[:, 0:256], scalar1=rstd_tm[:, tt:tt + 1], scalar2=None, op0=ALU.mult),
                 reads=[acc, rstd_tm], writes=[s])
            c.dma("sp", kv[VA_OFF + tt * 128:VA_OFF + (tt + 1) * 128, 256 * g:256 * (g + 1)], s[:, 0:256], reads=[s], writes=[kv])


def emit_B1(c, io):
    nc = c.nc
    NK = SEQ; NKC = NK // 128; LA = 2
    gath = io["gath"]

    def gk(row0, nrows):
        k = row0 // CH; off = row0 % CH
        rows = min(CH, KVROWS - k * CH)
        return io["gath%d" % k].ap.rearrange("(r f) t -> f r t", r=4)[off:off + nrows, :, :], io["gath%d" % k]

    sc = c.sb([128, NK], F32, "sc")
    tmpi = sc[:, :].bitcast(I32)
    posq_bc = c.sb([128, TT], F32, "posq_bc")
    c.dma("sp", tmpi[:, 0:TT], io["posq"].ap[0, :].partition_broadcast(128), reads=[io["posq"]], writes=[sc])
    c.op("dve", lambda e: e.tensor_copy(out=posq_bc[:], in_=tmpi[:, 0:TT]), reads=[sc], writes=[posq_bc])
    posk_bc = c.sb([128, NK], F32, "posk_bc")
    c.dma("sp", tmpi[:, :], io["posk"].ap[0, :].partition_broadcast(128), reads=[io["posk"]], writes=[sc])
    c.op("dve", lambda e: e.tensor_copy(out=posk_bc[:], in_=tmpi[:, :]), reads=[sc], writes=[posk_bc])
    pci = c.sb([128, 40], I32, "pci"); posk_col = c.sb([128, NKC], F32, "posk_col"); posq_col = c.sb([128, 8], F32, "posq_col")
    with nc.allow_non_contiguous_dma(reason="tiny position columns"):
        c.dma("sp", pci[:, 0:NKC], io["posk"].ap[0, :].rearrange("(kc p) -> p kc", p=128), reads=[io["posk"]], writes=[pci])
        c.dma("sp", pci[:, 32:40], io["posq"].ap[0, :].rearrange("(kc p) -> p kc", p=128), reads=[io["posq"]], writes=[pci])
    c.op("dve", lambda e: e.tensor_copy(out=posk_col[:], in_=pci[:, 0:NKC]), reads=[pci], writes=[posk_col])
    c.op("dve", lambda e: e.tensor_copy(out=posq_col[:], in_=pci[:, 32:40]), reads=[pci], writes=[posq_col])
    ones = c.sb([128, 128], BF16, "ones")
    c.op("pool", lambda e: e.memset(ones[:], 1.0), writes=[ones])
    ident = c.sb([128, 128], BF16, "ident")
    c.dma("sp", ident[:], io["identb"][:], reads=[io["identb"]], writes=[ident])

    KT = [c.sb([128, NK], BF16, f"KT{i}") for i in range(2)]
    VV = [c.sb([128, NKC, 128], BF16, f"VV{i}") for i in range(2)]
    QT = [c.sb([128, TT], BF16, f"QT{i}") for i in range(2)]
    QP = [c.sb([64, TT], BF16, f"QP{i}") for i in range(2)]
    kpe = c.sb([64, NK], BF16, "kpe")
    pts = [c.sb([128, 512], BF16, f"pt{i}") for i in range(4)]
    S_ps = [c.ps([128, 512], F32, f"S{i}") for i in range(3)]
    acc = c.ps([128, 512], F32, "acc")
    den = c.ps([128, 512], F32, "den")
    rden = c.sb([128, 512], F32, "rden")
    ost = [c.sb([128, 512], BF16, f"ost{i}") for i in range(2)]
    maskB = c.sb([128, NKC, TT], BF16, "maskB")
    st = {"s": 0, "p": 0, "o": 0, "b": 0}

    def attention_head(h, mla):
        b = st["b"] % 2; st["b"] += 1
        kt, vv, qt_, qp = KT[b], VV[b], QT[b], QP[b]
        koff, voff = (KA_OFF, VA_OFF) if mla else (KB_OFF, VB_OFF)
        ap_, gt = gk(koff + 128 * h, 128)
        c.dma("sp", kt[:, :].rearrange("p (r t) -> p r t", r=4), ap_, reads=[gt], writes=[kt])
        for r in range(4):
            for cc_ in range(2):
                gt = io["gath%d" % (voff // CH + cc_)]
                c.dma("sp", vv[:, 8 * r + 4 * cc_:8 * r + 4 * cc_ + 4, :],
                      gt.ap[r * CH:(r + 1) * CH, 128 * h:128 * (h + 1)].rearrange("(kc p) d -> p kc d", p=128), reads=[gt], writes=[vv])
        if mla:
            c.dma("sp", qt_[:], io["qaT"][h, 0:128, :], reads=[io["qaT"]], writes=[qt_])
            c.dma("sp", qp[:], io["qaT"][h, 128:192, :], reads=[io["qaT"]], writes=[qp])
            scale = 192.0 ** -0.5
        else:
            c.dma("sp", qt_[:], io["qbT"][h, :, :], reads=[io["qbT"]], writes=[qt_])
            scale = 128.0 ** -0.5
        for qh in range(2):
            qs = slice(qh * 512, (qh + 1) * 512)
            ptq = {}

            def front(kc):
                ks = slice(kc * 128, (kc + 1) * 128)
                S = S_ps[st["s"] % 3]; st["s"] += 1
                pt = pts[st["p"] % 4]; st["p"] += 1
                ptq[kc] = pt
                c.op("pe", lambda e: e.matmul(S[:], lhsT=kt[:, ks], rhs=qt_[:, qs], start=True, stop=not mla), reads=[kt, qt_], writes=[S])
                if mla:
                    c.op("pe", lambda e: e.matmul(S[:], lhsT=kpe[0:64, ks], rhs=qp[0:64, qs], start=False, stop=True), reads=[kpe, qp], writes=[S])
                c.op("act", lambda e: e.activation(out=pt[:], in_=S[:], func=AF.Exp, scale=scale), reads=[S], writes=[pt])
                if mla:
                    c.op("dve", lambda e: e.scalar_tensor_tensor(out=pt[:], in0=posq_bc[:, qs], scalar=posk_col[:, kc:kc + 1], in1=pt[:],
                                                                 op0=ALU.is_ge, op1=ALU.mult), reads=[posq_bc, posk_col, pt], writes=[pt])
                else:
                    c.op("dve", lambda e: e.tensor_tensor(out=pt[:], in0=pt[:], in1=maskB[:, kc, qs], op=ALU.mult), reads=[pt, maskB], writes=[pt])

            def back(kc):
                pt = ptq.pop(kc)
                c.op("pe", lambda e: e.matmul(acc[:], lhsT=vv[:, kc, :], rhs=pt[:], start=(kc == 0), stop=(kc == NKC - 1)), reads=[vv, pt], writes=[acc])
                c.op("pe", lambda e: e.matmul(den[:], lhsT=ones[:], rhs=pt[:], start=(kc == 0), stop=(kc == NKC - 1)), reads=[ones, pt], writes=[den])

            for step in range(NKC + LA):
                if step < NKC:
                    front(step)
                if step >= LA:
                    back(step - LA)
            c.op("dve", lambda e: e.reciprocal(out=rden[:], in_=den[:]), reads=[den], writes=[rden])
            o = ost[st["o"] % 2]; st["o"] += 1
            c.op("dve", lambda e: e.tensor_tensor(out=o[:], in0=acc[:], in1=rden[:], op=ALU.mult), reads=[acc, rden], writes=[o])
            dst = io["oaT"] if mla else io["obT"]
            c.dma("sp", dst[128 * h:128 * (h + 1), qs], o[:], reads=[o], writes=[dst])

    ki2 = c.sb([128, NK], BF16, "ki2")
    ap_, gt = gk(KI_OFF, 64)
    c.dma("sp", ki2[0:64, :].rearrange("p (r t) -> p r t", r=4), ap_, reads=[gt], writes=[ki2])
    c.dma("sp", ki2[64:128, :].rearrange("p (r t) -> p r t", r=4), ap_, reads=[gt], writes=[ki2])
    qi = c.sb([128, 8, TT], BF16, "qi")
    c.dma("sp", qi[:], io["qiT"].ap.rearrange("g p t -> p g t"), reads=[io["qiT"]], writes=[qi])
    wcol = c.sb([128, 8, 16], F32, "wcol")
    c.dma("sp", wcol[:], io["widx"].ap.rearrange("(qb p) h -> p qb h", p=128), reads=[io["widx"]], writes=[wcol])
    junk = c.sb([128, NK], BF16, "junk")
    rb = [c.sb([128, 512], F32, f"rb{i}") for i in range(3)]
    I_ps = [c.ps([128, 512], F32, "I0"), c.ps([128, 512], F32, "I1")]
    tp = c.ps([128, 512], BF16, "tp")
    sm = {k: c.sb([128, 1], F32, "sm_" + k) for k in ("lo", "hi", "mid", "cnt", "ge", "d1", "d2", "am")}
    ri = {"r": 0, "i": 0}

    def indexer(qb):
        qs = slice(qb * 128, (qb + 1) * 128)
        for kt_ in range(NK // 512):
            ksl = slice(kt_ * 512, (kt_ + 1) * 512)
            for h in range(16):
                pb = 64 * (h % 2)
                ip = I_ps[ri["i"] % 2]; ri["i"] += 1
                r = rb[ri["r"] % 3]; ri["r"] += 1
                c.op("pe", lambda e: e.matmul(ip[:], lhsT=qi[pb:pb + 64, h // 2, qs], rhs=ki2[pb:pb + 64, ksl], start=True, stop=True),
                     reads=[qi, ki2], writes=[ip])
                c.op("act", lambda e: e.activation(out=r[:], in_=ip[:], func=AF.Relu), reads=[ip], writes=[r])
                if h == 0:
                    c.op("dve", lambda e: e.tensor_scalar(out=sc[:, ksl], in0=r[:], scalar1=wcol[:, qb, 0:1], scalar2=None, op0=ALU.mult),
                         reads=[r, wcol], writes=[sc])
                else:
                    c.op("dve", lambda e: e.scalar_tensor_tensor(out=sc[:, ksl], in0=r[:], scalar=wcol[:, qb, h:h + 1], in1=sc[:, ksl],
                                                                 op0=ALU.mult, op1=ALU.add), reads=[r, wcol, sc], writes=[sc])
        c.op("dve", lambda e: e.tensor_reduce(out=sm["am"][:], in_=sc[:], axis=AX.X, op=ALU.max, apply_absolute_value=True),
             reads=[sc], writes=[sm["am"]])
        c.op("dve", lambda e: e.tensor_scalar(out=sm["hi"][:], in0=sm["am"][:], scalar1=1.001, scalar2=1e-6, op0=ALU.mult, op1=ALU.add),
             reads=[sm["am"]], writes=[sm["hi"]])
        c.op("dve", lambda e: e.tensor_scalar(out=sm["lo"][:], in0=sm["hi"][:], scalar1=-1.0, scalar2=None, op0=ALU.mult),
             reads=[sm["hi"]], writes=[sm["lo"]])
        c.op("pool", lambda e: e.tensor_scalar(out=junk[:], in0=posk_bc[:], scalar1=posq_col[:, qb:qb + 1], scalar2=-1e30,
                                               op0=ALU.is_gt, op1=ALU.mult), reads=[posk_bc, posq_col], writes=[junk])
        c.op("pool", lambda e: e.tensor_tensor(out=sc[:], in0=sc[:], in1=junk[:], op=ALU.add), reads=[sc, junk], writes=[sc])
        for it in range(NIT):
            c.op("dve", lambda e: e.tensor_tensor(out=sm["mid"][:], in0=sm["lo"][:], in1=sm["hi"][:], op=ALU.add),
                 reads=[sm["lo"], sm["hi"]], writes=[sm["mid"]])
            c.op("dve", lambda e: e.tensor_scalar(out=sm["mid"][:], in0=sm["mid"][:], scalar1=0.5, scalar2=None, op0=ALU.mult),
                 reads=[sm["mid"]], writes=[sm["mid"]])
            c.op("dve", lambda e: e.tensor_scalar(out=junk[:], in0=sc[:], scalar1=sm["mid"][:, 0:1], scalar2=0.0, op0=ALU.is_ge, op1=ALU.add,
                                                  accum_out=sm["cnt"][:]), reads=[sc, sm["mid"]], writes=[junk, sm["cnt"]])
            c.op("dve", lambda e: e.tensor_scalar(out=sm["ge"][:], in0=sm["cnt"][:], scalar1=TOPK - 0.5, scalar2=None, op0=ALU.is_ge),
                 reads=[sm["cnt"]], writes=[sm["ge"]])
            c.op("dve", lambda e: e.tensor_tensor(out=sm["d1"][:], in0=sm["mid"][:], in1=sm["lo"][:], op=ALU.subtract),
                 reads=[sm["mid"], sm["lo"]], writes=[sm["d1"]])
            c.op("dve", lambda e: e.tensor_tensor(out=sm["d2"][:], in0=sm["hi"][:], in1=sm["mid"][:], op=ALU.subtract),
                 reads=[sm["hi"], sm["mid"]], writes=[sm["d2"]])
            c.op("dve", lambda e: e.scalar_tensor_tensor(out=sm["lo"][:], in0=sm["d1"][:], scalar=sm["ge"][:, 0:1], in1=sm["lo"][:],
                                                         op0=ALU.mult, op1=ALU.add), reads=[sm["d1"], sm["ge"], sm["lo"]], writes=[sm["lo"]])
            c.op("dve", lambda e: e.scalar_tensor_tensor(out=sm["hi"][:], in0=sm["d2"][:], scalar=sm["ge"][:, 0:1], in1=sm["mid"][:],
                                                         op0=ALU.mult, op1=ALU.add), reads=[sm["d2"], sm["ge"], sm["mid"]], writes=[sm["hi"]])
        c.op("dve", lambda e: e.tensor_scalar(out=junk[:], in0=sc[:], scalar1=sm["lo"][:, 0:1], scalar2=None, op0=ALU.is_ge),
             reads=[sc, sm["lo"]], writes=[junk])
        for g in range(NKC // 4):
            for j in range(4):
                kc = 4 * g + j
                c.op("pe", lambda e: e.transpose(out=tp[:, j * 128:(j + 1) * 128], in_=junk[:, kc * 128:(kc + 1) * 128], identity=ident[:]),
                     reads=[junk, ident], writes=[tp])
            c.op("act", lambda e: e.activation(out=maskB[:, 4 * g:4 * g + 4, qs], in_=tp[:, :].rearrange("p (j q) -> p j q", j=4), func=AF.Copy),
                 reads=[tp], writes=[maskB])

    ap_, gt = gk(KPE_OFF, 64)
    c.dma("sp", kpe[:, :].rearrange("p (r t) -> p r t", r=4), ap_, reads=[gt], writes=[kpe])
    for i in range(8):
        indexer(i)
        attention_head(i, True)
    for h in range(8):
        attention_head(h, False)


def emit_ln(c, r, g_ap, b_ap, out_t, ps, onesf):
    nc = c.nc
    gcol = c.sb([128, 16], F32, "ln_g"); bcol = c.sb([128, 16], F32, "ln_b")
    with nc.allow_non_contiguous_dma(reason="tiny ln vectors"):
        c.dma("sp", gcol[:], g_ap.rearrange("(c p) -> p c", p=128), reads=[], writes=[gcol])
        c.dma("sp", bcol[:], b_ap.rearrange("(c p) -> p c", p=128), reads=[], writes=[bcol])
    tmp = c.sb([128, TT], F32, "ln_tmp")
    mean = c.sb([128, TT], F32, "ln_mean"); rstd = c.sb([128, TT], F32, "ln_rstd")
    for th in range(2):
        ts_ = slice(th * 512, (th + 1) * 512)
        for m in range(16):
            c.op("pe", lambda e: e.matmul(ps[th][:], lhsT=onesf[:, :], rhs=r[:, m, ts_], start=(m == 0), stop=(m == 15)),
                 reads=[onesf, r], writes=[ps[th]])
    for m in range(16):
        c.op("act", lambda e: e.activation(out=tmp[:, :], in_=r[:, m, :], func=AF.Square), reads=[r], writes=[tmp])
        for th in range(2):
            ts_ = slice(th * 512, (th + 1) * 512)
            c.op("pe", lambda e: e.matmul(ps[2 + th][:], lhsT=onesf[:, :], rhs=tmp[:, ts_], start=(m == 0), stop=(m == 15)),
                 reads=[onesf, tmp], writes=[ps[2 + th]])
    for th in range(2):
        ts_ = slice(th * 512, (th + 1) * 512)
        c.op("act", lambda e: e.activation(out=mean[:, ts_], in_=ps[th][:], func=AF.Copy, scale=1.0 / D), reads=[ps[th]], writes=[mean])
        c.op("dve", lambda e: e.tensor_tensor(out=tmp[:, ts_], in0=mean[:, ts_], in1=mean[:, ts_], op=ALU.mult), reads=[mean], writes=[tmp])
        c.op("dve", lambda e: e.scalar_tensor_tensor(out=rstd[:, ts_], in0=ps[2 + th][:], scalar=1.0 / D, in1=tmp[:, ts_], op0=ALU.mult, op1=ALU.subtract),
             reads=[ps[2 + th], tmp], writes=[rstd])
    c.op("dve", lambda e: e.tensor_scalar(out=rstd[:], in0=rstd[:], scalar1=1e-5, scalar2=None, op0=ALU.add), reads=[rstd], writes=[rstd])
    c.op("act", lambda e: e.activation(out=rstd[:], in_=rstd[:], func=AF.Sqrt), reads=[rstd], writes=[rstd])
    c.op("dve", lambda e: e.reciprocal(out=rstd[:], in_=rstd[:]), reads=[rstd], writes=[rstd])
    for m in range(16):
        c.op("dve", lambda e: e.tensor_tensor(out=r[:, m, :], in0=r[:, m, :], in1=mean[:], op=ALU.subtract), reads=[r, mean], writes=[r])
        c.op("pool", lambda e: e.tensor_tensor(out=r[:, m, :], in0=r[:, m, :], in1=rstd[:], op=ALU.mult), reads=[r, rstd], writes=[r])
        c.op("dve", lambda e: e.tensor_scalar(out=r[:, m, :], in0=r[:, m, :], scalar1=gcol[:, m:m + 1], scalar2=bcol[:, m:m + 1],
                                              op0=ALU.mult, op1=ALU.add), reads=[r, gcol, bcol], writes=[r])
    for i in range(4):
        c.dma("sp", out_t.ap[512 * i:512 * (i + 1), :].rearrange("(kc p) t -> p kc t", p=128), r[:, 4 * i:4 * i + 4, :], reads=[r], writes=[out_t])


def emit_B2(c, io, l, hin, hout):
    nc = c.nc
    win = io["w_in"].ap[l]; woa = io["w_o_a"].ap[l]; wob = io["w_o_b"].ap[l]; wout = io["w_out"].ap[l]
    hTb = c.sb([128, 16, TT], BF16, "hTb"); r = c.sb([128, 16, TT], F32, "r")
    for i in range(4):
        c.dma("pool", hTb[:, 4 * i:4 * i + 4, :], hin.ap[512 * i:512 * (i + 1), :].rearrange("(kc p) t -> p kc t", p=128), reads=[hin], writes=[hTb])
        c.dma("sp", r[:, 4 * i:4 * i + 4, :], hin.ap[512 * i:512 * (i + 1), :].rearrange("(kc p) t -> p kc t", p=128), reads=[hin], writes=[r])
    oa = c.sb([128, 8, TT], BF16, "oa"); ob = c.sb([128, 8, TT], BF16, "ob")
    c.dma("sp", oa[:], io["oaT"].ap.rearrange("(kc p) t -> p kc t", p=128), reads=[io["oaT"]], writes=[oa])
    c.dma("sp", ob[:], io["obT"].ap.rearrange("(kc p) t -> p kc t", p=128), reads=[io["obT"]], writes=[ob])
    yT = c.sb([128, 16, TT], BF16, "yT")
    onesf = c.sb([128, 128], F32, "onesf")
    c.op("pool", lambda e: e.memset(onesf[:], 1.0), writes=[onesf])
    wg = [c.sb([128, 16, 256], BF16, f"wg{i}") for i in range(2)]
    wo = [c.sb([128, 8, 256], BF16, f"wo{i}") for i in range(2)]
    ps = [c.ps([128, 512], F32, f"ps{i}") for i in range(8)]
    sg = [c.sb([128, 512], F32, f"sg{i}") for i in range(2)]
    ta = c.sb([128, 512], F32, "ta"); tb = c.sb([128, 512], F32, "tb")
    k = 0
    for grp in range(8):
        c.dma("pool", wg[0][:], win[:, C_GA + 256 * grp:C_GA + 256 * (grp + 1)].rearrange("(kc p) m -> p kc m", p=128), reads=[], writes=[wg[0]])
        c.dma("pool", wg[1][:], win[:, C_GB + 256 * grp:C_GB + 256 * (grp + 1)].rearrange("(kc p) m -> p kc m", p=128), reads=[], writes=[wg[1]])
        c.dma("pool", wo[0][:], woa[:, 256 * grp:256 * (grp + 1)].rearrange("(kc p) m -> p kc m", p=128), reads=[], writes=[wo[0]])
        c.dma("pool", wo[1][:], wob[:, 256 * grp:256 * (grp + 1)].rearrange("(kc p) m -> p kc m", p=128), reads=[], writes=[wo[1]])
        for j in range(2):
            m = 2 * grp + j
            ms = slice(128 * j, 128 * (j + 1))
            for th in range(2):
                ts_ = slice(th * 512, (th + 1) * 512)
                p4 = ps[4 * (k % 2):4 * (k % 2) + 4]; k += 1
                for kc in range(16):
                    c.op("pe", lambda e: e.matmul(p4[0][:], lhsT=wg[0][:, kc, ms], rhs=hTb[:, kc, ts_], start=(kc == 0), stop=(kc == 15)), reads=[wg[0], hTb], writes=[p4[0]])
                for kc in range(16):
                    c.op("pe", lambda e: e.matmul(p4[1][:], lhsT=wg[1][:, kc, ms], rhs=hTb[:, kc, ts_], start=(kc == 0), stop=(kc == 15)), reads=[wg[1], hTb], writes=[p4[1]])
                for kc in range(8):
                    c.op("pe", lambda e: e.matmul(p4[2][:], lhsT=wo[0][:, kc, ms], rhs=oa[:, kc, ts_], start=(kc == 0), stop=(kc == 7)), reads=[wo[0], oa], writes=[p4[2]])
                for kc in range(8):
                    c.op("pe", lambda e: e.matmul(p4[3][:], lhsT=wo[1][:, kc, ms], rhs=ob[:, kc, ts_], start=(kc == 0), stop=(kc == 7)), reads=[wo[1], ob], writes=[p4[3]])
                c.op("act", lambda e: e.activation(out=sg[0][:], in_=p4[0][:], func=AF.Sigmoid), reads=[p4[0]], writes=[sg[0]])
                c.op("act", lambda e: e.activation(out=sg[1][:], in_=p4[1][:], func=AF.Sigmoid), reads=[p4[1]], writes=[sg[1]])
                c.op("dve", lambda e: e.tensor_tensor(out=ta[:], in0=p4[2][:], in1=sg[0][:], op=ALU.mult), reads=[p4[2], sg[0]], writes=[ta])
                c.op("dve", lambda e: e.tensor_tensor(out=tb[:], in0=p4[3][:], in1=sg[1][:], op=ALU.mult), reads=[p4[3], sg[1]], writes=[tb])
                c.op("pool", lambda e: e.tensor_tensor(out=yT[:, m, ts_], in0=ta[:], in1=tb[:], op=ALU.add), reads=[ta, tb], writes=[yT])
    for grp in range(8):
        w = wg[grp % 2]
        c.dma("pool", w[:], wout[:, 256 * grp:256 * (grp + 1)].rearrange("(kc p) m -> p kc m", p=128), reads=[], writes=[w])
        for j in range(2):
            m = 2 * grp + j
            ms = slice(128 * j, 128 * (j + 1))
            for th in range(2):
                ts_ = slice(th * 512, (th + 1) * 512)
                p = ps[k % 8]; k += 1
                for kc in range(16):
                    c.op("pe", lambda e: e.matmul(p[:], lhsT=w[:, kc, ms], rhs=yT[:, kc, ts_], start=(kc == 0), stop=(kc == 15)), reads=[w, yT], writes=[p])
                c.op("dve", lambda e: e.scalar_tensor_tensor(out=r[:, m, ts_], in0=r[:, m, ts_], scalar=ALPHA, in1=p[:], op0=ALU.mult, op1=ALU.add),
                     reads=[r, p], writes=[r])
    emit_ln(c, r, io["ln1_g"].ap[l], io["ln1_b"].ap[l], hout, ps, onesf)


def emit_router(c, io, l, r, g_ps):
    nc = c.nc
    wr = c.sb([128, 16, 72], F32, "wr"); bias = c.sb([128, 72], F32, "bias")
    with nc.allow_non_contiguous_dma(reason="small router weights"):
        c.dma("sp", wr[:, :, 0:8], io["w_group"].ap[l].rearrange("(kc p) m -> p kc m", p=128), reads=[], writes=[wr])
        c.dma("sp", wr[:, :, 8:72], io["w_router"].ap[l].rearrange("(kc p) m -> p kc m", p=128), reads=[], writes=[wr])
    c.dma("sp", bias[:, 0:8], io["b_group"].ap[l].partition_broadcast(128), reads=[], writes=[bias])
    c.dma("sp", bias[:, 8:72], io["b_router"].ap[l].partition_broadcast(128), reads=[], writes=[bias])
    CW = c.sb([128, 8, 64], F32, "CW")
    GM = c.sb([128, 8, 8], F32, "GM")
    lg = c.sb([128, 72], F32, "lg"); em = c.sb([128, 64], F32, "em"); em2 = c.sb([128, 64], F32, "em2")
    mk1 = c.sb([128, 64], F32, "mk1"); mk2 = c.sb([128, 64], F32, "mk2"); gex = c.sb([128, 8], F32, "gex")
    s = {k: c.sb([128, 1], F32, "r_" + k) for k in ("gmax", "ngmax", "gsum", "gp", "m1", "m2", "d", "ex", "w1", "w2")}
    pen = c.sb([128, 8], F32, "pen")
    for tt in range(8):
        tsl = slice(tt * 128, (tt + 1) * 128)
        gmask = GM[:, tt, :]
        for kc in range(16):
            c.op("pe", lambda e: e.matmul(g_ps[:, 0:72], lhsT=r[:, kc, tsl], rhs=wr[:, kc, :], start=(kc == 0), stop=(kc == 15)), reads=[r, wr], writes=[g_ps])
        c.op("dve", lambda e: e.tensor_tensor(out=lg[:], in0=g_ps[:, 0:72], in1=bias[:], op=ALU.add), reads=[g_ps, bias], writes=[lg])
        c.op("dve", lambda e: e.tensor_reduce(out=s["gmax"][:], in_=lg[:, 0:8], axis=AX.X, op=ALU.max), reads=[lg], writes=[s["gmax"]])
        c.op("dve", lambda e: e.tensor_scalar(out=gmask, in0=lg[:, 0:8], scalar1=s["gmax"][:, 0:1], scalar2=None, op0=ALU.is_ge), reads=[lg, s["gmax"]], writes=[GM])
        c.op("dve", lambda e: e.tensor_scalar(out=s["ngmax"][:], in0=s["gmax"][:], scalar1=-1.0, scalar2=None, op0=ALU.mult), reads=[s["gmax"]], writes=[s["ngmax"]])
        c.op("act", lambda e: e.activation(out=gex[:], in_=lg[:, 0:8], func=AF.Exp, bias=s["ngmax"][:, 0:1], accum_out=s["gsum"][:]),
             reads=[lg, s["ngmax"]], writes=[gex, s["gsum"]])
        c.op("dve", lambda e: e.reciprocal(out=s["gp"][:], in_=s["gsum"][:]), reads=[s["gsum"]], writes=[s["gp"]])
        c.op("dve", lambda e: e.tensor_scalar(out=pen[:], in0=gmask, scalar1=-1.0, scalar2=1e30, op0=ALU.add, op1=ALU.mult), reads=[GM], writes=[pen])
        for g in range(8):
            c.op("dve", lambda e: e.tensor_scalar(out=em[:, 8 * g:8 * g + 8], in0=lg[:, 8 + 8 * g:16 + 8 * g], scalar1=pen[:, g:g + 1], scalar2=None, op0=ALU.add),
                 reads=[lg, pen], writes=[em])
        c.op("dve", lambda e: e.tensor_reduce(out=s["m1"][:], in_=em[:], axis=AX.X, op=ALU.max), reads=[em], writes=[s["m1"]])
        c.op("dve", lambda e: e.tensor_scalar(out=mk1[:], in0=em[:], scalar1=s["m1"][:, 0:1], scalar2=None, op0=ALU.is_ge), reads=[em, s["m1"]], writes=[mk1])
        c.op("dve", lambda e: e.scalar_tensor_tensor(out=em2[:], in0=mk1[:], scalar=-1e30, in1=em[:], op0=ALU.mult, op1=ALU.add), reads=[mk1, em], writes=[em2])
        c.op("dve", lambda e: e.tensor_reduce(out=s["m2"][:], in_=em2[:], axis=AX.X, op=ALU.max), reads=[em2], writes=[s["m2"]])
        c.op("dve", lambda e: e.tensor_scalar(out=mk2[:], in0=em2[:], scalar1=s["m2"][:, 0:1], scalar2=None, op0=ALU.is_ge), reads=[em2, s["m2"]], writes=[mk2])
        c.op("dve", lambda e: e.tensor_tensor(out=s["d"][:], in0=s["m2"][:], in1=s["m1"][:], op=ALU.subtract), reads=[s["m1"], s["m2"]], writes=[s["d"]])
        c.op("act", lambda e: e.activation(out=s["ex"][:], in_=s["d"][:], func=AF.Exp), reads=[s["d"]], writes=[s["ex"]])
        c.op("dve", lambda e: e.tensor_scalar(out=s["ex"][:], in0=s["ex"][:], scalar1=1.0, scalar2=None, op0=ALU.add), reads=[s["ex"]], writes=[s["ex"]])
        c.op("dve", lambda e: e.reciprocal(out=s["ex"][:], in_=s["ex"][:]), reads=[s["ex"]], writes=[s["ex"]])
        c.op("dve", lambda e: e.tensor_tensor(out=s["w1"][:], in0=s["gp"][:], in1=s["ex"][:], op=ALU.mult), reads=[s["gp"], s["ex"]], writes=[s["w1"]])
        c.op("dve", lambda e: e.tensor_tensor(out=s["w2"][:], in0=s["gp"][:], in1=s["w1"][:], op=ALU.subtract), reads=[s["gp"], s["w1"]], writes=[s["w2"]])
        c.op("dve", lambda e: e.tensor_scalar(out=mk1[:], in0=mk1[:], scalar1=s["w1"][:, 0:1], scalar2=None, op0=ALU.mult), reads=[mk1, s["w1"]], writes=[mk1])
        c.op("dve", lambda e: e.scalar_tensor_tensor(out=CW[:, tt, :], in0=mk2[:], scalar=s["w2"][:, 0:1], in1=mk1[:], op0=ALU.mult, op1=ALU.add),
             reads=[mk2, s["w2"], mk1], writes=[CW])
    return CW, GM


def emit_B3_dense(c, io, l, hin, hout, n_tt=8, n_e=64):
    nc = c.nc
    hTb = c.sb([128, 16, TT], BF16, "hTb"); r = c.sb([128, 16, TT], F32, "r")
    for i in range(4):
        c.dma("pool", hTb[:, 4 * i:4 * i + 4, :], hin.ap[512 * i:512 * (i + 1), :].rearrange("(kc p) t -> p kc t", p=128), reads=[hin], writes=[hTb])
        c.dma("sp", r[:, 4 * i:4 * i + 4, :], hin.ap[512 * i:512 * (i + 1), :].rearrange("(kc p) t -> p kc t", p=128), reads=[hin], writes=[r])
    identb = c.sb([128, 128], BF16, "identb"); identf = c.sb([128, 128], F32, "identf")
    c.dma("sp", identb[:], io["identb"][:], reads=[], writes=[identb])
    c.dma("sp", identf[:], io["identf"][:], reads=[], writes=[identf])
    onesf = c.sb([128, 128], F32, "onesf")
    c.op("pool", lambda e: e.memset(onesf[:], 1.0), writes=[onesf])
    y_ps = [c.ps([128, 512], F32, f"y{i}") for i in range(4)]
    g_ps = c.ps([128, 512], F32, "g"); u_ps = c.ps([128, 512], F32, "u")
    tp_ps = c.ps([128, 512], BF16, "tp")
    CW, GM = emit_router(c, io, l, r, g_ps)
    Wg = [c.sb([128, 16, 512], BF16, f"Wg{i}") for i in range(2)]
    Wu = [c.sb([128, 16, 512], BF16, f"Wu{i}") for i in range(1)] * 2
    Wd = [c.sb([128, 4, D], BF16, f"Wd{i}") for i in range(1)] * 2
    sg = c.sb([128, 512], F32, "sg"); hid = c.sb([128, 512], BF16, "hid"); hidT = c.sb([128, 4, 128], BF16, "hidT")
    ysb = c.sb([128, D], F32, "ysb")
    k = 0
    for tt in range(n_tt):
        tsl = slice(tt * 128, (tt + 1) * 128)
        for ex in range(n_e):
            wgb, wub, wdb = Wg[k % 2], Wu[k % 2], Wd[k % 2]; k += 1
            c.dma("pool", wgb[:], io["w_e_gate"].ap[l, ex].rearrange("(kc p) m -> p kc m", p=128), reads=[], writes=[wgb])
            c.dma("pool", wub[:], io["w_e_up"].ap[l, ex].rearrange("(kc p) m -> p kc m", p=128), reads=[], writes=[wub])
            c.dma("pool", wdb[:], io["w_e_down"].ap[l, ex].rearrange("(kc p) m -> p kc m", p=128), reads=[], writes=[wdb])
            for kc in range(16):
                c.op("pe", lambda e: e.matmul(g_ps[:], lhsT=hTb[:, kc, tsl], rhs=wgb[:, kc, :], start=(kc == 0), stop=(kc == 15)), reads=[hTb, wgb], writes=[g_ps])
            for kc in range(16):
                c.op("pe", lambda e: e.matmul(u_ps[:], lhsT=hTb[:, kc, tsl], rhs=wub[:, kc, :], start=(kc == 0), stop=(kc == 15)), reads=[hTb, wub], writes=[u_ps])
            c.op("act", lambda e: e.activation(out=sg[:], in_=g_ps[:], func=AF.Silu), reads=[g_ps], writes=[sg])
            c.op("dve", lambda e: e.scalar_tensor_tensor(out=hid[:], in0=sg[:], scalar=CW[:, tt, ex:ex + 1], in1=u_ps[:], op0=ALU.mult, op1=ALU.mult),
                 reads=[sg, CW, u_ps], writes=[hid])
            for fc in range(4):
                c.op("pe", lambda e: e.transpose(out=tp_ps[:, fc * 128:(fc + 1) * 128], in_=hid[:, fc * 128:(fc + 1) * 128], identity=identb[:]),
                     reads=[hid, identb], writes=[tp_ps])
            c.op("act", lambda e: e.activation(out=hidT[:, :, :], in_=tp_ps[:, :].rearrange("p (f t) -> p f t", f=4), func=AF.Copy), reads=[tp_ps], writes=[hidT])
            for n in range(4):
                for fc in range(4):
                    c.op("pe", lambda e: e.matmul(y_ps[n][:], lhsT=hidT[:, fc, :], rhs=wdb[:, fc, n * 512:(n + 1) * 512],
                                                  start=(ex == 0 and fc == 0), stop=(ex == n_e - 1 and fc == 3)), reads=[hidT, wdb], writes=[y_ps[n]])
        for n in range(4):
            c.op("act", lambda e: e.activation(out=ysb[:, n * 512:(n + 1) * 512], in_=y_ps[n][:], func=AF.Copy), reads=[y_ps[n]], writes=[ysb])
        for m in range(16):
            c.op("pe", lambda e: e.transpose(out=u_ps[:, 0:128], in_=ysb[:, m * 128:(m + 1) * 128], identity=identf[:]), reads=[ysb, identf], writes=[u_ps])
            c.op("dve", lambda e: e.scalar_tensor_tensor(out=r[:, m, tsl], in0=r[:, m, tsl], scalar=ALPHA, in1=u_ps[:, 0:128], op0=ALU.mult, op1=ALU.add),
                 reads=[r, u_ps], writes=[r])
    for tt in range(n_tt, 8):
        tsl = slice(tt * 128, (tt + 1) * 128)
        c.op("dve", lambda e: e.tensor_scalar(out=r[:, :, tsl], in0=r[:, :, tsl], scalar1=ALPHA, scalar2=None, op0=ALU.mult), reads=[r], writes=[r])
    emit_ln(c, r, io["ln2_g"].ap[l], io["ln2_b"].ap[l], hout, y_ps, onesf)


def emit_B3(c, io, l, hin, hout, n_e=64):
    nc = c.nc
    c.phase_begin()
    r = c.sb([128, 16, TT], F32, "r")
    for i in range(4):
        c.dma("sp", r[:, 4 * i:4 * i + 4, :], hin.ap[512 * i:512 * (i + 1), :].rearrange("(kc p) t -> p kc t", p=128), reads=[hin], writes=[r])
    g_ps = c.ps([128, 512], F32, "g")
    CW, GM = emit_router(c, io, l, r, g_ps)
    c.dma("sp", io["cwd"].ap.rearrange("(tt p) e -> p tt e", p=128), CW[:], reads=[CW], writes=[io["cwd"]])
    c.phase_end()
    c.phase_begin()
    hTb = c.sb([128, 16, TT], BF16, "hTb")
    for i in range(4):
        c.dma("pool", hTb[:, 4 * i:4 * i + 4, :], hin.ap[512 * i:512 * (i + 1), :].rearrange("(kc p) t -> p kc t", p=128), reads=[hin], writes=[hTb])
    CW = c.sb([128, 8, 64], F32, "CW2")
    c.dma("sp", CW[:], io["cwd"].ap.rearrange("(tt p) e -> p tt e", p=128), reads=[io["cwd"]], writes=[CW])
    identb = c.sb([128, 128], BF16, "identb")
    c.dma("sp", identb[:], io["identb"][:], reads=[], writes=[identb])
    y_ps = [c.ps([128, 512], F32, f"y{i}") for i in range(4)]
    g_ps = c.ps([128, 512], F32, "g"); u_ps = c.ps([128, 512], F32, "u")
    tp_ps = c.ps([128, 512], BF16, "tp")
    y_sb = [c.sb([128, D], F32, f"ysb{i}") for i in range(8)]
    Wg = [c.sb([128, 16, 512], BF16, f"Wg{i}") for i in range(2)]
    Wu = [c.sb([128, 16, 512], BF16, f"Wu{i}") for i in range(2)]
    Wd = [c.sb([128, 4, D], BF16, f"Wd{i}") for i in range(2)]
    sg = c.sb([128, 512], F32, "sg"); hid = c.sb([128, 512], BF16, "hid"); hidT = c.sb([128, 4, 128], BF16, "hidT")
    items = [(ex, tt) for ex in range(n_e) for tt in range(8)]

    def load_w(ex):
        c.dma("pool", Wg[ex % 2][:], io["w_e_gate"].ap[l, ex].rearrange("(kc p) m -> p kc m", p=128), reads=[], writes=[Wg[ex % 2]])
        c.dma("pool", Wu[ex % 2][:], io["w_e_up"].ap[l, ex].rearrange("(kc p) m -> p kc m", p=128), reads=[], writes=[Wu[ex % 2]])
        c.dma("pool", Wd[ex % 2][:], io["w_e_down"].ap[l, ex].rearrange("(kc p) m -> p kc m", p=128), reads=[], writes=[Wd[ex % 2]])

    def gate(i):
        ex, tt = items[i]; tsl = slice(tt * 128, (tt + 1) * 128); wgb = Wg[ex % 2]
        for kc in range(16):
            c.op("pe", lambda e: e.matmul(g_ps[:], lhsT=hTb[:, kc, tsl], rhs=wgb[:, kc, :], start=(kc == 0), stop=(kc == 15)), reads=[hTb, wgb], writes=[g_ps])
        c.op("act", lambda e: e.activation(out=sg[:], in_=g_ps[:], func=AF.Silu), reads=[g_ps], writes=[sg])

    def up(i):
        ex, tt = items[i]; tsl = slice(tt * 128, (tt + 1) * 128); wub = Wu[ex % 2]
        for kc in range(16):
            c.op("pe", lambda e: e.matmul(u_ps[:], lhsT=hTb[:, kc, tsl], rhs=wub[:, kc, :], start=(kc == 0), stop=(kc == 15)), reads=[hTb, wub], writes=[u_ps])
        c.op("dve", lambda e: e.scalar_tensor_tensor(out=hid[:], in0=sg[:], scalar=CW[:, tt, ex:ex + 1], in1=u_ps[:], op0=ALU.mult, op1=ALU.mult),
             reads=[sg, CW, u_ps], writes=[hid])

    def trans(i):
        for fc in range(4):
            c.op("pe", lambda e: e.transpose(out=tp_ps[:, fc * 128:(fc + 1) * 128], in_=hid[:, fc * 128:(fc + 1) * 128], identity=identb[:]),
                 reads=[hid, identb], writes=[tp_ps])
        c.op("act", lambda e: e.activation(out=hidT[:, :, :], in_=tp_ps[:, :].rearrange("p (f t) -> p f t", f=4), func=AF.Copy), reads=[tp_ps], writes=[hidT])

    def down(i):
        ex, tt = items[i]; wdb = Wd[ex % 2]
        for n in range(4):
            for fc in range(4):
                c.op("pe", lambda e: e.matmul(y_ps[n][:], lhsT=hidT[:, fc, :], rhs=wdb[:, fc, n * 512:(n + 1) * 512],
                                              start=(fc == 0), stop=(fc == 3)), reads=[hidT, wdb], writes=[y_ps[n]])
        for n in range(4):
            ns = slice(n * 512, (n + 1) * 512)
            if ex == 0:
                c.op("act", lambda e: e.activation(out=y_sb[tt][:, ns], in_=y_ps[n][:], func=AF.Copy), reads=[y_ps[n]], writes=[y_sb[tt]])
            else:
                c.op("dve", lambda e: e.tensor_tensor(out=y_sb[tt][:, ns], in0=y_sb[tt][:, ns], in1=y_ps[n][:], op=ALU.add),
                     reads=[y_sb[tt], y_ps[n]], writes=[y_sb[tt]])

    load_w(0)
    gate(0); up(0)
    for i in range(len(items)):
        ex, tt = items[i]
        if tt == 0 and ex + 1 < n_e:
            load_w(ex + 1)
        nxt = i + 1 < len(items)
        if nxt:
            gate(i + 1)
        trans(i)
        if nxt:
            up(i + 1)
        down(i)
    for tt in range(8):
        c.dma("sp", io["yd"].ap[tt * 128:(tt + 1) * 128, :], y_sb[tt][:], reads=[y_sb[tt]], writes=[io["yd"]])
    c.phase_end()
    c.phase_begin()
    r = c.sb([128, 16, TT], F32, "r")
    for i in range(4):
        c.dma("sp", r[:, 4 * i:4 * i + 4, :], hin.ap[512 * i:512 * (i + 1), :].rearrange("(kc p) t -> p kc t", p=128), reads=[hin], writes=[r])
    identf = c.sb([128, 128], F32, "identf")
    c.dma("sp", identf[:], io["identf"][:], reads=[], writes=[identf])
    onesf = c.sb([128, 128], F32, "onesf")
    c.op("pool", lambda e: e.memset(onesf[:], 1.0), writes=[onesf])
    ps = [c.ps([128, 512], F32, f"ps{i}") for i in range(6)]
    yl = [c.sb([128, D], F32, f"yl{i}") for i in range(2)]
    k = 0
    for tt in range(8):
        tsl = slice(tt * 128, (tt + 1) * 128)
        y = yl[tt % 2]
        c.dma("sp", y[:], io["yd"].ap[tt * 128:(tt + 1) * 128, :], reads=[io["yd"]], writes=[y])
        for m in range(16):
            p = ps[4 + (k % 2)]; k += 1
            c.op("pe", lambda e: e.transpose(out=p[:, 0:128], in_=y[:, m * 128:(m + 1) * 128], identity=identf[:]), reads=[y, identf], writes=[p])
            c.op("dve", lambda e: e.scalar_tensor_tensor(out=r[:, m, tsl], in0=r[:, m, tsl], scalar=ALPHA, in1=p[:, 0:128], op0=ALU.mult, op1=ALU.add),
                 reads=[r, p], writes=[r])
    emit_ln(c, r, io["ln2_g"].ap[l], io["ln2_b"].ap[l], hout, ps, onesf)
    c.phase_end()

W_SPECS = (("w_in", [D, IN_W]), ("g_q", [512]), ("w_q_b", [512, 1536]), ("g_kv", [512]), ("w_kv_b", [512, 2048]),
           ("w_o_a", [1024, D]), ("w_o_b", [1024, D]), ("w_out", [D, D]), ("ln1_g", [D]), ("ln1_b", [D]),
           ("w_group", [D, 8]), ("b_group", [8]), ("w_router", [D, 64]), ("b_router", [64]),
           ("w_e_gate", [64, D, 512]), ("w_e_up", [64, D, 512]), ("w_e_down", [64, 512, D]), ("ln2_g", [D]), ("ln2_b", [D]))


def build_fused(depth=2, moe="dense", moe_kw=None, n_exp=64):
    c = Ctx(); nc = c.nc
    io = {}
    io["xT"] = c.dram("xT", [D, TT], F32, "ExternalInput")
    io["posq"] = c.dram("posq", [1, TT], I32, "ExternalInput")
    io["posk"] = c.dram("posk", [1, SEQ], I32, "ExternalInput")
    for nm, shp in W_SPECS:
        if nm.startswith("w_e_"):
            shp = [n_exp] + shp[1:]
        io[nm] = c.dram(nm, [depth] + shp, F32, "ExternalInput")
    io["inv"] = c.dram("inv", [128, 3], F32, "ExternalInput")
    io["perm"] = c.dram("perm", [3, 128, 128], F32, "ExternalInput")
    io["identb"] = c.dram("identb", [128, 128], BF16, "ExternalInput")
    io["identf"] = c.dram("identf", [128, 128], F32, "ExternalInput")
    io["outT"] = c.dram("outT", [D, TT], F32, "ExternalOutput")
    io["qaT"] = c.dram("qaT", [8, 192, TT], BF16); io["qbT"] = c.dram("qbT", [8, 128, TT], BF16); io["qiT"] = c.dram("qiT", [8, 128, TT], BF16)
    io["widx"] = c.dram("widx", [TT, 16], F32)
    io["kvpack"] = c.dram("kvpack", [KVROWS, TT], BF16)
    io["gath"] = None
    for k in range(NCH):
        rows = min(CH, KVROWS - k * CH)
        io["gath%d" % k] = c.dram("gath%d" % k, [4 * rows, TT], BF16)
    io["oaT"] = c.dram("oaT", [1024, TT], BF16); io["obT"] = c.dram("obT", [1024, TT], BF16)
    io["hA"] = c.dram("hA", [D, TT], F32); io["hB"] = c.dram("hB", [D, TT], F32)
    io["cwd"] = c.dram("cwd", [TT, 64], F32); io["yd"] = c.dram("yd", [TT, D], F32)
    hin = io["xT"]
    for l in range(depth):
        c.phase_begin(); emit_A(c, io, l, hin); c.phase_end()
        if UPTO < 1:
            break
        for k in range(NCH):
            rows = min(CH, KVROWS - k * CH)
            c.collective("AllGather", io["kvpack"], io["kvpack"].ap[k * CH:k * CH + rows, :], io["gath%d" % k], io["gath%d" % k].ap[:, :], GROUPS)
        if UPTO < 2:
            break
        c.phase_begin(); emit_B1(c, io); c.phase_end()
        if UPTO < 3:
            break
        c.phase_begin(); emit_B2(c, io, l, hin, io["hA"]); c.phase_end()
        if UPTO < 4:
            break
        hout = io["outT"] if l == depth - 1 else io["hB"]
        if moe == "dense":
            c.phase_begin()
            emit_B3_dense(c, io, l, io["hA"], hout, **(moe_kw or {}))
            c.phase_end()
        else:
            emit_B3(c, io, l, io["hA"], hout, **(moe_kw or {}))
        hin = hout
    c.finish()
    return nc


_CACHE = {}
MOE_MODE = "v2"
UPTO = 99


def kernel(x, positions, w_in, g_q_lora, w_q_b, g_kv_lora, w_kv_b, w_o_a, w_o_b, w_out, ln1_g, ln1_b,
           w_group, b_group, w_router, b_router, w_e_gate, w_e_up, w_e_down, ln2_g, ln2_b):
    x = np.asarray(x); positions = np.asarray(positions)
    depth = int(np.asarray(w_in).shape[0])
    key = ("fused", depth, MOE_MODE)
    if key not in _CACHE:
        _CACHE[key] = build_fused(depth, MOE_MODE)
    nc = _CACHE[key]
    inv, perm = _rope_consts()
    identf = np.eye(128, dtype=np.float32); identb = identf.astype(NPBF)
    wts = {"w_in": w_in, "g_q": g_q_lora, "w_q_b": w_q_b, "g_kv": g_kv_lora, "w_kv_b": w_kv_b, "w_o_a": w_o_a, "w_o_b": w_o_b,
           "w_out": w_out, "ln1_g": ln1_g, "ln1_b": ln1_b, "w_group": w_group, "b_group": b_group, "w_router": w_router,
           "b_router": b_router, "w_e_gate": w_e_gate, "w_e_up": w_e_up, "w_e_down": w_e_down, "ln2_g": ln2_g, "ln2_b": ln2_b}
    wts = {k: np.ascontiguousarray(np.asarray(v), dtype=np.float32) for k, v in wts.items()}
    nb = x.shape[0]
    cores = [(b, j) for b in range(nb) for j in range(4)]
    in_maps = []
    for (b, j) in cores:
        m = dict(wts)
        m["xT"] = np.ascontiguousarray(x[b, j * TT:(j + 1) * TT, :].T)
        m["posq"] = np.ascontiguousarray(positions[b, j * TT:(j + 1) * TT][None, :]).astype(np.int32)
        m["posk"] = np.ascontiguousarray(positions[b][None, :]).astype(np.int32)
        m["inv"] = inv; m["perm"] = perm; m["identb"] = identb; m["identf"] = identf
        in_maps.append(m)
    res = run_bass_kernel_spmd(nc, in_maps, core_ids=list(range(len(cores)))).results
    out = np.empty(x.shape, np.float32)
    for ci, (b, j) in enumerate(cores):
        out[b, j * TT:(j + 1) * TT, :] = res[ci]["outT"].T
    return out
```

```python
import math
import numpy as np
from contextlib import ExitStack
import ml_dtypes
import concourse.bass as bass
import concourse.mybir as mybir
from concourse.bass_utils import run_bass_kernel_spmd

F32 = mybir.dt.float32; BF16 = mybir.dt.bfloat16; I32 = mybir.dt.int32
AF = mybir.ActivationFunctionType; ALU = mybir.AluOpType; AX = mybir.AxisListType
NPBF = ml_dtypes.bfloat16

D = 2048; TT = 1024; NCORE = 8; SEQ = 4096
IN_W = 9360
C_KPE = 1024; C_QB = 1088; C_KB = 2112; C_VB = 3136; C_QI = 4160; C_KI = 5184; C_WI = 5248; C_GA = 5264; C_GB = 7312
ALPHA = 4 ** 0.25
THETA = 500000.0


class T:
    __slots__ = ("ap", "w", "r", "name", "excl")

    def __init__(self, ap, name="", excl=False):
        self.ap = ap; self.w = None; self.r = {}; self.name = name; self.excl = excl

    def __getitem__(self, k):
        return self.ap[k]


class E:
    def __init__(self, name, obj, sem):
        self.name = name; self.obj = obj; self.sem = sem; self.count = 0; self.seen = {}


class Ctx:
    NDMASEM = 8

    def __init__(self):
        self.nc = nc = bass.Bass("TRN2", target_bir_lowering=False)
        self.es = ExitStack()
        self.eng = {}
        for name, obj in (("pe", nc.tensor), ("act", nc.scalar), ("dve", nc.vector), ("pool", nc.gpsimd), ("sp", nc.sync)):
            sem = self.es.enter_context(nc.semaphore("s_" + name))
            self.eng[name] = E(name, obj, sem)
        self.dq = {}
        for q in ("sp", "pool", "act"):
            lst = []
            for i in range(self.NDMASEM):
                sem = self.es.enter_context(nc.semaphore(f"d_{q}{i}"))
                lst.append(E(f"d_{q}{i}", None, sem))
            self.dq[q] = [lst, 0]
        self.cc = E("cc", None, self.es.enter_context(nc.semaphore("cc_sem")))
        self.pes = None
        self.ntile = 0
        self.rr = 0

    def sb(self, shape, dt, name=None):
        self.ntile += 1
        name = f"sb_{name or 't'}_{self.ntile}"
        h = (self.pes or self.es).enter_context(self.nc.sbuf_tensor(name, list(shape), dt))
        return T(h, name)

    def ps(self, shape, dt=F32, name=None):
        self.ntile += 1
        name = f"ps_{name or 'p'}_{self.ntile}"
        h = (self.pes or self.es).enter_context(self.nc.psum_tensor(name, list(shape), dt))
        return T(h, name, excl=True)

    def dram(self, name, shape, dt, kind="Internal"):
        h = self.nc.dram_tensor(name, list(shape), dt, kind=kind)
        return T(h.ap(), name)

    def _deps(self, e, reads, writes, extra=()):
        deps = {}

        def add(d):
            if d is None:
                return
            p, idx = d
            if p is e and e.name in ("pe", "sp"):
                return
            if deps.get(p, 0) < idx:
                deps[p] = idx
        for t in reads:
            add(t.w)
            if t.excl:
                for p, idx in t.r.items():
                    add((p, idx))
        for t in writes:
            add(t.w)
            for p, idx in t.r.items():
                add((p, idx))
        for d in extra:
            add(d)
        for p, idx in deps.items():
            if e.seen.get(p, 0) < idx:
                e.obj.wait_ge(p.sem, idx)
                e.seen[p] = idx

    def op(self, en, fn, reads=(), writes=()):
        e = self.eng[en]
        self._deps(e, reads, writes)
        inst = fn(e.obj)
        e.count += 1
        inst.then_inc(e.sem, 1)
        for t in writes:
            t.w = (e, e.count); t.r = {}
        for t in reads:
            t.r[e] = e.count
        return inst

    def dma(self, q, out, in_, reads=(), writes=(), **kw):
        e = self.eng[q]
        lst, i = self.dq[q]
        d = lst[i % len(lst)]; self.dq[q][1] = i + 1
        extra = [(d, d.count)] if d.count else []
        self._deps(e, reads, writes, extra)
        inst = e.obj.dma_start(out=out, in_=in_, **kw)
        d.count += 16
        inst.then_inc(d.sem, 16)
        for t in writes:
            t.w = (d, d.count); t.r = {}
        for t in reads:
            t.r[d] = d.count
        return inst

    def barrier(self):
        engs = list(self.eng.values())
        others = engs + [d for (lst, _) in self.dq.values() for d in lst] + [self.cc]
        for e in engs:
            for p in others:
                if p is e:
                    continue
                if p.count and e.seen.get(p, 0) < p.count:
                    e.obj.wait_ge(p.sem, p.count); e.seen[p] = p.count

    def phase_begin(self):
        self.pes = ExitStack()

    def phase_end(self):
        self.barrier()
        self.pes.close(); self.pes = None

    def collective(self, kind, in_t, in_ap, out_t, out_ap, groups):
        e = self.eng["pool"]; d = self.cc
        self._deps(e, [in_t], [out_t])
        inst = e.obj.collective_compute(kind, ALU.bypass, replica_groups=groups, ins=[in_ap], outs=[out_ap])
        d.count += 1
        inst.then_inc(d.sem, 1)
        out_t.w = (d, d.count); out_t.r = {}
        in_t.r[d] = d.count

    def finish(self):
        e = self.eng["sp"]
        for p in list(self.eng.values()):
            if p is not e and p.count and e.seen.get(p, 0) < p.count:
                e.obj.wait_ge(p.sem, p.count); e.seen[p] = p.count
        for q, (lst, _) in self.dq.items():
            for d in lst:
                if d.count and e.seen.get(d, 0) < d.count:
                    e.obj.wait_ge(d.sem, d.count); e.seen[d] = d.count
        if self.cc.count and e.seen.get(self.cc, 0) < self.cc.count:
            e.obj.wait_ge(self.cc.sem, self.cc.count)
        self.es.close()

    def alt(self, engs=("dve", "act")):
        self.rr += 1
        return engs[self.rr % len(engs)]


def _rope_consts():
    inv = np.zeros((128, 3), np.float32)
    perm = np.zeros((3, 128, 128), np.float32)
    f = THETA ** (-np.arange(0, 64, 2, dtype=np.float32) / 64.0)
    inv[0:32, 0] = f; inv[32:64, 0] = f
    for i in range(32):
        perm[0, 32 + i, i] = -1.0; perm[0, i, 32 + i] = 1.0
    f = THETA ** (-np.arange(0, 32, 2, dtype=np.float32) / 32.0)
    inv[0:16, 1] = f; inv[16:32, 1] = f
    for i in range(16):
        perm[1, 16 + i, i] = -1.0; perm[1, i, 16 + i] = 1.0
    f = THETA ** (-np.arange(0, 16, 2, dtype=np.float32) / 16.0)
    for base in (0, 64):
        inv[base:base + 8, 2] = f; inv[base + 8:base + 16, 2] = f
        for i in range(8):
            perm[2, base + 8 + i, base + i] = -1.0; perm[2, base + i, base + 8 + i] = 1.0
    inv = (inv.astype(np.float64) / (2 * np.pi)).astype(np.float32)
    return inv, perm


KA_OFF = 0; KB_OFF = 1024; VA_OFF = 2048; VB_OFF = 3072; KPE_OFF = 4096; KI_OFF = 4160; KVROWS = 4224
CH = 512; NCH = 9
NIT = 18
TOPK = 256
GROUPS = [[0, 1, 2, 3], [4, 5, 6, 7]]


def emit_A(c, io, l, hin):
    nc = c.nc
    win_d = io["w_in"]; wqb_d = io["w_q_b"]; wkvb_d = io["w_kv_b"]
    kv = io["kvpack"]
    hTb = c.sb([128, 16, TT], BF16, "hTb")
    for i in range(4):
        c.dma("pool", hTb[:, 4 * i:4 * i + 4, :], hin.ap[512 * i:512 * (i + 1), :].rearrange("(kc p) t -> p kc t", p=128),
              reads=[hin], writes=[hTb])
    inv = c.sb([128, 3], F32, "inv"); c.dma("sp", inv[:], io["inv"][:], reads=[io["inv"]], writes=[inv])
    perm = c.sb([128, 3, 128], F32, "perm")
    c.dma("sp", perm[:], io["perm"].ap.rearrange("r k m -> k r m"), reads=[io["perm"]], writes=[perm])
    gq = c.sb([128, 4], F32, "gq"); gkv = c.sb([128, 4], F32, "gkv")
    with nc.allow_non_contiguous_dma(reason="tiny gain vectors"):
        c.dma("sp", gq[:], io["g_q"].ap[l].rearrange("(c p) -> p c", p=128), reads=[io["g_q"]], writes=[gq])
        c.dma("sp", gkv[:], io["g_kv"].ap[l].rearrange("(c p) -> p c", p=128), reads=[io["g_kv"]], writes=[gkv])
    ones = c.sb([128, 128], BF16, "ones")
    c.op("pool", lambda e: e.memset(ones[:], 1.0), writes=[ones])

    posi = c.sb([128, TT], I32, "posi")
    c.dma("sp", posi[:], io["posq"].ap[0, :].partition_broadcast(128), reads=[io["posq"]], writes=[posi])
    posf = c.sb([128, TT], F32, "posf")
    c.op("dve", lambda e: e.tensor_copy(out=posf[:], in_=posi[:]), reads=[posi], writes=[posf])
    CC = c.sb([128, 3, TT], F32, "CC"); SS = c.sb([128, 3, TT], F32, "SS")
    u = c.sb([128, TT], F32, "u"); uf = c.sb([128, TT], F32, "uf")
    for r in range(3):
        for which, dst in ((0, SS), (1, CC)):
            c.op("dve", lambda e: e.tensor_scalar(out=u[:], in0=posf[:], scalar1=inv[:, r:r + 1], scalar2=0.25 * which,
                                                  op0=ALU.mult, op1=ALU.add), reads=[posf, inv], writes=[u])
            c.op("dve", lambda e: e.tensor_copy(out=posi[:], in_=u[:]), reads=[u], writes=[posi])
            c.op("dve", lambda e: e.tensor_copy(out=uf[:], in_=posi[:]), reads=[posi], writes=[uf])
            c.op("dve", lambda e: e.tensor_tensor(out=u[:], in0=u[:], in1=uf[:], op=ALU.subtract), reads=[u, uf], writes=[u])
            c.op("dve", lambda e: e.scalar_tensor_tensor(out=uf[:], in0=u[:], scalar=0.5, in1=u[:], op0=ALU.is_gt, op1=ALU.subtract),
                 reads=[u], writes=[uf])
            c.op("act", lambda e: e.activation(out=dst[:, r, :], in_=uf[:], func=AF.Sin, scale=-6.283185), reads=[uf], writes=[dst])

    wbufs = [c.sb([128, 16, 512], BF16, f"wbuf{i}") for i in range(2)]
    accs = [c.ps([128, TT], F32, f"acc{i}") for i in range(2)]
    rot_ps = c.ps([128, TT], F32, "rot_ps")
    rep_ps = c.ps([128, TT], F32, "rep_ps")
    stg = [c.sb([128, TT], BF16, f"stg{i}") for i in range(3)]
    pf = [c.sb([128, TT], F32, f"pf{i}") for i in range(2)]
    t1 = [c.sb([128, TT], F32, f"t1_{i}") for i in range(2)]
    st = {"w": 0, "a": 0, "s": 0, "p": 0}

    def load_w(w_ap, K, col0, ncols):
        KC = K // 128
        wb = wbufs[st["w"] % 2]; st["w"] += 1
        c.dma("pool", wb[:, 0:KC, 0:ncols], w_ap[:, col0:col0 + ncols].rearrange("(kc p) m -> p kc m", p=128),
              reads=[], writes=[wb])
        return wb

    def fm_chunk(wb, KC, off, M, rhs, epi):
        acc = accs[st["a"] % 2]; st["a"] += 1
        for th in range(2):
            for kc in range(KC):
                c.op("pe", lambda e: e.matmul(acc[0:M, th * 512:(th + 1) * 512], lhsT=wb[:, kc, off:off + M],
                                              rhs=rhs[:, kc, th * 512:(th + 1) * 512], start=(kc == 0), stop=(kc == KC - 1)),
                     reads=[wb, rhs], writes=[acc])
        epi(acc, M)

    def epi_plain(dst_ap, dst_t, rep=None):
        def f(acc, M):
            s = stg[st["s"] % 3]; st["s"] += 1
            if rep is not None:
                c.op("dve", lambda e: e.tensor_tensor(out=s[0:M, :], in0=acc[0:M, :], in1=rep[0:M, :], op=ALU.mult),
                     reads=[acc, rep], writes=[s])
            else:
                en = c.alt()
                if en == "act":
                    c.op("act", lambda e: e.activation(out=s[0:M, :], in_=acc[0:M, :], func=AF.Copy), reads=[acc], writes=[s])
                else:
                    c.op("dve", lambda e: e.tensor_copy(out=s[0:M, :], in_=acc[0:M, :]), reads=[acc], writes=[s])
            c.dma("sp", dst_ap, s[0:M, :], reads=[s], writes=[dst_t])
        return f

    def epi_rope(r, dst_ap, dst_t, rep=None):
        def f(acc, M):
            p = pf[st["p"] % 2]; q = t1[st["p"] % 2]; st["p"] += 1
            if rep is not None:
                c.op("dve", lambda e: e.tensor_tensor(out=p[0:M, :], in0=acc[0:M, :], in1=rep[0:M, :], op=ALU.mult),
                     reads=[acc, rep], writes=[p])
            else:
                c.op("act", lambda e: e.activation(out=p[0:M, :], in_=acc[0:M, :], func=AF.Copy), reads=[acc], writes=[p])
            for th in range(2):
                c.op("pe", lambda e: e.matmul(rot_ps[0:M, th * 512:(th + 1) * 512], lhsT=perm[0:M, r, 0:M],
                                              rhs=p[0:M, th * 512:(th + 1) * 512], start=True, stop=True),
                     reads=[perm, p], writes=[rot_ps])
            c.op("dve", lambda e: e.tensor_tensor(out=q[0:M, :], in0=rot_ps[0:M, :], in1=SS[0:M, r, :], op=ALU.mult),
                 reads=[rot_ps, SS], writes=[q])
            c.op("pool", lambda e: e.tensor_tensor(out=p[0:M, :], in0=p[0:M, :], in1=CC[0:M, r, :], op=ALU.mult),
                 reads=[p, CC], writes=[p])
            s = stg[st["s"] % 3]; st["s"] += 1
            c.op("dve", lambda e: e.tensor_tensor(out=s[0:M, :], in0=p[0:M, :], in1=q[0:M, :], op=ALU.add),
                 reads=[p, q], writes=[s])
            c.dma("sp", dst_ap, s[0:M, :], reads=[s], writes=[dst_t])
        return f

    cn = {"q": c.sb([128, 4, TT], BF16, "cqn"), "kv": c.sb([128, 4, TT], BF16, "ckvn")}
    sq = c.sb([128, 4, TT], BF16, "sq")
    rstd = {"q": c.sb([128, TT], F32, "rstd_q"), "kv": c.sb([128, TT], F32, "rstd_kv")}
    rstd_tm = c.sb([128, 8], F32, "rstd_tm")
    eps_t = c.sb([128, 1], F32, "eps")
    c.op("pool", lambda e: e.memset(eps_t[:], 1e-6), writes=[eps_t])

    def epi_rms(which, i, g):
        def f(acc, M):
            c.op("act", lambda e: e.activation(out=sq[:, i, :], in_=acc[:, :], func=AF.Square), reads=[acc], writes=[sq])
            c.op("dve", lambda e: e.tensor_scalar(out=cn[which][:, i, :], in0=acc[:, :], scalar1=g[:, i:i + 1], scalar2=None, op0=ALU.mult),
                 reads=[acc, g], writes=[cn[which]])
        return f

    win = win_d.ap[l]
    for which, col0, g in (("q", 0, gq), ("kv", 512, gkv)):
        wb = load_w(win, D, col0, 512)
        for i in range(4):
            fm_chunk(wb, 16, 128 * i, 128, hTb, epi_rms(which, i, g))
        for th in range(2):
            for i in range(4):
                c.op("pe", lambda e: e.matmul(rep_ps[:, th * 512:(th + 1) * 512], lhsT=ones[:, :], rhs=sq[:, i, th * 512:(th + 1) * 512],
                                              start=(i == 0), stop=(i == 3)), reads=[ones, sq], writes=[rep_ps])
        c.op("act", lambda e: e.activation(out=rstd[which][:], in_=rep_ps[:], func=AF.Sqrt, scale=1.0 / 512.0, bias=eps_t[:, 0:1]),
             reads=[rep_ps, eps_t], writes=[rstd[which]])
        c.op("dve", lambda e: e.reciprocal(out=rstd[which][:], in_=rstd[which][:]), reads=[rstd[which]], writes=[rstd[which]])
        if which == "kv":
            for tt in range(8):
                for i in range(4):
                    c.op("pe", lambda e: e.matmul(rep_ps[:, tt:tt + 1], lhsT=sq[:, i, tt * 128:(tt + 1) * 128], rhs=ones[:, 0:1],
                                                  start=(i == 0), stop=(i == 3)), reads=[sq, ones], writes=[rep_ps])
            c.op("act", lambda e: e.activation(out=rstd_tm[:], in_=rep_ps[:, 0:8], func=AF.Sqrt, scale=1.0 / 512.0, bias=eps_t[:, 0:1]),
                 reads=[rep_ps, eps_t], writes=[rstd_tm])
            c.op("dve", lambda e: e.reciprocal(out=rstd_tm[:], in_=rstd_tm[:]), reads=[rstd_tm], writes=[rstd_tm])

    wb = load_w(win, D, C_KPE, 64)
    fm_chunk(wb, 16, 0, 64, hTb, epi_rope(0, kv[KPE_OFF:KPE_OFF + 64, :], kv))
    for g in range(2):
        wb = load_w(win, D, C_QB + 512 * g, 512)
        for j in range(4):
            fm_chunk(wb, 16, 128 * j, 128, hTb, epi_rope(1, io["qbT"][4 * g + j, :, :], io["qbT"]))
    for g in range(2):
        wb = load_w(win, D, C_KB + 512 * g, 512)
        for j in range(4):
            h = 4 * g + j
            fm_chunk(wb, 16, 128 * j, 128, hTb, epi_rope(1, kv[KB_OFF + 128 * h:KB_OFF + 128 * (h + 1), :], kv))
    for g in range(2):
        wb = load_w(win, D, C_VB + 512 * g, 512)
        for tt in range(8):
            acc = accs[st["a"] % 2]; st["a"] += 1
            for kc in range(16):
                c.op("pe", lambda e: e.matmul(acc[:, 0:512], lhsT=hTb[:, kc, tt * 128:(tt + 1) * 128], rhs=wb[:, kc, 0:512],
                                              start=(kc == 0), stop=(kc == 15)), reads=[hTb, wb], writes=[acc])
            s = stg[st["s"] % 3]; st["s"] += 1
            en = c.alt()
            if en == "act":
                c.op("act", lambda e: e.activation(out=s[:, 0:512], in_=acc[:, 0:512], func=AF.Copy), reads=[acc], writes=[s])
            else:
                c.op("dve", lambda e: e.tensor_copy(out=s[:, 0:512], in_=acc[:, 0:512]), reads=[acc], writes=[s])
            c.dma("sp", kv[VB_OFF + tt * 128:VB_OFF + (tt + 1) * 128, 512 * g:512 * (g + 1)], s[:, 0:512], reads=[s], writes=[kv])
    for g in range(2):
        wb = load_w(win, D, C_QI + 512 * g, 512)
        for j in range(4):
            fm_chunk(wb, 16, 128 * j, 128, hTb, epi_rope(2, io["qiT"][4 * g + j, :, :], io["qiT"]))
    wb = load_w(win, D, C_KI, 80)
    fm_chunk(wb, 16, 0, 64, hTb, epi_rope(2, kv[KI_OFF:KI_OFF + 64, :], kv))
    wst = c.sb([128, 8, 16], F32, "wst")
    for tt in range(8):
        acc = accs[st["a"] % 2]; st["a"] += 1
        for kc in range(16):
            c.op("pe", lambda e: e.matmul(acc[:, 0:16], lhsT=hTb[:, kc, tt * 128:(tt + 1) * 128], rhs=wb[:, kc, 64:80],
                                          start=(kc == 0), stop=(kc == 15)), reads=[hTb, wb], writes=[acc])
        c.op("act", lambda e: e.activation(out=wst[:, tt, :], in_=acc[:, 0:16], func=AF.Copy, scale=1.0 / 32.0), reads=[acc], writes=[wst])
    c.dma("sp", io["widx"].ap.rearrange("(tt p) h -> p tt h", p=128), wst[:], reads=[wst], writes=[io["widx"]])
    wqb = wqb_d.ap[l]
    for g in range(4):
        wb = load_w(wqb, 512, 384 * g, 384)
        for j in range(2):
            h = 2 * g + j
            fm_chunk(wb, 4, 192 * j, 128, cn["q"], epi_plain(io["qaT"][h, 0:128, :], io["qaT"], rep=rstd["q"]))
            fm_chunk(wb, 4, 192 * j + 128, 64, cn["q"], epi_rope(0, io["qaT"][h, 128:192, :], io["qaT"], rep=rstd["q"]))
    wkvb = wkvb_d.ap[l]
    for g in range(4):
        wb = load_w(wkvb, 512, 512 * g, 512)
        for j in range(2):
            h = 2 * g + j
            fm_chunk(wb, 4, 256 * j, 128, cn["kv"], epi_plain(kv[KA_OFF + 128 * h:KA_OFF + 128 * (h + 1), :], kv, rep=rstd["kv"]))
        for tt in range(8):
            acc = accs[st["a"] % 2]; st["a"] += 1
            for j in range(2):
                for kc in range(4):
                    c.op("pe", lambda e: e.matmul(acc[:, 128 * j:128 * (j + 1)], lhsT=cn["kv"][:, kc, tt * 128:(tt + 1) * 128],
                                                  rhs=wb[:, kc, 256 * j + 128:256 * j + 256], start=(kc == 0), stop=(kc == 3)),
                         reads=[cn["kv"], wb], writes=[acc])
            s = stg[st["s"] % 3]; st["s"] += 1
            c.op("dve", lambda e: e.tensor_scalar(out=s[:, 0:256], in0=acc[:, 0:256], scalar1=rstd_tm[:, tt:tt + 1], scalar2=None, op0=ALU.mult),
                 reads=[acc, rstd_tm], writes=[s])
            c.dma("sp", kv[VA_OFF + tt * 128:VA_OFF + (tt + 1) * 128, 256 * g:256 * (g + 1)], s[:, 0:256], reads=[s], writes=[kv])


def emit_B1(c, io):
    nc = c.nc
    NK = SEQ; NKC = NK // 128; LA = 2
    gath = io["gath"]

    def gk(row0, nrows):
        k = row0 // CH; off = row0 % CH
        rows = min(CH, KVROWS - k * CH)
        return io["gath%d" % k].ap.rearrange("(r f) t -> f r t", r=4)[off:off + nrows, :, :], io["gath%d" % k]

    sc = c.sb([128, NK], F32, "sc")
    tmpi = sc[:, :].bitcast(I32)
    posq_bc = c.sb([128, TT], F32, "posq_bc")
    c.dma("sp", tmpi[:, 0:TT], io["posq"].ap[0, :].partition_broadcast(128), reads=[io["posq"]], writes=[sc])
    c.op("dve", lambda e: e.tensor_copy(out=posq_bc[:], in_=tmpi[:, 0:TT]), reads=[sc], writes=[posq_bc])
    posk_bc = c.sb([128, NK], F32, "posk_bc")
    c.dma("sp", tmpi[:, :], io["posk"].ap[0, :].partition_broadcast(128), reads=[io["posk"]], writes=[sc])
    c.op("dve", lambda e: e.tensor_copy(out=posk_bc[:], in_=tmpi[:, :]), reads=[sc], writes=[posk_bc])
    pci = c.sb([128, 40], I32, "pci"); posk_col = c.sb([128, NKC], F32, "posk_col"); posq_col = c.sb([128, 8], F32, "posq_col")
    with nc.allow_non_contiguous_dma(reason="tiny position columns"):
        c.dma("sp", pci[:, 0:NKC], io["posk"].ap[0, :].rearrange("(kc p) -> p kc", p=128), reads=[io["posk"]], writes=[pci])
        c.dma("sp", pci[:, 32:40], io["posq"].ap[0, :].rearrange("(kc p) -> p kc", p=128), reads=[io["posq"]], writes=[pci])
    c.op("dve", lambda e: e.tensor_copy(out=posk_col[:], in_=pci[:, 0:NKC]), reads=[pci], writes=[posk_col])
    c.op("dve", lambda e: e.tensor_copy(out=posq_col[:], in_=pci[:, 32:40]), reads=[pci], writes=[posq_col])
    ones = c.sb([128, 128], BF16, "ones")
    c.op("pool", lambda e: e.memset(ones[:], 1.0), writes=[ones])
    ident = c.sb([128, 128], BF16, "ident")
    c.dma("sp", ident[:], io["identb"][:], reads=[io["identb"]], writes=[ident])

    KT = [c.sb([128, NK], BF16, f"KT{i}") for i in range(2)]
    VV = [c.sb([128, NKC, 128], BF16, f"VV{i}") for i in range(2)]
    QT = [c.sb([128, TT], BF16, f"QT{i}") for i in range(2)]
    QP = [c.sb([64, TT], BF16, f"QP{i}") for i in range(2)]
    kpe = c.sb([64, NK], BF16, "kpe")
    pts = [c.sb([128, 512], BF16, f"pt{i}") for i in range(4)]
    S_ps = [c.ps([128, 512], F32, f"S{i}") for i in range(3)]
    acc = c.ps([128, 512], F32, "acc")
    den = c.ps([128, 512], F32, "den")
    rden = c.sb([128, 512], F32, "rden")
    ost = [c.sb([128, 512], BF16, f"ost{i}") for i in range(2)]
    maskB = c.sb([128, NKC, TT], BF16, "maskB")
    st = {"s": 0, "p": 0, "o": 0, "b": 0}

    def attention_head(h, mla, bg=None):
        b = st["b"] % 2; st["b"] += 1
        kt, vv, qt_, qp = KT[b], VV[b], QT[b], QP[b]
        koff, voff = (KA_OFF, VA_OFF) if mla else (KB_OFF, VB_OFF)
        ap_, gt = gk(koff + 128 * h, 128)
        c.dma("sp", kt[:, :].rearrange("p (r t) -> p r t", r=4), ap_, reads=[gt], writes=[kt])
        for r in range(4):
            for cc_ in range(2):
                gt = io["gath%d" % (voff // CH + cc_)]
                c.dma("sp", vv[:, 8 * r + 4 * cc_:8 * r + 4 * cc_ + 4, :],
                      gt.ap[r * CH:(r + 1) * CH, 128 * h:128 * (h + 1)].rearrange("(kc p) d -> p kc d", p=128), reads=[gt], writes=[vv])
        if mla:
            c.dma("sp", qt_[:], io["qaT"][h, 0:128, :], reads=[io["qaT"]], writes=[qt_])
            c.dma("sp", qp[:], io["qaT"][h, 128:192, :], reads=[io["qaT"]], writes=[qp])
            scale = 192.0 ** -0.5
        else:
            c.dma("sp", qt_[:], io["qbT"][h, :, :], reads=[io["qbT"]], writes=[qt_])
            scale = 128.0 ** -0.5
        for qh in range(2):
            qs = slice(qh * 512, (qh + 1) * 512)
            ptq = {}

            def front(kc):
                ks = slice(kc * 128, (kc + 1) * 128)
                S = S_ps[st["s"] % 3]; st["s"] += 1
                pt = pts[st["p"] % 4]; st["p"] += 1
                ptq[kc] = pt
                c.op("pe", lambda e: e.matmul(S[:], lhsT=kt[:, ks], rhs=qt_[:, qs], start=True, stop=not mla), reads=[kt, qt_], writes=[S])
                if mla:
                    c.op("pe", lambda e: e.matmul(S[:], lhsT=kpe[0:64, ks], rhs=qp[0:64, qs], start=False, stop=True), reads=[kpe, qp], writes=[S])
                c.op("act", lambda e: e.activation(out=pt[:], in_=S[:], func=AF.Exp, scale=scale), reads=[S], writes=[pt])
                if mla:
                    c.op("dve", lambda e: e.scalar_tensor_tensor(out=pt[:], in0=posq_bc[:, qs], scalar=posk_col[:, kc:kc + 1], in1=pt[:],
                                                                 op0=ALU.is_ge, op1=ALU.mult), reads=[posq_bc, posk_col, pt], writes=[pt])
                else:
                    c.op("dve", lambda e: e.tensor_tensor(out=pt[:], in0=pt[:], in1=maskB[:, kc, qs], op=ALU.mult), reads=[pt, maskB], writes=[pt])

            def back(kc):
                pt = ptq.pop(kc)
                c.op("pe", lambda e: e.matmul(acc[:], lhsT=vv[:, kc, :], rhs=pt[:], start=(kc == 0), stop=(kc == NKC - 1)), reads=[vv, pt], writes=[acc])
                c.op("pe", lambda e: e.matmul(den[:], lhsT=ones[:], rhs=pt[:], start=(kc == 0), stop=(kc == NKC - 1)), reads=[ones, pt], writes=[den])

            for step in range(NKC + LA):
                if step < NKC:
                    front(step)
                if step >= LA:
                    back(step - LA)
                if bg is not None and step % 3 == 2:
                    next(bg, None)
            c.op("dve", lambda e: e.reciprocal(out=rden[:], in_=den[:]), reads=[den], writes=[rden])
            o = ost[st["o"] % 2]; st["o"] += 1
            c.op("dve", lambda e: e.tensor_tensor(out=o[:], in0=acc[:], in1=rden[:], op=ALU.mult), reads=[acc, rden], writes=[o])
            dst = io["oaT"] if mla else io["obT"]
            c.dma("sp", dst[128 * h:128 * (h + 1), qs], o[:], reads=[o], writes=[dst])

    ki2 = c.sb([128, NK], BF16, "ki2")
    ap_, gt = gk(KI_OFF, 64)
    c.dma("sp", ki2[0:64, :].rearrange("p (r t) -> p r t", r=4), ap_, reads=[gt], writes=[ki2])
    c.dma("sp", ki2[64:128, :].rearrange("p (r t) -> p r t", r=4), ap_, reads=[gt], writes=[ki2])
    qi = c.sb([128, 8, TT], BF16, "qi")
    c.dma("sp", qi[:], io["qiT"].ap.rearrange("g p t -> p g t"), reads=[io["qiT"]], writes=[qi])
    wcol = c.sb([128, 8, 16], F32, "wcol")
    c.dma("sp", wcol[:], io["widx"].ap.rearrange("(qb p) h -> p qb h", p=128), reads=[io["widx"]], writes=[wcol])
    junk = c.sb([128, NK], BF16, "junk")
    rb = [c.sb([128, 512], F32, f"rb{i}") for i in range(3)]
    I_ps = [c.ps([128, 512], F32, "I0"), c.ps([128, 512], F32, "I1")]
    tp = c.ps([128, 512], BF16, "tp")
    sm = {k: c.sb([128, 1], F32, "sm_" + k) for k in ("lo", "hi", "mid", "cnt", "ge", "d1", "d2", "am")}
    ri = {"r": 0, "i": 0}

    def indexer_gen(qb):
        qs = slice(qb * 128, (qb + 1) * 128)
        for kt_ in range(NK // 512):
            ksl = slice(kt_ * 512, (kt_ + 1) * 512)
            for h in range(16):
                pb = 64 * (h % 2)
                ip = I_ps[ri["i"] % 2]; ri["i"] += 1
                r = rb[ri["r"] % 3]; ri["r"] += 1
                c.op("pe", lambda e: e.matmul(ip[:], lhsT=qi[pb:pb + 64, h // 2, qs], rhs=ki2[pb:pb + 64, ksl], start=True, stop=True),
                     reads=[qi, ki2], writes=[ip])
                c.op("act", lambda e: e.activation(out=r[:], in_=ip[:], func=AF.Relu), reads=[ip], writes=[r])
                if h == 0:
                    c.op("dve", lambda e: e.tensor_scalar(out=sc[:, ksl], in0=r[:], scalar1=wcol[:, qb, 0:1], scalar2=None, op0=ALU.mult),
                         reads=[r, wcol], writes=[sc])
                else:
                    c.op("dve", lambda e: e.scalar_tensor_tensor(out=sc[:, ksl], in0=r[:], scalar=wcol[:, qb, h:h + 1], in1=sc[:, ksl],
                                                                 op0=ALU.mult, op1=ALU.add), reads=[r, wcol, sc], writes=[sc])
        yield
        c.op("dve", lambda e: e.tensor_reduce(out=sm["am"][:], in_=sc[:], axis=AX.X, op=ALU.max, apply_absolute_value=True),
             reads=[sc], writes=[sm["am"]])
        c.op("dve", lambda e: e.tensor_scalar(out=sm["hi"][:], in0=sm["am"][:], scalar1=1.001, scalar2=1e-6, op0=ALU.mult, op1=ALU.add),
             reads=[sm["am"]], writes=[sm["hi"]])
        c.op("dve", lambda e: e.tensor_scalar(out=sm["lo"][:], in0=sm["hi"][:], scalar1=-1.0, scalar2=None, op0=ALU.mult),
             reads=[sm["hi"]], writes=[sm["lo"]])
        c.op("pool", lambda e: e.tensor_scalar(out=junk[:], in0=posk_bc[:], scalar1=posq_col[:, qb:qb + 1], scalar2=-1e30,
                                               op0=ALU.is_gt, op1=ALU.mult), reads=[posk_bc, posq_col], writes=[junk])
        c.op("pool", lambda e: e.tensor_tensor(out=sc[:], in0=sc[:], in1=junk[:], op=ALU.add), reads=[sc, junk], writes=[sc])
        yield
        for it in range(NIT):
            if it:
                yield
            c.op("dve", lambda e: e.tensor_tensor(out=sm["mid"][:], in0=sm["lo"][:], in1=sm["hi"][:], op=ALU.add),
                 reads=[sm["lo"], sm["hi"]], writes=[sm["mid"]])
            c.op("dve", lambda e: e.tensor_scalar(out=sm["mid"][:], in0=sm["mid"][:], scalar1=0.5, scalar2=None, op0=ALU.mult),
                 reads=[sm["mid"]], writes=[sm["mid"]])
            c.op("dve", lambda e: e.tensor_scalar(out=junk[:], in0=sc[:], scalar1=sm["mid"][:, 0:1], scalar2=0.0, op0=ALU.is_ge, op1=ALU.add,
                                                  accum_out=sm["cnt"][:]), reads=[sc, sm["mid"]], writes=[junk, sm["cnt"]])
            c.op("dve", lambda e: e.tensor_scalar(out=sm["ge"][:], in0=sm["cnt"][:], scalar1=TOPK - 0.5, scalar2=None, op0=ALU.is_ge),
                 reads=[sm["cnt"]], writes=[sm["ge"]])
            c.op("dve", lambda e: e.tensor_tensor(out=sm["d1"][:], in0=sm["mid"][:], in1=sm["lo"][:], op=ALU.subtract),
                 reads=[sm["mid"], sm["lo"]], writes=[sm["d1"]])
            c.op("dve", lambda e: e.tensor_tensor(out=sm["d2"][:], in0=sm["hi"][:], in1=sm["mid"][:], op=ALU.subtract),
                 reads=[sm["hi"], sm["mid"]], writes=[sm["d2"]])
            c.op("dve", lambda e: e.scalar_tensor_tensor(out=sm["lo"][:], in0=sm["d1"][:], scalar=sm["ge"][:, 0:1], in1=sm["lo"][:],
                                                         op0=ALU.mult, op1=ALU.add), reads=[sm["d1"], sm["ge"], sm["lo"]], writes=[sm["lo"]])
            c.op("dve", lambda e: e.scalar_tensor_tensor(out=sm["hi"][:], in0=sm["d2"][:], scalar=sm["ge"][:, 0:1], in1=sm["mid"][:],
                                                         op0=ALU.mult, op1=ALU.add), reads=[sm["d2"], sm["ge"], sm["mid"]], writes=[sm["hi"]])
        c.op("dve", lambda e: e.tensor_scalar(out=junk[:], in0=sc[:], scalar1=sm["lo"][:, 0:1], scalar2=None, op0=ALU.is_ge),
             reads=[sc, sm["lo"]], writes=[junk])
        for g in range(NKC // 4):
            for j in range(4):
                kc = 4 * g + j
                c.op("pe", lambda e: e.transpose(out=tp[:, j * 128:(j + 1) * 128], in_=junk[:, kc * 128:(kc + 1) * 128], identity=ident[:]),
                     reads=[junk, ident], writes=[tp])
            c.op("act", lambda e: e.activation(out=maskB[:, 4 * g:4 * g + 4, qs], in_=tp[:, :].rearrange("p (j q) -> p j q", j=4), func=AF.Copy),
                 reads=[tp], writes=[maskB])

    ap_, gt = gk(KPE_OFF, 64)
    c.dma("sp", kpe[:, :].rearrange("p (r t) -> p r t", r=4), ap_, reads=[gt], writes=[kpe])
    for i in range(8):
        g_ = indexer_gen(i)
        next(g_)
        attention_head(i, True, bg=g_)
        for _ in g_:
            pass
    for h in range(8):
        attention_head(h, False)


def emit_ln(c, r, g_ap, b_ap, out_t, ps, onesf):
    nc = c.nc
    gcol = c.sb([128, 16], F32, "ln_g"); bcol = c.sb([128, 16], F32, "ln_b")
    with nc.allow_non_contiguous_dma(reason="tiny ln vectors"):
        c.dma("sp", gcol[:], g_ap.rearrange("(c p) -> p c", p=128), reads=[], writes=[gcol])
        c.dma("sp", bcol[:], b_ap.rearrange("(c p) -> p c", p=128), reads=[], writes=[bcol])
    tmp = c.sb([128, TT], F32, "ln_tmp")
    mean = c.sb([128, TT], F32, "ln_mean"); rstd = c.sb([128, TT], F32, "ln_rstd")
    for th in range(2):
        ts_ = slice(th * 512, (th + 1) * 512)
        for m in range(16):
            c.op("pe", lambda e: e.matmul(ps[th][:], lhsT=onesf[:, :], rhs=r[:, m, ts_], start=(m == 0), stop=(m == 15)),
                 reads=[onesf, r], writes=[ps[th]])
    for m in range(16):
        c.op("act", lambda e: e.activation(out=tmp[:, :], in_=r[:, m, :], func=AF.Square), reads=[r], writes=[tmp])
        for th in range(2):
            ts_ = slice(th * 512, (th + 1) * 512)
            c.op("pe", lambda e: e.matmul(ps[2 + th][:], lhsT=onesf[:, :], rhs=tmp[:, ts_], start=(m == 0), stop=(m == 15)),
                 reads=[onesf, tmp], writes=[ps[2 + th]])
    for th in range(2):
        ts_ = slice(th * 512, (th + 1) * 512)
        c.op("act", lambda e: e.activation(out=mean[:, ts_], in_=ps[th][:], func=AF.Copy, scale=1.0 / D), reads=[ps[th]], writes=[mean])
        c.op("dve", lambda e: e.tensor_tensor(out=tmp[:, ts_], in0=mean[:, ts_], in1=mean[:, ts_], op=ALU.mult), reads=[mean], writes=[tmp])
        c.op("dve", lambda e: e.scalar_tensor_tensor(out=rstd[:, ts_], in0=ps[2 + th][:], scalar=1.0 / D, in1=tmp[:, ts_], op0=ALU.mult, op1=ALU.subtract),
             reads=[ps[2 + th], tmp], writes=[rstd])
    c.op("dve", lambda e: e.tensor_scalar(out=rstd[:], in0=rstd[:], scalar1=1e-5, scalar2=None, op0=ALU.add), reads=[rstd], writes=[rstd])
    c.op("act", lambda e: e.activation(out=rstd[:], in_=rstd[:], func=AF.Sqrt), reads=[rstd], writes=[rstd])
    c.op("dve", lambda e: e.reciprocal(out=rstd[:], in_=rstd[:]), reads=[rstd], writes=[rstd])
    for m in range(16):
        c.op("dve", lambda e: e.tensor_tensor(out=r[:, m, :], in0=r[:, m, :], in1=mean[:], op=ALU.subtract), reads=[r, mean], writes=[r])
        c.op("pool", lambda e: e.tensor_tensor(out=r[:, m, :], in0=r[:, m, :], in1=rstd[:], op=ALU.mult), reads=[r, rstd], writes=[r])
        c.op("dve", lambda e: e.tensor_scalar(out=r[:, m, :], in0=r[:, m, :], scalar1=gcol[:, m:m + 1], scalar2=bcol[:, m:m + 1],
                                              op0=ALU.mult, op1=ALU.add), reads=[r, gcol, bcol], writes=[r])
    for i in range(4):
        c.dma("sp", out_t.ap[512 * i:512 * (i + 1), :].rearrange("(kc p) t -> p kc t", p=128), r[:, 4 * i:4 * i + 4, :], reads=[r], writes=[out_t])


def emit_B2(c, io, l, hin, hout):
    nc = c.nc
    win = io["w_in"].ap[l]; woa = io["w_o_a"].ap[l]; wob = io["w_o_b"].ap[l]; wout = io["w_out"].ap[l]
    hTb = c.sb([128, 16, TT], BF16, "hTb"); r = c.sb([128, 16, TT], F32, "r")
    for i in range(4):
        c.dma("pool", hTb[:, 4 * i:4 * i + 4, :], hin.ap[512 * i:512 * (i + 1), :].rearrange("(kc p) t -> p kc t", p=128), reads=[hin], writes=[hTb])
        c.dma("sp", r[:, 4 * i:4 * i + 4, :], hin.ap[512 * i:512 * (i + 1), :].rearrange("(kc p) t -> p kc t", p=128), reads=[hin], writes=[r])
    oa = c.sb([128, 8, TT], BF16, "oa"); ob = c.sb([128, 8, TT], BF16, "ob")
    c.dma("sp", oa[:], io["oaT"].ap.rearrange("(kc p) t -> p kc t", p=128), reads=[io["oaT"]], writes=[oa])
    c.dma("sp", ob[:], io["obT"].ap.rearrange("(kc p) t -> p kc t", p=128), reads=[io["obT"]], writes=[ob])
    yT = c.sb([128, 16, TT], BF16, "yT")
    onesf = c.sb([128, 128], F32, "onesf")
    c.op("pool", lambda e: e.memset(onesf[:], 1.0), writes=[onesf])
    wg = [c.sb([128, 16, 256], BF16, f"wg{i}") for i in range(2)]
    wo = [c.sb([128, 8, 256], BF16, f"wo{i}") for i in range(2)]
    ps = [c.ps([128, 512], F32, f"ps{i}") for i in range(8)]
    sg = [c.sb([128, 512], F32, f"sg{i}") for i in range(2)]
    ta = c.sb([128, 512], F32, "ta"); tb = c.sb([128, 512], F32, "tb")
    k = 0
    for grp in range(8):
        c.dma("pool", wg[0][:], win[:, C_GA + 256 * grp:C_GA + 256 * (grp + 1)].rearrange("(kc p) m -> p kc m", p=128), reads=[], writes=[wg[0]])
        c.dma("pool", wg[1][:], win[:, C_GB + 256 * grp:C_GB + 256 * (grp + 1)].rearrange("(kc p) m -> p kc m", p=128), reads=[], writes=[wg[1]])
        c.dma("pool", wo[0][:], woa[:, 256 * grp:256 * (grp + 1)].rearrange("(kc p) m -> p kc m", p=128), reads=[], writes=[wo[0]])
        c.dma("pool", wo[1][:], wob[:, 256 * grp:256 * (grp + 1)].rearrange("(kc p) m -> p kc m", p=128), reads=[], writes=[wo[1]])
        for j in range(2):
            m = 2 * grp + j
            ms = slice(128 * j, 128 * (j + 1))
            for th in range(2):
                ts_ = slice(th * 512, (th + 1) * 512)
                p4 = ps[4 * (k % 2):4 * (k % 2) + 4]; k += 1
                for kc in range(16):
                    c.op("pe", lambda e: e.matmul(p4[0][:], lhsT=wg[0][:, kc, ms], rhs=hTb[:, kc, ts_], start=(kc == 0), stop=(kc == 15)), reads=[wg[0], hTb], writes=[p4[0]])
                for kc in range(16):
                    c.op("pe", lambda e: e.matmul(p4[1][:], lhsT=wg[1][:, kc, ms], rhs=hTb[:, kc, ts_], start=(kc == 0), stop=(kc == 15)), reads=[wg[1], hTb], writes=[p4[1]])
                for kc in range(8):
                    c.op("pe", lambda e: e.matmul(p4[2][:], lhsT=wo[0][:, kc, ms], rhs=oa[:, kc, ts_], start=(kc == 0), stop=(kc == 7)), reads=[wo[0], oa], writes=[p4[2]])
                for kc in range(8):
                    c.op("pe", lambda e: e.matmul(p4[3][:], lhsT=wo[1][:, kc, ms], rhs=ob[:, kc, ts_], start=(kc == 0), stop=(kc == 7)), reads=[wo[1], ob], writes=[p4[3]])
                c.op("act", lambda e: e.activation(out=sg[0][:], in_=p4[0][:], func=AF.Sigmoid), reads=[p4[0]], writes=[sg[0]])
                c.op("act", lambda e: e.activation(out=sg[1][:], in_=p4[1][:], func=AF.Sigmoid), reads=[p4[1]], writes=[sg[1]])
                c.op("dve", lambda e: e.tensor_tensor(out=ta[:], in0=p4[2][:], in1=sg[0][:], op=ALU.mult), reads=[p4[2], sg[0]], writes=[ta])
                c.op("dve", lambda e: e.tensor_tensor(out=tb[:], in0=p4[3][:], in1=sg[1][:], op=ALU.mult), reads=[p4[3], sg[1]], writes=[tb])
                c.op("pool", lambda e: e.tensor_tensor(out=yT[:, m, ts_], in0=ta[:], in1=tb[:], op=ALU.add), reads=[ta, tb], writes=[yT])
    for grp in range(8):
        w = wg[grp % 2]
        c.dma("pool", w[:], wout[:, 256 * grp:256 * (grp + 1)].rearrange("(kc p) m -> p kc m", p=128), reads=[], writes=[w])
        for j in range(2):
            m = 2 * grp + j
            ms = slice(128 * j, 128 * (j + 1))
            for th in range(2):
                ts_ = slice(th * 512, (th + 1) * 512)
                p = ps[k % 8]; k += 1
                for kc in range(16):
                    c.op("pe", lambda e: e.matmul(p[:], lhsT=w[:, kc, ms], rhs=yT[:, kc, ts_], start=(kc == 0), stop=(kc == 15)), reads=[w, yT], writes=[p])
                c.op("dve", lambda e: e.scalar_tensor_tensor(out=r[:, m, ts_], in0=r[:, m, ts_], scalar=ALPHA, in1=p[:], op0=ALU.mult, op1=ALU.add),
                     reads=[r, p], writes=[r])
    emit_ln(c, r, io["ln1_g"].ap[l], io["ln1_b"].ap[l], hout, ps, onesf)


def emit_router(c, io, l, r, g_ps):
    nc = c.nc
    wr = c.sb([128, 16, 72], F32, "wr"); bias = c.sb([128, 72], F32, "bias")
    with nc.allow_non_contiguous_dma(reason="small router weights"):
        c.dma("sp", wr[:, :, 0:8], io["w_group"].ap[l].rearrange("(kc p) m -> p kc m", p=128), reads=[], writes=[wr])
        c.dma("sp", wr[:, :, 8:72], io["w_router"].ap[l].rearrange("(kc p) m -> p kc m", p=128), reads=[], writes=[wr])
    c.dma("sp", bias[:, 0:8], io["b_group"].ap[l].partition_broadcast(128), reads=[], writes=[bias])
    c.dma("sp", bias[:, 8:72], io["b_router"].ap[l].partition_broadcast(128), reads=[], writes=[bias])
    CW = c.sb([128, 8, 64], F32, "CW")
    GM = c.sb([128, 8, 8], F32, "GM")
    lg = c.sb([128, 72], F32, "lg"); em = c.sb([128, 64], F32, "em"); em2 = c.sb([128, 64], F32, "em2")
    mk1 = c.sb([128, 64], F32, "mk1"); mk2 = c.sb([128, 64], F32, "mk2"); gex = c.sb([128, 8], F32, "gex")
    s = {k: c.sb([128, 1], F32, "r_" + k) for k in ("gmax", "ngmax", "gsum", "gp", "m1", "m2", "d", "ex", "w1", "w2")}
    pen = c.sb([128, 8], F32, "pen")
    for tt in range(8):
        tsl = slice(tt * 128, (tt + 1) * 128)
        gmask = GM[:, tt, :]
        for kc in range(16):
            c.op("pe", lambda e: e.matmul(g_ps[:, 0:72], lhsT=r[:, kc, tsl], rhs=wr[:, kc, :], start=(kc == 0), stop=(kc == 15)), reads=[r, wr], writes=[g_ps])
        c.op("dve", lambda e: e.tensor_tensor(out=lg[:], in0=g_ps[:, 0:72], in1=bias[:], op=ALU.add), reads=[g_ps, bias], writes=[lg])
        c.op("dve", lambda e: e.tensor_reduce(out=s["gmax"][:], in_=lg[:, 0:8], axis=AX.X, op=ALU.max), reads=[lg], writes=[s["gmax"]])
        c.op("dve", lambda e: e.tensor_scalar(out=gmask, in0=lg[:, 0:8], scalar1=s["gmax"][:, 0:1], scalar2=None, op0=ALU.is_ge), reads=[lg, s["gmax"]], writes=[GM])
        c.op("dve", lambda e: e.tensor_scalar(out=s["ngmax"][:], in0=s["gmax"][:], scalar1=-1.0, scalar2=None, op0=ALU.mult), reads=[s["gmax"]], writes=[s["ngmax"]])
        c.op("act", lambda e: e.activation(out=gex[:], in_=lg[:, 0:8], func=AF.Exp, bias=s["ngmax"][:, 0:1], accum_out=s["gsum"][:]),
             reads=[lg, s["ngmax"]], writes=[gex, s["gsum"]])
        c.op("dve", lambda e: e.reciprocal(out=s["gp"][:], in_=s["gsum"][:]), reads=[s["gsum"]], writes=[s["gp"]])
        c.op("dve", lambda e: e.tensor_scalar(out=pen[:], in0=gmask, scalar1=-1.0, scalar2=1e30, op0=ALU.add, op1=ALU.mult), reads=[GM], writes=[pen])
        for g in range(8):
            c.op("dve", lambda e: e.tensor_scalar(out=em[:, 8 * g:8 * g + 8], in0=lg[:, 8 + 8 * g:16 + 8 * g], scalar1=pen[:, g:g + 1], scalar2=None, op0=ALU.add),
                 reads=[lg, pen], writes=[em])
        c.op("dve", lambda e: e.tensor_reduce(out=s["m1"][:], in_=em[:], axis=AX.X, op=ALU.max), reads=[em], writes=[s["m1"]])
        c.op("dve", lambda e: e.tensor_scalar(out=mk1[:], in0=em[:], scalar1=s["m1"][:, 0:1], scalar2=None, op0=ALU.is_ge), reads=[em, s["m1"]], writes=[mk1])
        c.op("dve", lambda e: e.scalar_tensor_tensor(out=em2[:], in0=mk1[:], scalar=-1e30, in1=em[:], op0=ALU.mult, op1=ALU.add), reads=[mk1, em], writes=[em2])
        c.op("dve", lambda e: e.tensor_reduce(out=s["m2"][:], in_=em2[:], axis=AX.X, op=ALU.max), reads=[em2], writes=[s["m2"]])
        c.op("dve", lambda e: e.tensor_scalar(out=mk2[:], in0=em2[:], scalar1=s["m2"][:, 0:1], scalar2=None, op0=ALU.is_ge), reads=[em2, s["m2"]], writes=[mk2])
        c.op("dve", lambda e: e.tensor_tensor(out=s["d"][:], in0=s["m2"][:], in1=s["m1"][:], op=ALU.subtract), reads=[s["m1"], s["m2"]], writes=[s["d"]])
        c.op("act", lambda e: e.activation(out=s["ex"][:], in_=s["d"][:], func=AF.Exp), reads=[s["d"]], writes=[s["ex"]])
        c.op("dve", lambda e: e.tensor_scalar(out=s["ex"][:], in0=s["ex"][:], scalar1=1.0, scalar2=None, op0=ALU.add), reads=[s["ex"]], writes=[s["ex"]])
        c.op("dve", lambda e: e.reciprocal(out=s["ex"][:], in_=s["ex"][:]), reads=[s["ex"]], writes=[s["ex"]])
        c.op("dve", lambda e: e.tensor_tensor(out=s["w1"][:], in0=s["gp"][:], in1=s["ex"][:], op=ALU.mult), reads=[s["gp"], s["ex"]], writes=[s["w1"]])
        c.op("dve", lambda e: e.tensor_tensor(out=s["w2"][:], in0=s["gp"][:], in1=s["w1"][:], op=ALU.subtract), reads=[s["gp"], s["w1"]], writes=[s["w2"]])
        c.op("dve", lambda e: e.tensor_scalar(out=mk1[:], in0=mk1[:], scalar1=s["w1"][:, 0:1], scalar2=None, op0=ALU.mult), reads=[mk1, s["w1"]], writes=[mk1])
        c.op("dve", lambda e: e.scalar_tensor_tensor(out=CW[:, tt, :], in0=mk2[:], scalar=s["w2"][:, 0:1], in1=mk1[:], op0=ALU.mult, op1=ALU.add),
             reads=[mk2, s["w2"], mk1], writes=[CW])
    return CW, GM


def emit_B3_dense(c, io, l, hin, hout, n_tt=8, n_e=64):
    nc = c.nc
    hTb = c.sb([128, 16, TT], BF16, "hTb"); r = c.sb([128, 16, TT], F32, "r")
    for i in range(4):
        c.dma("pool", hTb[:, 4 * i:4 * i + 4, :], hin.ap[512 * i:512 * (i + 1), :].rearrange("(kc p) t -> p kc t", p=128), reads=[hin], writes=[hTb])
        c.dma("sp", r[:, 4 * i:4 * i + 4, :], hin.ap[512 * i:512 * (i + 1), :].rearrange("(kc p) t -> p kc t", p=128), reads=[hin], writes=[r])
    identb = c.sb([128, 128], BF16, "identb"); identf = c.sb([128, 128], F32, "identf")
    c.dma("sp", identb[:], io["identb"][:], reads=[], writes=[identb])
    c.dma("sp", identf[:], io["identf"][:], reads=[], writes=[identf])
    onesf = c.sb([128, 128], F32, "onesf")
    c.op("pool", lambda e: e.memset(onesf[:], 1.0), writes=[onesf])
    y_ps = [c.ps([128, 512], F32, f"y{i}") for i in range(4)]
    g_ps = c.ps([128, 512], F32, "g"); u_ps = c.ps([128, 512], F32, "u")
    tp_ps = c.ps([128, 512], BF16, "tp")
    CW, GM = emit_router(c, io, l, r, g_ps)
    Wg = [c.sb([128, 16, 512], BF16, f"Wg{i}") for i in range(2)]
    Wu = [c.sb([128, 16, 512], BF16, f"Wu{i}") for i in range(1)] * 2
    Wd = [c.sb([128, 4, D], BF16, f"Wd{i}") for i in range(1)] * 2
    sg = c.sb([128, 512], F32, "sg"); hid = c.sb([128, 512], BF16, "hid"); hidT = c.sb([128, 4, 128], BF16, "hidT")
    ysb = c.sb([128, D], F32, "ysb")
    k = 0
    for tt in range(n_tt):
        tsl = slice(tt * 128, (tt + 1) * 128)
        for ex in range(n_e):
            wgb, wub, wdb = Wg[k % 2], Wu[k % 2], Wd[k % 2]; k += 1
            c.dma("pool", wgb[:], io["w_e_gate"].ap[l, ex].rearrange("(kc p) m -> p kc m", p=128), reads=[], writes=[wgb])
            c.dma("pool", wub[:], io["w_e_up"].ap[l, ex].rearrange("(kc p) m -> p kc m", p=128), reads=[], writes=[wub])
            c.dma("pool", wdb[:], io["w_e_down"].ap[l, ex].rearrange("(kc p) m -> p kc m", p=128), reads=[], writes=[wdb])
            for kc in range(16):
                c.op("pe", lambda e: e.matmul(g_ps[:], lhsT=hTb[:, kc, tsl], rhs=wgb[:, kc, :], start=(kc == 0), stop=(kc == 15)), reads=[hTb, wgb], writes=[g_ps])
            for kc in range(16):
                c.op("pe", lambda e: e.matmul(u_ps[:], lhsT=hTb[:, kc, tsl], rhs=wub[:, kc, :], start=(kc == 0), stop=(kc == 15)), reads=[hTb, wub], writes=[u_ps])
            c.op("act", lambda e: e.activation(out=sg[:], in_=g_ps[:], func=AF.Silu), reads=[g_ps], writes=[sg])
            c.op("dve", lambda e: e.scalar_tensor_tensor(out=hid[:], in0=sg[:], scalar=CW[:, tt, ex:ex + 1], in1=u_ps[:], op0=ALU.mult, op1=ALU.mult),
                 reads=[sg, CW, u_ps], writes=[hid])
            for fc in range(4):
                c.op("pe", lambda e: e.transpose(out=tp_ps[:, fc * 128:(fc + 1) * 128], in_=hid[:, fc * 128:(fc + 1) * 128], identity=identb[:]),
                     reads=[hid, identb], writes=[tp_ps])
            c.op("act", lambda e: e.activation(out=hidT[:, :, :], in_=tp_ps[:, :].rearrange("p (f t) -> p f t", f=4), func=AF.Copy), reads=[tp_ps], writes=[hidT])
            for n in range(4):
                for fc in range(4):
                    c.op("pe", lambda e: e.matmul(y_ps[n][:], lhsT=hidT[:, fc, :], rhs=wdb[:, fc, n * 512:(n + 1) * 512],
                                                  start=(ex == 0 and fc == 0), stop=(ex == n_e - 1 and fc == 3)), reads=[hidT, wdb], writes=[y_ps[n]])
        for n in range(4):
            c.op("act", lambda e: e.activation(out=ysb[:, n * 512:(n + 1) * 512], in_=y_ps[n][:], func=AF.Copy), reads=[y_ps[n]], writes=[ysb])
        for m in range(16):
            c.op("pe", lambda e: e.transpose(out=u_ps[:, 0:128], in_=ysb[:, m * 128:(m + 1) * 128], identity=identf[:]), reads=[ysb, identf], writes=[u_ps])
            c.op("dve", lambda e: e.scalar_tensor_tensor(out=r[:, m, tsl], in0=r[:, m, tsl], scalar=ALPHA, in1=u_ps[:, 0:128], op0=ALU.mult, op1=ALU.add),
                 reads=[r, u_ps], writes=[r])
    for tt in range(n_tt, 8):
        tsl = slice(tt * 128, (tt + 1) * 128)
        c.op("dve", lambda e: e.tensor_scalar(out=r[:, :, tsl], in0=r[:, :, tsl], scalar1=ALPHA, scalar2=None, op0=ALU.mult), reads=[r], writes=[r])
    emit_ln(c, r, io["ln2_g"].ap[l], io["ln2_b"].ap[l], hout, y_ps, onesf)


def emit_B3(c, io, l, hin, hout, n_e=64):
    nc = c.nc
    c.phase_begin()
    r = c.sb([128, 16, TT], F32, "r")
    for i in range(4):
        c.dma("sp", r[:, 4 * i:4 * i + 4, :], hin.ap[512 * i:512 * (i + 1), :].rearrange("(kc p) t -> p kc t", p=128), reads=[hin], writes=[r])
    g_ps = c.ps([128, 512], F32, "g")
    CW, GM = emit_router(c, io, l, r, g_ps)
    c.dma("sp", io["cwd"].ap.rearrange("(tt p) e -> p tt e", p=128), CW[:], reads=[CW], writes=[io["cwd"]])
    c.phase_end()
    c.phase_begin()
    hTb = c.sb([128, 16, TT], BF16, "hTb")
    for i in range(4):
        c.dma("pool", hTb[:, 4 * i:4 * i + 4, :], hin.ap[512 * i:512 * (i + 1), :].rearrange("(kc p) t -> p kc t", p=128), reads=[hin], writes=[hTb])
    CW = c.sb([128, 8, 64], F32, "CW2")
    c.dma("sp", CW[:], io["cwd"].ap.rearrange("(tt p) e -> p tt e", p=128), reads=[io["cwd"]], writes=[CW])
    identb = c.sb([128, 128], BF16, "identb")
    c.dma("sp", identb[:], io["identb"][:], reads=[], writes=[identb])
    y_ps = [c.ps([128, 512], F32, f"y{i}") for i in range(4)]
    g_ps = c.ps([128, 512], F32, "g"); u_ps = c.ps([128, 512], F32, "u")
    tp_ps = c.ps([128, 512], BF16, "tp")
    y_sb = [c.sb([128, D], F32, f"ysb{i}") for i in range(8)]
    Wg = [c.sb([128, 16, 512], BF16, f"Wg{i}") for i in range(2)]
    Wu = [c.sb([128, 16, 512], BF16, f"Wu{i}") for i in range(2)]
    Wd = [c.sb([128, 4, D], BF16, f"Wd{i}") for i in range(2)]
    sg = c.sb([128, 512], F32, "sg"); hid = c.sb([128, 512], BF16, "hid"); hidT = c.sb([128, 4, 128], BF16, "hidT")
    items = [(ex, tt) for ex in range(n_e) for tt in range(8)]

    def load_w(ex):
        c.dma("pool", Wg[ex % 2][:], io["w_e_gate"].ap[l, ex].rearrange("(kc p) m -> p kc m", p=128), reads=[], writes=[Wg[ex % 2]])
        c.dma("pool", Wu[ex % 2][:], io["w_e_up"].ap[l, ex].rearrange("(kc p) m -> p kc m", p=128), reads=[], writes=[Wu[ex % 2]])
        c.dma("pool", Wd[ex % 2][:], io["w_e_down"].ap[l, ex].rearrange("(kc p) m -> p kc m", p=128), reads=[], writes=[Wd[ex % 2]])

    def gate(i):
        ex, tt = items[i]; tsl = slice(tt * 128, (tt + 1) * 128); wgb = Wg[ex % 2]
        for kc in range(16):
            c.op("pe", lambda e: e.matmul(g_ps[:], lhsT=hTb[:, kc, tsl], rhs=wgb[:, kc, :], start=(kc == 0), stop=(kc == 15)), reads=[hTb, wgb], writes=[g_ps])
        c.op("act", lambda e: e.activation(out=sg[:], in_=g_ps[:], func=AF.Silu), reads=[g_ps], writes=[sg])

    def up(i):
        ex, tt = items[i]; tsl = slice(tt * 128, (tt + 1) * 128); wub = Wu[ex % 2]
        for kc in range(16):
            c.op("pe", lambda e: e.matmul(u_ps[:], lhsT=hTb[:, kc, tsl], rhs=wub[:, kc, :], start=(kc == 0), stop=(kc == 15)), reads=[hTb, wub], writes=[u_ps])
        c.op("dve", lambda e: e.scalar_tensor_tensor(out=hid[:], in0=sg[:], scalar=CW[:, tt, ex:ex + 1], in1=u_ps[:], op0=ALU.mult, op1=ALU.mult),
             reads=[sg, CW, u_ps], writes=[hid])

    def trans(i):
        for fc in range(4):
            c.op("pe", lambda e: e.transpose(out=tp_ps[:, fc * 128:(fc + 1) * 128], in_=hid[:, fc * 128:(fc + 1) * 128], identity=identb[:]),
                 reads=[hid, identb], writes=[tp_ps])
        c.op("act", lambda e: e.activation(out=hidT[:, :, :], in_=tp_ps[:, :].rearrange("p (f t) -> p f t", f=4), func=AF.Copy), reads=[tp_ps], writes=[hidT])

    def down(i):
        ex, tt = items[i]; wdb = Wd[ex % 2]
        for n in range(4):
            for fc in range(4):
                c.op("pe", lambda e: e.matmul(y_ps[n][:], lhsT=hidT[:, fc, :], rhs=wdb[:, fc, n * 512:(n + 1) * 512],
                                              start=(fc == 0), stop=(fc == 3)), reads=[hidT, wdb], writes=[y_ps[n]])
        for n in range(4):
            ns = slice(n * 512, (n + 1) * 512)
            if ex == 0:
                c.op("act", lambda e: e.activation(out=y_sb[tt][:, ns], in_=y_ps[n][:], func=AF.Copy), reads=[y_ps[n]], writes=[y_sb[tt]])
            else:
                c.op("dve", lambda e: e.tensor_tensor(out=y_sb[tt][:, ns], in0=y_sb[tt][:, ns], in1=y_ps[n][:], op=ALU.add),
                     reads=[y_sb[tt], y_ps[n]], writes=[y_sb[tt]])

    load_w(0)
    gate(0); up(0)
    for i in range(len(items)):
        ex, tt = items[i]
        if tt == 0 and ex + 1 < n_e:
            load_w(ex + 1)
        nxt = i + 1 < len(items)
        if nxt:
            gate(i + 1)
        trans(i)
        if nxt:
            up(i + 1)
        down(i)
    for tt in range(8):
        c.dma("sp", io["yd"].ap[tt * 128:(tt + 1) * 128, :], y_sb[tt][:], reads=[y_sb[tt]], writes=[io["yd"]])
    c.phase_end()
    c.phase_begin()
    r = c.sb([128, 16, TT], F32, "r")
    for i in range(4):
        c.dma("sp", r[:, 4 * i:4 * i + 4, :], hin.ap[512 * i:512 * (i + 1), :].rearrange("(kc p) t -> p kc t", p=128), reads=[hin], writes=[r])
    identf = c.sb([128, 128], F32, "identf")
    c.dma("sp", identf[:], io["identf"][:], reads=[], writes=[identf])
    onesf = c.sb([128, 128], F32, "onesf")
    c.op("pool", lambda e: e.memset(onesf[:], 1.0), writes=[onesf])
    ps = [c.ps([128, 512], F32, f"ps{i}") for i in range(6)]
    yl = [c.sb([128, D], F32, f"yl{i}") for i in range(2)]
    k = 0
    for tt in range(8):
        tsl = slice(tt * 128, (tt + 1) * 128)
        y = yl[tt % 2]
        c.dma("sp", y[:], io["yd"].ap[tt * 128:(tt + 1) * 128, :], reads=[io["yd"]], writes=[y])
        for m in range(16):
            p = ps[4 + (k % 2)]; k += 1
            c.op("pe", lambda e: e.transpose(out=p[:, 0:128], in_=y[:, m * 128:(m + 1) * 128], identity=identf[:]), reads=[y, identf], writes=[p])
            c.op("dve", lambda e: e.scalar_tensor_tensor(out=r[:, m, tsl], in0=r[:, m, tsl], scalar=ALPHA, in1=p[:, 0:128], op0=ALU.mult, op1=ALU.add),
                 reads=[r, p], writes=[r])
    emit_ln(c, r, io["ln2_g"].ap[l], io["ln2_b"].ap[l], hout, ps, onesf)
    c.phase_end()

W_SPECS = (("w_in", [D, IN_W]), ("g_q", [512]), ("w_q_b", [512, 1536]), ("g_kv", [512]), ("w_kv_b", [512, 2048]),
           ("w_o_a", [1024, D]), ("w_o_b", [1024, D]), ("w_out", [D, D]), ("ln1_g", [D]), ("ln1_b", [D]),
           ("w_group", [D, 8]), ("b_group", [8]), ("w_router", [D, 64]), ("b_router", [64]),
           ("w_e_gate", [64, D, 512]), ("w_e_up", [64, D, 512]), ("w_e_down", [64, 512, D]), ("ln2_g", [D]), ("ln2_b", [D]))


def build_fused(depth=2, moe="dense", moe_kw=None, n_exp=64):
    c = Ctx(); nc = c.nc
    io = {}
    io["xT"] = c.dram("xT", [D, TT], F32, "ExternalInput")
    io["posq"] = c.dram("posq", [1, TT], I32, "ExternalInput")
    io["posk"] = c.dram("posk", [1, SEQ], I32, "ExternalInput")
    for nm, shp in W_SPECS:
        if nm.startswith("w_e_"):
            shp = [n_exp] + shp[1:]
        io[nm] = c.dram(nm, [depth] + shp, F32, "ExternalInput")
    io["inv"] = c.dram("inv", [128, 3], F32, "ExternalInput")
    io["perm"] = c.dram("perm", [3, 128, 128], F32, "ExternalInput")
    io["identb"] = c.dram("identb", [128, 128], BF16, "ExternalInput")
    io["identf"] = c.dram("identf", [128, 128], F32, "ExternalInput")
    io["outT"] = c.dram("outT", [D, TT], F32, "ExternalOutput")
    io["qaT"] = c.dram("qaT", [8, 192, TT], BF16); io["qbT"] = c.dram("qbT", [8, 128, TT], BF16); io["qiT"] = c.dram("qiT", [8, 128, TT], BF16)
    io["widx"] = c.dram("widx", [TT, 16], F32)
    io["kvpack"] = c.dram("kvpack", [KVROWS, TT], BF16)
    io["gath"] = None
    for k in range(NCH):
        rows = min(CH, KVROWS - k * CH)
        io["gath%d" % k] = c.dram("gath%d" % k, [4 * rows, TT], BF16)
    io["oaT"] = c.dram("oaT", [1024, TT], BF16); io["obT"] = c.dram("obT", [1024, TT], BF16)
    io["hA"] = c.dram("hA", [D, TT], F32); io["hB"] = c.dram("hB", [D, TT], F32)
    io["cwd"] = c.dram("cwd", [TT, 64], F32); io["yd"] = c.dram("yd", [TT, D], F32)
    hin = io["xT"]
    for l in range(depth):
        c.phase_begin(); emit_A(c, io, l, hin); c.phase_end()
        if UPTO < 1:
            break
        for k in range(NCH):
            rows = min(CH, KVROWS - k * CH)
            c.collective("AllGather", io["kvpack"], io["kvpack"].ap[k * CH:k * CH + rows, :], io["gath%d" % k], io["gath%d" % k].ap[:, :], GROUPS)
        if UPTO < 2:
            break
        c.phase_begin(); emit_B1(c, io); c.phase_end()
        if UPTO < 3:
            break
        c.phase_begin(); emit_B2(c, io, l, hin, io["hA"]); c.phase_end()
        if UPTO < 4:
            break
        hout = io["outT"] if l == depth - 1 else io["hB"]
        if moe == "dense":
            c.phase_begin()
            emit_B3_dense(c, io, l, io["hA"], hout, **(moe_kw or {}))
            c.phase_end()
        else:
            emit_B3(c, io, l, io["hA"], hout, **(moe_kw or {}))
        hin = hout
    c.finish()
    return nc


_CACHE = {}
MOE_MODE = "v2"
UPTO = 99


def kernel(x, positions, w_in, g_q_lora, w_q_b, g_kv_lora, w_kv_b, w_o_a, w_o_b, w_out, ln1_g, ln1_b,
           w_group, b_group, w_router, b_router, w_e_gate, w_e_up, w_e_down, ln2_g, ln2_b):
    x = np.asarray(x); positions = np.asarray(positions)
    depth = int(np.asarray(w_in).shape[0])
    key = ("fused", depth, MOE_MODE)
    if key not in _CACHE:
        _CACHE[key] = build_fused(depth, MOE_MODE)
    nc = _CACHE[key]
    inv, perm = _rope_consts()
    identf = np.eye(128, dtype=np.float32); identb = identf.astype(NPBF)
    wts = {"w_in": w_in, "g_q": g_q_lora, "w_q_b": w_q_b, "g_kv": g_kv_lora, "w_kv_b": w_kv_b, "w_o_a": w_o_a, "w_o_b": w_o_b,
           "w_out": w_out, "ln1_g": ln1_g, "ln1_b": ln1_b, "w_group": w_group, "b_group": b_group, "w_router": w_router,
           "b_router": b_router, "w_e_gate": w_e_gate, "w_e_up": w_e_up, "w_e_down": w_e_down, "ln2_g": ln2_g, "ln2_b": ln2_b}
    wts = {k: np.ascontiguousarray(np.asarray(v), dtype=np.float32) for k, v in wts.items()}
    nb = x.shape[0]
    cores = [(b, j) for b in range(nb) for j in range(4)]
    in_maps = []
    for (b, j) in cores:
        m = dict(wts)
        m["xT"] = np.ascontiguousarray(x[b, j * TT:(j + 1) * TT, :].T)
        m["posq"] = np.ascontiguousarray(positions[b, j * TT:(j + 1) * TT][None, :]).astype(np.int32)
        m["posk"] = np.ascontiguousarray(positions[b][None, :]).astype(np.int32)
        m["inv"] = inv; m["perm"] = perm; m["identb"] = identb; m["identf"] = identf
        in_maps.append(m)
    res = run_bass_kernel_spmd(nc, in_maps, core_ids=list(range(len(cores)))).results
    out = np.empty(x.shape, np.float32)
    for ci, (b, j) in enumerate(cores):
        out[b, j * TT:(j + 1) * TT, :] = res[ci]["outT"].T
    return out
```
